# Optimizing a Trainium2 kernel written in Bass

```python
import math
import jax, jax.numpy as jnp
from jax import lax
import numpy as np

D_MODEL = 1024
BATCH = 8
SEQ = 4096
DEPTH = 1

MIX_WIDTH = D_MODEL
ATTN_WIDTH = MIX_WIDTH // 2
LRU_WIDTH = MIX_WIDTH - ATTN_WIDTH
DIFF_HEAD_DIM = 64
DIFF_V_DIM = 2 * DIFF_HEAD_DIM
N_DIFF_HEADS = ATTN_WIDTH // DIFF_V_DIM
ROPE_DIM = DIFF_HEAD_DIM // 4
ROPE_THETA = 500000.0
Q_BLOCK = 128
LRU_BLOCKS = 8
LRU_BLOCK_WIDTH = LRU_WIDTH // LRU_BLOCKS
CONV_WIDTH = 4
CONV_PAD = (2, 1)
LRU_C = 8.0
N_DIRS = 2
N_KEYS = 128
N_EXPERTS = N_KEYS * N_KEYS
PEER_HEADS = 8
PEER_TOPK = 16
DIM_KEY = 128
HALF_KEY = DIM_KEY // 2
PEER_BLOCK = 128
PLE_DIM = 256
POS_OFFSET_MAX = 1024
EPS = 1e-6

Q_COLS = N_DIFF_HEADS * 2 * DIFF_HEAD_DIM
K_COLS = N_DIFF_HEADS * 2 * DIFF_HEAD_DIM
V_COLS = N_DIFF_HEADS * DIFF_V_DIM
IN_COLS = Q_COLS + K_COLS + V_COLS + 2 * LRU_WIDTH

kernel_name = "hybrid_diffattn_rglru_peer_encoder"


def rms_norm(x, g):
    xf = x.astype(jnp.float32)
    y = xf * lax.rsqrt(jnp.mean(xf * xf, axis=-1, keepdims=True) + EPS)
    return (y * g.astype(jnp.float32)).astype(x.dtype)


def rope_cos_sin(positions):
    inv_freq = ROPE_THETA ** (-jnp.arange(0, ROPE_DIM, 2, dtype=jnp.float32) / ROPE_DIM)
    ang = positions.astype(jnp.float32)[..., None] * inv_freq
    return jnp.cos(ang), jnp.sin(ang)


def apply_partial_rope(t, cos, sin):
    half = ROPE_DIM // 2
    c = cos[:, :, None, None, :]
    s = sin[:, :, None, None, :]
    tf = t.astype(jnp.float32)
    x1 = tf[..., :half]
    x2 = tf[..., half:ROPE_DIM]
    rot = jnp.concatenate([x1 * c - x2 * s, x2 * c + x1 * s], axis=-1)
    return jnp.concatenate([rot, tf[..., ROPE_DIM:]], axis=-1).astype(t.dtype)


def diff_attention(q, k, v, lam, positions):
    B, S = q.shape[0], q.shape[1]
    cos, sin = rope_cos_sin(positions)
    q = apply_partial_rope(q, cos, sin) * (DIFF_HEAD_DIM ** -0.5)
    k = apply_partial_rope(k, cos, sin)
    kh = k.transpose(0, 2, 3, 1, 4)
    vh = v.transpose(0, 2, 1, 3)
    nb = S // Q_BLOCK
    q_blocks = q.transpose(0, 2, 3, 1, 4).reshape(
        B, N_DIFF_HEADS, 2, nb, Q_BLOCK, DIFF_HEAD_DIM).transpose(3, 0, 1, 2, 4, 5)

    def block(qb):
        s = jnp.einsum('bhmqd,bhmkd->bhmqk', qb, kh).astype(jnp.float32)
        pr = jax.nn.softmax(s, axis=-1)
        w = pr[:, :, 0] - lam * pr[:, :, 1]
        return jnp.einsum('bhqk,bhkd->bhqd', w.astype(vh.dtype), vh)

    o = lax.map(block, q_blocks)
    return o.transpose(1, 0, 3, 2, 4).reshape(B, S, N_DIFF_HEADS, DIFF_V_DIM)


def _lin_combine(left, right):
    a_l, b_l = left
    a_r, b_r = right
    return a_l * a_r, a_r * b_l + b_r


def block_diag_linear(u, w, b):
    B, S = u.shape[0], u.shape[1]
    ub = u.reshape(B, S, LRU_BLOCKS, LRU_BLOCK_WIDTH)
    y = jnp.einsum('bsnc,nce->bsne', ub, w).reshape(B, S, LRU_WIDTH)
    return y + b


def rg_lru_direction(u, wa, ba, wx, bx, lam, reverse):
    uf = u.astype(jnp.float32)
    r = jax.nn.sigmoid(block_diag_linear(u, wa, ba).astype(jnp.float32))
    i = jax.nn.sigmoid(block_diag_linear(u, wx, bx).astype(jnp.float32))
    log_a = -LRU_C * r * jax.nn.softplus(-lam.astype(jnp.float32))
    a = jnp.exp(log_a)
    mult = jnp.sqrt(-jnp.expm1(2.0 * log_a))
    bterm = mult * (i * uf)
    _, h = lax.associative_scan(_lin_combine, (a, bterm), reverse=reverse, axis=1)
    return h


def bi_rglru(u, gate, conv_w, conv_b, lru_wa, lru_ba, lru_wx, lru_bx, lru_lambda):
    C = u.shape[-1]
    uc = lax.conv_general_dilated(
        u, conv_w[:, None, :].astype(u.dtype), window_strides=(1,), padding=[CONV_PAD],
        dimension_numbers=('NWC', 'WIO', 'NWC'), feature_group_count=C) + conv_b
    h_f = rg_lru_direction(uc, lru_wa[0], lru_ba[0], lru_wx[0], lru_bx[0], lru_lambda[0], False)
    h_b = rg_lru_direction(uc, lru_wa[1], lru_ba[1], lru_wx[1], lru_bx[1], lru_lambda[1], True)
    y = (h_f + h_b) * jax.nn.gelu(gate.astype(jnp.float32))
    return y.astype(u.dtype)


def peer_layer(xn, wq, keys1, keys2, exp_u, exp_v):
    B, S, D = xn.shape
    q = (xn @ wq).reshape(B, S, PEER_HEADS, 2, HALF_KEY)
    s1 = jnp.einsum('bshd,kd->bshk', q[..., 0, :], keys1).astype(jnp.float32)
    s2 = jnp.einsum('bshd,kd->bshk', q[..., 1, :], keys2).astype(jnp.float32)
    v1, i1 = lax.top_k(s1, PEER_TOPK)
    v2, i2 = lax.top_k(s2, PEER_TOPK)
    cand = (v1[..., :, None] + v2[..., None, :]).reshape(B, S, PEER_HEADS, PEER_TOPK * PEER_TOPK)
    cand_idx = (i1[..., :, None] * N_KEYS + i2[..., None, :]).reshape(
        B, S, PEER_HEADS, PEER_TOPK * PEER_TOPK)
    sc, pos = lax.top_k(cand, PEER_TOPK)
    idx = jnp.take_along_axis(cand_idx, pos, axis=-1)
    g = jax.nn.softmax(sc, axis=-1)

    T = B * S
    nb = T // PEER_BLOCK
    NK = PEER_HEADS * PEER_TOPK
    xt = xn.reshape(nb, PEER_BLOCK, D)
    idx_t = idx.reshape(nb, PEER_BLOCK, NK)
    g_t = g.reshape(nb, PEER_BLOCK, NK)

    def block(args):
        xb, ib, gb = args
        ue = exp_u[ib]
        logits = jnp.einsum('td,tkd->tk', xb, ue).astype(jnp.float32)
        act = (jax.nn.gelu(logits) * gb).astype(xb.dtype)
        ve = exp_v[ib]
        return jnp.einsum('tk,tkd->td', act, ve)

    out = lax.map(block, (xt, idx_t, g_t))
    return out.reshape(B, S, D)


def setup_inputs(seed: int = 0) -> dict:
    key = jax.random.key(seed)
    ks = iter(jax.random.split(key, 40))

    def nrm(shape, scale):
        return jax.random.normal(next(ks), shape, jnp.float32) * scale

    def gain(shape):
        return 1.0 + nrm(shape, 0.01)

    x = nrm((BATCH, SEQ, D_MODEL), 1.0)
    p = nrm((DEPTH, BATCH, SEQ, PLE_DIM), 1.0)
    positions = (jnp.arange(SEQ, dtype=jnp.int32)[None, :]
                 + jax.random.randint(next(ks), (BATCH, 1), 0, POS_OFFSET_MAX, dtype=jnp.int32))
    u_a = jax.random.uniform(next(ks), (DEPTH, N_DIRS, LRU_WIDTH), jnp.float32, 0.9, 0.999)
    a0 = u_a ** (1.0 / LRU_C)
    lru_lambda = jnp.log(a0) - jnp.log1p(-a0)
    return {
        "x": x,
        "p": p,
        "positions": positions,
        "norm_mix_g": gain((DEPTH, D_MODEL)),
        "w_in": nrm((DEPTH, D_MODEL, IN_COLS), D_MODEL ** -0.5),
        "lambda_q1": nrm((DEPTH, DIFF_HEAD_DIM), 0.1),
        "lambda_k1": nrm((DEPTH, DIFF_HEAD_DIM), 0.1),
        "lambda_q2": nrm((DEPTH, DIFF_HEAD_DIM), 0.1),
        "lambda_k2": nrm((DEPTH, DIFF_HEAD_DIM), 0.1),
        "diff_norm_g": gain((DEPTH, DIFF_V_DIM)),
        "conv_w": nrm((DEPTH, CONV_WIDTH, LRU_WIDTH), CONV_WIDTH ** -0.5),
        "conv_b": nrm((DEPTH, LRU_WIDTH), 0.01),
        "lru_wa": nrm((DEPTH, N_DIRS, LRU_BLOCKS, LRU_BLOCK_WIDTH, LRU_BLOCK_WIDTH), LRU_BLOCK_WIDTH ** -0.5),
        "lru_ba": nrm((DEPTH, N_DIRS, LRU_WIDTH), 0.01),
        "lru_wx": nrm((DEPTH, N_DIRS, LRU_BLOCKS, LRU_BLOCK_WIDTH, LRU_BLOCK_WIDTH), LRU_BLOCK_WIDTH ** -0.5),
        "lru_bx": nrm((DEPTH, N_DIRS, LRU_WIDTH), 0.01),
        "lru_lambda": lru_lambda,
        "lru_norm_g": gain((DEPTH, LRU_WIDTH)),
        "w_out": nrm((DEPTH, MIX_WIDTH, D_MODEL), MIX_WIDTH ** -0.5),
        "norm_ffn_g": gain((DEPTH, D_MODEL)),
        "peer_wq": nrm((DEPTH, D_MODEL, PEER_HEADS * DIM_KEY), D_MODEL ** -0.5),
        "peer_keys1": nrm((DEPTH, N_KEYS, HALF_KEY), HALF_KEY ** -0.5),
        "peer_keys2": nrm((DEPTH, N_KEYS, HALF_KEY), HALF_KEY ** -0.5),
        "peer_u": nrm((DEPTH, N_EXPERTS, D_MODEL), D_MODEL ** -0.5),
        "peer_v": nrm((DEPTH, N_EXPERTS, D_MODEL), PEER_HEADS ** -0.5),
        "norm_ple_g": gain((DEPTH, D_MODEL)),
        "ple_w_gate": nrm((DEPTH, D_MODEL, D_MODEL), D_MODEL ** -0.5),
        "ple_w_proj": nrm((DEPTH, PLE_DIM, D_MODEL), PLE_DIM ** -0.5),
        "final_norm_g": gain((D_MODEL,)),
    }


def reference(x, p, positions, norm_mix_g, w_in, lambda_q1, lambda_k1, lambda_q2, lambda_k2,
              diff_norm_g, conv_w, conv_b, lru_wa, lru_ba, lru_wx, lru_bx, lru_lambda,
              lru_norm_g, w_out, norm_ffn_g, peer_wq, peer_keys1, peer_keys2, peer_u, peer_v,
              norm_ple_g, ple_w_gate, ple_w_proj, final_norm_g):
    B, S, _ = x.shape
    h = x
    for i in range(DEPTH):
        lambda_init = 0.8 - 0.6 * math.exp(-0.3 * i)
        xn = rms_norm(h, norm_mix_g[i])
        proj = xn @ w_in[i]
        q = proj[..., :Q_COLS].reshape(B, S, N_DIFF_HEADS, 2, DIFF_HEAD_DIM)
        k = proj[..., Q_COLS:Q_COLS + K_COLS].reshape(B, S, N_DIFF_HEADS, 2, DIFF_HEAD_DIM)
        o0 = Q_COLS + K_COLS
        v = proj[..., o0:o0 + V_COLS].reshape(B, S, N_DIFF_HEADS, DIFF_V_DIM)
        o1 = o0 + V_COLS
        lru_in = proj[..., o1:o1 + LRU_WIDTH]
        lru_gate = proj[..., o1 + LRU_WIDTH:o1 + 2 * LRU_WIDTH]

        lam = (jnp.exp(jnp.sum(lambda_q1[i].astype(jnp.float32) * lambda_k1[i].astype(jnp.float32)))
               - jnp.exp(jnp.sum(lambda_q2[i].astype(jnp.float32) * lambda_k2[i].astype(jnp.float32)))
               + lambda_init)
        attn = diff_attention(q, k, v, lam, positions)
        attn = (rms_norm(attn, diff_norm_g[i]) * (1.0 - lambda_init)).reshape(B, S, ATTN_WIDTH)

        rec = bi_rglru(lru_in, lru_gate, conv_w[i], conv_b[i], lru_wa[i], lru_ba[i],
                       lru_wx[i], lru_bx[i], lru_lambda[i])
        rec = rms_norm(rec, lru_norm_g[i])

        mixed = jnp.concatenate([attn.astype(h.dtype), rec.astype(h.dtype)], axis=-1)
        h = h + mixed @ w_out[i]

        xn2 = rms_norm(h, norm_ffn_g[i])
        h = h + peer_layer(xn2, peer_wq[i], peer_keys1[i], peer_keys2[i], peer_u[i], peer_v[i])

        xn3 = rms_norm(h, norm_ple_g[i])
        gate = jax.nn.sigmoid((xn3 @ ple_w_gate[i]).astype(jnp.float32)).astype(h.dtype)
        h = h + gate * (p[i] @ ple_w_proj[i])
    return rms_norm(h, final_norm_g)
```

```python
import math
import contextlib
import numpy as np
import concourse.bass as bass
import concourse.mybir as mybir
from concourse.bass_utils import run_bass_kernel_spmd

F32 = mybir.dt.float32
BF16 = mybir.dt.bfloat16
I32 = mybir.dt.int32
U32 = mybir.dt.uint32
AF = mybir.ActivationFunctionType
ALU = mybir.AluOpType
AX = mybir.AxisListType

SB_LO = 16512
SB_HI = 229344
EPS = 1e-6
D = 1024
NEXP = 16384
PI = math.pi


class Tok:
    __slots__ = ("w", "r")

    def __init__(self):
        self.w = None
        self.r = {}


class Src:
    def __init__(self, sem, name):
        self.sem = sem
        self.name = name
        self.count = 0


class Eng(Src):
    def __init__(self, K, h, sem, name):
        super().__init__(sem, name)
        self.K = K
        self.h = h
        self.seen = {}

    def wait(self, ev):
        if ev is None:
            return
        src, val = ev
        if self.seen.get(src, 0) >= val:
            return
        self.h.wait_ge(src.sem, val)
        self.seen[src] = val

    def _deps(self, reads, writes, extra):
        for t in reads:
            if t.w is not None and t.w[0] is self and self is self.K.pe:
                continue
            if t.w is not None and t.w[0] is self and self is self.K.dve and self.count - t.w[1] >= 8:
                continue
            self.wait(t.w)
        for t in writes:
            if t.w is not None and t.w[0] is not self:
                self.wait(t.w)
            for s, v in t.r.items():
                if s is not self:
                    self.wait((s, v))
        for e in extra:
            self.wait(e)

    def _mark(self, ev, reads, writes):
        for t in reads:
            if t.r.get(ev[0], 0) < ev[1]:
                t.r[ev[0]] = ev[1]
        for t in writes:
            t.w = ev
            t.r = {}

    def op(self, fn, reads=(), writes=(), extra=()):
        self._deps(reads, writes, extra)
        ins = fn(self.h)
        self.count += 1
        ins.then_inc(self.sem, 1)
        ev = (self, self.count)
        self._mark(ev, reads, writes)
        return ev

    def dma(self, pairs, reads=(), writes=(), extra=(), fn=None):
        K = self.K
        ds = K.next_dsem()
        if ds.count:
            self.wait((ds, ds.count))
        self._deps(reads, writes, extra)
        for (o, i) in pairs:
            if fn is not None:
                ins = fn(self.h, o, i)
            else:
                ins = self.h.dma_start(out=o, in_=i)
            ds.count += 16
            ins.then_inc(ds.sem, 16)
        ev = (ds, ds.count)
        self._mark(ev, reads, writes)
        return ev


class Kern:
    def __init__(self, nc, es, n_dsem=56):
        self.nc = nc
        self.es = es
        mk = lambda n: es.enter_context(nc.semaphore(n))
        self.pe = Eng(self, nc.tensor, mk("s_pe"), "pe")
        self.act = Eng(self, nc.scalar, mk("s_act"), "act")
        self.dve = Eng(self, nc.vector, mk("s_dve"), "dve")
        self.pool = Eng(self, nc.gpsimd, mk("s_pool"), "pool")
        self.sp = Eng(self, nc.sync, mk("s_sp"), "sp")
        self.engs = [self.pe, self.act, self.dve, self.pool, self.sp]
        self.dsems = [Src(mk(f"s_d{i}"), f"d{i}") for i in range(n_dsem)]
        self.di = 0
        self.sb_off = SB_LO
        self.sb_peak = SB_LO
        self.n_alloc = 0
        self.pes = None

    def next_dsem(self):
        d = self.dsems[self.di]
        self.di = (self.di + 1) % len(self.dsems)
        return d

    def sb(self, shape, dt, name=None, at=None):
        esz = {F32: 4, BF16: 2, I32: 4, U32: 4}[dt]
        n = 1
        for s in shape[1:]:
            n *= s
        nbytes = (n * esz + 63) // 64 * 64
        if at is not None:
            self.n_alloc += 1
            return self.nc.alloc_sbuf_tensor_at(f"{name or 't'}_{self.n_alloc}", list(shape), dt, offset=at)
        off = self.sb_off
        assert off + nbytes <= SB_HI, f"SBUF overflow {name} {off}+{nbytes}"
        self.sb_off += nbytes
        self.sb_peak = max(self.sb_peak, self.sb_off)
        self.n_alloc += 1
        return self.nc.alloc_sbuf_tensor_at(f"{name or 't'}_{self.n_alloc}", list(shape), dt, offset=off)

    def mark(self):
        return self.sb_off

    def release(self, m):
        self.sb_off = m

    def ps(self, shape, dt=F32, name=None):
        self.n_alloc += 1
        return self.pes.enter_context(self.nc.psum_tensor(f"{name or 'ps'}_{self.n_alloc}", list(shape), dt))

    def barrier(self):
        srcs = [(e, e.count) for e in self.engs if e.count] + [(d, d.count) for d in self.dsems if d.count]
        for e in self.engs:
            for ev in srcs:
                e.wait(ev)


def build(S, debug=False, stop_after=None):
    NT = S // 128
    NG = S // 512
    L = min(1024, S)
    NSEG = S // L
    nc = bass.Bass("TRN2", target_bir_lowering=False)

    def din(n, s, d=F32):
        return nc.dram_tensor(n, list(s), d, kind="ExternalInput").ap()

    x_d = din("x", [S, D])
    p_d = din("p", [S, 256])
    pos_d = din("pos", [128, NT], I32)
    cst_d = din("consts", [128, 152])
    w_in_d = din("w_in", [D, 2560])
    w_out_d = din("w_out", [D, D])
    wq_d = din("peer_wq", [D, D])
    wg_d = din("ple_w_gate", [D, D])
    wp_d = din("ple_w_proj", [256, D])
    gmix_d = din("norm_mix_g", [1, D])
    gffn_d = din("norm_ffn_g", [1, D])
    gple_d = din("norm_ple_g", [1, D])
    gfin_d = din("final_norm_g", [1, D])
    gdiff_d = din("diff_norm_g", [1, 128])
    lam_d = din("lam4", [1, 256])
    colp_d = din("colp", [128, 48])
    wa_d = din("lru_wa", [2, 8, 64, 64])
    wx_d = din("lru_wx", [2, 8, 64, 64])
    k1T_d = din("keys1T", [64, 128])
    k2T_d = din("keys2T", [64, 128])
    pu_d = din("peer_u", [NEXP, D])
    pv_d = din("peer_v", [NEXP, D])
    out_d = nc.dram_tensor("out", [S, D], F32, kind="ExternalOutput").ap()
    lru_scr = nc.dram_tensor("lru_scr", [1024, S], F32, kind="Internal").ap()
    h1_scr = nc.dram_tensor("h1_scr", [S, D], F32, kind="Internal").ap()
    puv_bf = nc.dram_tensor("puv_bf", [NEXP, 2 * D], BF16, kind="Internal").ap()

    with contextlib.ExitStack() as es:
        K = Kern(nc, es)
        pe, act, dve, pool, sp = K.pe, K.act, K.dve, K.pool, K.sp

        def cast_load(dst3, src2, nk, ncol, tok):
            pairs = []
            for kc in range(nk):
                for c0 in range(0, ncol, 1024):
                    c1 = min(ncol, c0 + 1024)
                    pairs.append((dst3[:, kc, c0:c1], src2[kc * 128:(kc + 1) * 128, c0:c1]))
            return pool.dma(pairs, writes=[tok])

        def bcast_load(dst, src_row, tok):
            return sp.dma([(dst, src_row.partition_broadcast(128))], writes=[tok])

        cst = K.sb([128, 152], F32, "cst")
        t_cst = Tok()
        sp.dma([(cst[:], cst_d)], writes=[t_cst])
        ident = K.sb([128, 128], BF16, "ident")
        t_id = Tok()
        dve.op(lambda e: e.tensor_copy(out=ident[:], in_=cst[:, 0:128]), reads=[t_cst], writes=[t_id])
        iota16 = cst[:, 128:144]
        invf = cst[:, 144:152]
        one_c = K.sb([128, 1], F32, "one")
        t_one = Tok()
        dve.op(lambda e: e.memset(one_c[:], 1.0), writes=[t_one])
        ones_bf = K.sb([128, 1], BF16, "onesbf")
        t_onesbf = Tok()
        dve.op(lambda e: e.memset(ones_bf[:], 1.0), writes=[t_onesbf])
        junk = K.sb([128, 1024], F32, "junk")
        t_junk = Tok()
        rstd_rec = K.sb([128, NT], F32, "rstdrec")
        t_rstd_rec = Tok()

        def rstd_from_ssq(ssq_ap, n, out_ap, tok_in, tok_out, shape_tmp):
            dve.op(lambda e: e.tensor_scalar(out=out_ap, in0=ssq_ap, scalar1=1.0 / n, scalar2=EPS,
                                             op0=ALU.mult, op1=ALU.add), reads=[tok_in], writes=[tok_out])
            act.op(lambda e: e.activation(out=out_ap, in_=out_ap, func=AF.Sqrt), reads=[tok_out], writes=[tok_out])
            dve.op(lambda e: e.reciprocal(out=out_ap, in_=out_ap), reads=[tok_out], writes=[tok_out])

        lamt = K.sb([128, 256], F32, "lamt")
        t_lam = Tok()
        bcast_load(lamt[:], lam_d[0], t_lam)
        lsum = K.sb([128, 2], F32, "lsum")
        neglam = K.sb([128, 1], F32, "neglam")
        t_nl = Tok()
        dve.op(lambda e: e.scalar_tensor_tensor(out=junk[:, 0:64], in0=lamt[:, 0:64], scalar=1.0, in1=lamt[:, 64:128],
                                                op0=ALU.mult, op1=ALU.mult, accum_out=lsum[:, 0:1]),
               reads=[t_lam], writes=[t_nl, t_junk])
        dve.op(lambda e: e.scalar_tensor_tensor(out=junk[:, 0:64], in0=lamt[:, 128:192], scalar=1.0, in1=lamt[:, 192:256],
                                                op0=ALU.mult, op1=ALU.mult, accum_out=lsum[:, 1:2]),
               reads=[t_lam], writes=[t_nl, t_junk])
        act.op(lambda e: e.activation(out=lsum[:], in_=lsum[:], func=AF.Exp), reads=[t_nl], writes=[t_nl])
        dve.op(lambda e: e.tensor_tensor(out=neglam[:], in0=lsum[:, 1:2], in1=lsum[:, 0:1], op=ALU.subtract),
               reads=[t_nl], writes=[t_nl])
        dve.op(lambda e: e.tensor_scalar(out=neglam[:], in0=neglam[:], scalar1=-0.2, scalar2=None, op0=ALU.add),
               reads=[t_nl], writes=[t_nl])

        posi = K.sb([128, NT], I32, "posi")
        t_pos = Tok()
        sp.dma([(posi[:], pos_d)], writes=[t_pos])
        posf = K.sb([128, NT], F32, "posf")
        ang = K.sb([128, NT, 8], F32, "ang")
        t_ang = Tok()
        dve.op(lambda e: e.tensor_copy(out=posf[:], in_=posi[:]), reads=[t_pos], writes=[t_ang])
        dve.op(lambda e: e.tensor_tensor(out=ang[:], in0=posf[:].unsqueeze(2).broadcast_to([128, NT, 8]),
                                         in1=invf.unsqueeze(1).broadcast_to([128, NT, 8]), op=ALU.mult),
               reads=[t_ang, t_cst], writes=[t_ang])
        cosk = K.sb([128, NT, 8], F32, "cosk")
        sink = K.sb([128, NT, 8], F32, "sink")
        cosq = K.sb([128, NT, 8], F32, "cosq")
        sinq = K.sb([128, NT, 8], F32, "sinq")
        t_trig = Tok()
        colp = K.sb([128, 48], F32, "colp")
        scl = K.sb([128, 4, 2], F32, "scl")
        scl2 = K.sb([128, 4, 2], F32, "scl2")
        Wbd = K.sb([128, 16, 128], BF16, "Wbd")
        keysbd = K.sb([128, 256], BF16, "keysbd")
        m_trig = K.mark()
        wst = K.sb([128, 16, 128], F32, "wst")
        kst = K.sb([128, 256], F32, "kst")
        tr_a = K.sb([128, NT, 8], F32)
        tr_n = K.sb([128, NT, 8], F32)
        tr_i = K.sb([128, NT, 8], I32)
        tr_m = K.sb([128, NT, 8], F32)
        t_tr = Tok()
        for dst, shift in ((sink, 0.0), (cosk, PI / 2)):
            dve.op(lambda e: e.tensor_scalar(out=tr_a[:], in0=ang[:], scalar1=shift, scalar2=None, op0=ALU.add),
                   reads=[t_ang], writes=[t_tr])
            dve.op(lambda e: e.tensor_scalar(out=tr_n[:], in0=tr_a[:], scalar1=1.0 / (2 * PI), scalar2=None, op0=ALU.mult),
                   reads=[t_tr], writes=[t_tr])
            dve.op(lambda e: e.tensor_copy(out=tr_i[:], in_=tr_n[:]), reads=[t_tr], writes=[t_tr])
            dve.op(lambda e: e.tensor_copy(out=tr_n[:], in_=tr_i[:]), reads=[t_tr], writes=[t_tr])
            dve.op(lambda e: e.scalar_tensor_tensor(out=tr_a[:], in0=tr_n[:], scalar=-2 * PI, in1=tr_a[:],
                                                    op0=ALU.mult, op1=ALU.add), reads=[t_tr], writes=[t_tr])
            dve.op(lambda e: e.tensor_single_scalar(out=tr_m[:], in_=tr_a[:], scalar=PI, op=ALU.is_gt),
                   reads=[t_tr], writes=[t_tr])
            dve.op(lambda e: e.scalar_tensor_tensor(out=tr_a[:], in0=tr_m[:], scalar=-2 * PI, in1=tr_a[:],
                                                    op0=ALU.mult, op1=ALU.add), reads=[t_tr], writes=[t_tr])
            dve.op(lambda e: e.tensor_single_scalar(out=tr_m[:], in_=tr_a[:], scalar=-PI, op=ALU.is_lt),
                   reads=[t_tr], writes=[t_tr])
            dve.op(lambda e: e.scalar_tensor_tensor(out=tr_a[:], in0=tr_m[:], scalar=2 * PI, in1=tr_a[:],
                                                    op0=ALU.mult, op1=ALU.add), reads=[t_tr], writes=[t_tr])
            dve.op(lambda e: e.tensor_scalar(out=tr_a[:], in0=tr_a[:], scalar1=3.1415925, scalar2=-3.1415925,
                                             op0=ALU.min, op1=ALU.max), reads=[t_tr], writes=[t_tr])
            act.op(lambda e: e.activation(out=dst[:], in_=tr_a[:], func=AF.Sin), reads=[t_tr], writes=[t_trig])
        dve.op(lambda e: e.tensor_scalar(out=cosq[:], in0=cosk[:], scalar1=0.125, scalar2=None, op0=ALU.mult),
               reads=[t_trig], writes=[t_trig])
        dve.op(lambda e: e.tensor_scalar(out=sinq[:], in0=sink[:], scalar1=0.125, scalar2=None, op0=ALU.mult),
               reads=[t_trig], writes=[t_trig])
        t_colp = Tok()
        sp.dma([(colp[:], colp_d)], writes=[t_colp])
        colp3 = colp[:].rearrange("p (c k) -> p c k", k=12)
        t_scl = Tok()
        act.op(lambda e: e.activation(out=scl[:], in_=colp3[:, :, 9:11], func=AF.Exp, scale=-1.0), reads=[t_colp], writes=[t_scl])
        dve.op(lambda e: e.tensor_scalar(out=scl[:], in0=scl[:], scalar1=1.0, scalar2=None, op0=ALU.add), reads=[t_scl], writes=[t_scl])
        act.op(lambda e: e.activation(out=scl[:], in_=scl[:], func=AF.Ln), reads=[t_scl], writes=[t_scl])
        dve.op(lambda e: e.tensor_scalar(out=scl2[:], in0=scl[:], scalar1=-16.0, scalar2=None, op0=ALU.mult), reads=[t_scl], writes=[t_scl])
        dve.op(lambda e: e.tensor_scalar(out=scl[:], in0=scl[:], scalar1=-8.0, scalar2=None, op0=ALU.mult), reads=[t_scl], writes=[t_scl])
        t_wst, t_Wbd = Tok(), Tok()
        dve.op(lambda e: e.memset(wst[:], 0.0), writes=[t_wst])
        pairs = []
        for typ, wd in ((0, wa_d), (1, wx_d)):
            for d in range(2):
                base = (typ * 2 + d) * 4
                for ehalf in range(2):
                    src = wd[d, ehalf::2].rearrange("n i o -> i n o")
                    pairs.append((wst[ehalf * 64:(ehalf + 1) * 64, base:base + 4, ehalf * 64:(ehalf + 1) * 64], src))
        sp.dma(pairs, writes=[t_wst])
        dve.op(lambda e: e.tensor_copy(out=Wbd[:], in_=wst[:]), reads=[t_wst], writes=[t_Wbd])

        t_kst, t_keys = Tok(), Tok()
        dve.op(lambda e: e.memset(kst[:], 0.0), writes=[t_kst])
        sp.dma([(kst[0:64, 0:128], k1T_d), (kst[64:128, 128:256], k2T_d)], writes=[t_kst])
        dve.op(lambda e: e.tensor_copy(out=keysbd[:], in_=kst[:]), reads=[t_kst], writes=[t_keys])

        K.barrier()
        K.release(m_trig)
        if stop_after == "setup":
            return nc

        attnT = K.sb([128, 4, S], BF16, "attnT", at=SB_HI - 8 * S)
        yT = K.sb([128, 4, S], BF16, "yT", at=SB_HI - 16 * S)
        t_attnT = Tok()
        t_yT = Tok()

        t_xr = Tok()

        def rope(ps3, dst3, ct, st, t, t_ps, t_dst, tmp):
            C = ct[:, t, :].unsqueeze(1).broadcast_to([128, 8, 8])
            Sn = st[:, t, :].unsqueeze(1).broadcast_to([128, 8, 8])
            t1, t2, t3, t4, xr = tmp
            tt = Tok()
            act.op(lambda e: e.activation(out=xr[:], in_=ps3[:, :, 0:16], func=AF.Copy), reads=[t_ps], writes=[t_xr])
            t_ps = t_xr
            x1 = xr[:, :, 0:8]
            x2 = xr[:, :, 8:16]
            dve.op(lambda e: e.tensor_tensor(out=t1[:], in0=x1, in1=C, op=ALU.mult), reads=[t_ps, t_trig], writes=[tt])
            dve.op(lambda e: e.tensor_tensor(out=t2[:], in0=x2, in1=Sn, op=ALU.mult), reads=[t_ps, t_trig], writes=[tt])
            dve.op(lambda e: e.tensor_tensor(out=t3[:], in0=x2, in1=C, op=ALU.mult), reads=[t_ps, t_trig], writes=[tt])
            dve.op(lambda e: e.tensor_tensor(out=t4[:], in0=x1, in1=Sn, op=ALU.mult), reads=[t_ps, t_trig], writes=[tt])
            dve.op(lambda e: e.tensor_tensor(out=dst3[:, :, 0:8], in0=t1[:], in1=t2[:], op=ALU.subtract),
                   reads=[tt], writes=[t_dst])
            dve.op(lambda e: e.tensor_tensor(out=dst3[:, :, 8:16], in0=t3[:], in1=t4[:], op=ALU.add),
                   reads=[tt], writes=[t_dst])

        mA = K.mark()
        QT = K.sb([128, 4, S], BF16, "QT")
        KT = K.sb([128, 4, S], BF16, "KT")
        Vs = K.sb([128, NT, 4, 129], BF16, "V")
        t_QT, t_KT, t_V = Tok(), Tok(), Tok()
        dve.op(lambda e: e.memset(Vs[:, :, :, 128:129], 1.0), writes=[t_V])
        mA2 = K.mark()
        w_in_sb = K.sb([128, 8, 2560], BF16, "w_in")
        t_win = Tok()
        t_win_l = Tok()
        pool.dma([(w_in_sb[:, kc, c0:c0 + 768], w_in_d[kc * 128:(kc + 1) * 128, c0:c0 + 768])
                  for kc in range(8) for c0 in (0, 768)], writes=[t_win])
        pool.dma([(w_in_sb[:, kc, 1536:2560], w_in_d[kc * 128:(kc + 1) * 128, 1536:2560]) for kc in range(8)], writes=[t_win_l])
        gmix = K.sb([128, D], F32, "gmix")
        t_gmix = Tok()
        bcast_load(gmix[:], gmix_d[0], t_gmix)
        xts = [K.sb([128, D], F32, f"xt{i}") for i in range(4)]
        t_xt = [Tok() for _ in range(4)]
        xnb = [K.sb([128, D], BF16, f"xnb{i}") for i in range(2)]
        t_xnb = [Tok() for _ in range(2)]
        xnT = [K.sb([128, 8, 512], BF16, f"xnT{i}") for i in range(2)]
        t_xnT = [Tok() for _ in range(2)]
        ssqA = K.sb([128, 4], F32, "ssqA")
        rstdA = K.sb([128, 4], F32, "rstdA")
        t_ssqA, t_rstdA = Tok(), Tok()
        q_sb = K.sb([128, 8, 64], BF16, "q_sb")
        k_sb = K.sb([128, 8, 64], BF16, "k_sb")
        t_qsb, t_ksb = Tok(), Tok()
        rtmp = [K.sb([128, 8, 8], F32) for _ in range(4)] + [K.sb([128, 8, 16], F32)]
        stage = [K.sb([128, 512], F32, f"stg{i}") for i in range(3)]
        t_stage = [Tok() for _ in range(3)]
        if stop_after == "A0":
            K.barrier()
            return nc
        with contextlib.ExitStack() as pes:
            K.pes = pes
            ps_tr = [K.ps([128, 8, 128], BF16) for _ in range(2)]
            t_ps_tr = [Tok(), Tok()]
            ps_qkv = [K.ps([128, 512], F32) for _ in range(3)]
            t_ps_qkv = [Tok() for _ in range(3)]
            ps_qk = K.ps([128, 8, 128], BF16)
            t_ps_qk = Tok()
            ps_l = [K.ps([128, 512], F32) for _ in range(2)]
            t_ps_l = [Tok(), Tok()]
            itr = 0
            il = 0
            for g in range(NG):
                b = g % 2
                for j in range(4):
                    t = 4 * g + j
                    sp.dma([(xts[j][:], x_d[t * 128:(t + 1) * 128, :])], writes=[t_xt[j]])
                    dve.op(lambda e: e.scalar_tensor_tensor(out=junk[:], in0=xts[j][:], scalar=1.0, in1=xts[j][:],
                                                            op0=ALU.mult, op1=ALU.mult, accum_out=ssqA[:, j:j + 1]),
                           reads=[t_xt[j]], writes=[t_ssqA, t_junk])
                rstd_from_ssq(ssqA[:], D, rstdA[:], t_ssqA, t_rstdA, None)
                for j in range(4):
                    xb = j % 2
                    dve.op(lambda e: e.scalar_tensor_tensor(out=xnb[xb][:], in0=xts[j][:], scalar=rstdA[:, j:j + 1],
                                                            in1=gmix[:], op0=ALU.mult, op1=ALU.mult),
                           reads=[t_xt[j], t_rstdA, t_gmix], writes=[t_xnb[xb]])
                    pt = itr % 2
                    itr += 1
                    for kc in range(8):
                        pe.op(lambda e: e.transpose(out=ps_tr[pt][:, kc, :], in_=xnb[xb][:, kc * 128:(kc + 1) * 128],
                                                    identity=ident[:]),
                              reads=[t_xnb[xb], t_id], writes=[t_ps_tr[pt]])
                    act.op(lambda e: e.activation(out=xnT[b][:, :, j * 128:(j + 1) * 128], in_=ps_tr[pt][:], func=AF.Copy),
                           reads=[t_ps_tr[pt]], writes=[t_xnT[b]])
                if stop_after == "A1":
                    K.barrier()
                    return nc
                for j in range(4):
                    t = 4 * g + j
                    for cg in range(3):
                        for kc in range(8):
                            pe.op(lambda e: e.matmul(ps_qkv[cg][:], lhsT=xnT[b][:, kc, j * 128:(j + 1) * 128],
                                                     rhs=w_in_sb[:, kc, cg * 512:(cg + 1) * 512],
                                                     start=(kc == 0), stop=(kc == 7)),
                                  reads=[t_xnT[b], t_win], writes=[t_ps_qkv[cg]])
                    psq3 = ps_qkv[0][:].rearrange("p (g d) -> p g d", d=64)
                    psk3 = ps_qkv[1][:].rearrange("p (g d) -> p g d", d=64)
                    act.op(lambda e: e.activation(out=q_sb[:, :, 16:64], in_=psq3[:, :, 16:64], func=AF.Copy, scale=0.125),
                           reads=[t_ps_qkv[0]], writes=[t_qsb])
                    if stop_after == "A2a":
                        K.barrier()
                        return nc
                    rope(psq3, q_sb, cosq, sinq, t, t_ps_qkv[0], t_qsb, rtmp)
                    if stop_after == "A2b":
                        K.barrier()
                        return nc
                    act.op(lambda e: e.activation(out=k_sb[:, :, 16:64], in_=psk3[:, :, 16:64], func=AF.Copy),
                           reads=[t_ps_qkv[1]], writes=[t_ksb])
                    rope(psk3, k_sb, cosk, sink, t, t_ps_qkv[1], t_ksb, rtmp)
                    act.op(lambda e: e.activation(out=Vs[:, t, :, 0:128],
                                                  in_=ps_qkv[2][:].rearrange("p (h d) -> p h d", d=128), func=AF.Copy),
                           reads=[t_ps_qkv[2]], writes=[t_V])
                    if stop_after == "A2c":
                        K.barrier()
                        return nc
                    qf = q_sb[:].rearrange("p g d -> p (g d)")
                    kf = k_sb[:].rearrange("p g d -> p (g d)")
                    for h in range(4):
                        pe.op(lambda e: e.transpose(out=ps_qk[:, h, :], in_=qf[:, h * 128:(h + 1) * 128], identity=ident[:]),
                              reads=[t_qsb, t_id], writes=[t_ps_qk])
                    for h in range(4):
                        pe.op(lambda e: e.transpose(out=ps_qk[:, 4 + h, :], in_=kf[:, h * 128:(h + 1) * 128], identity=ident[:]),
                              reads=[t_ksb, t_id], writes=[t_ps_qk])
                    if stop_after == "A2d":
                        K.barrier()
                        return nc
                    act.op(lambda e: e.activation(out=QT[:, :, t * 128:(t + 1) * 128], in_=ps_qk[:, 0:4, :], func=AF.Copy),
                           reads=[t_ps_qk], writes=[t_QT])
                    act.op(lambda e: e.activation(out=KT[:, :, t * 128:(t + 1) * 128], in_=ps_qk[:, 4:8, :], func=AF.Copy),
                           reads=[t_ps_qk], writes=[t_KT])
                if stop_after == "A2":
                    K.barrier()
                    return nc
                for cc in range(8):
                    pl = il % 2
                    sg = il % 3
                    il += 1
                    for kc in range(8):
                        pe.op(lambda e: e.matmul(ps_l[pl][:], lhsT=w_in_sb[:, kc, 1536 + cc * 128:1536 + (cc + 1) * 128],
                                                 rhs=xnT[b][:, kc, :], start=(kc == 0), stop=(kc == 7)),
                              reads=[t_xnT[b], t_win_l], writes=[t_ps_l[pl]])
                    act.op(lambda e: e.activation(out=stage[sg][:], in_=ps_l[pl][:], func=AF.Copy),
                           reads=[t_ps_l[pl]], writes=[t_stage[sg]])
                    sp.dma([(lru_scr[cc * 128:(cc + 1) * 128, g * 512:(g + 1) * 512], stage[sg][:])], reads=[t_stage[sg]])
        K.barrier()
        K.release(mA2)
        if stop_after == "A":
            return nc

        gd = K.sb([128, 128], F32, "gdiff")
        t_gd = Tok()
        bcast_load(gd[:], gdiff_d[0], t_gd)
        dve.op(lambda e: e.tensor_scalar(out=gd[:], in0=gd[:], scalar1=0.8, scalar2=None, op0=ALU.mult),
               reads=[t_gd], writes=[t_gd])
        Qz = [[K.sb([128, 512], BF16) for _ in range(2)] for _ in range(2)]
        t_Qz = [[Tok(), Tok()], [Tok(), Tok()]]
        for sl_ in range(2):
            pool.op(lambda e: e.memset(Qz[0][sl_][:], 0.0), writes=[t_Qz[0][sl_]])
            pool.op(lambda e: e.memset(Qz[1][sl_][:], 0.0), writes=[t_Qz[1][sl_]])
        iq = 0
        pT = [[K.sb([128, 512], BF16) for _ in range(2)] for _ in range(2)]
        t_pT = [[Tok(), Tok()], [Tok(), Tok()]]
        o_sb = [K.sb([128, 128], F32) for _ in range(4)]
        t_osb = [Tok() for _ in range(4)]
        otmp = K.sb([128, 128], F32)
        rr = K.sb([128, 8], F32)
        ssB = K.sb([128, 4], F32)
        rsB = K.sb([128, 4], F32)
        t_rr, t_ssB, t_rsB, t_otmp = Tok(), Tok(), Tok(), Tok()
        on_bf = K.sb([128, 4, 128], BF16)
        t_on = Tok()
        cstg = [K.sb([128, 4, D], BF16) for _ in range(2)]
        t_cstg = [Tok(), Tok()]
        cstf = [K.sb([128, 4, D], F32) for _ in range(2)]
        t_cstf = [Tok(), Tok()]
        t_tab = Tok()
        conv_state = {"i": 0}
        NCONV = 2 * (NEXP // 512)

        def conv_parts(i):
            src_t, hcol = (pu_d, 0) if i < NCONV // 2 else (pv_d, 1)
            ci = i % (NCONV // 2)
            rows = slice(ci * 512, (ci + 1) * 512)
            return src_t, hcol, rows

        def conv_load(i):
            src_t, hcol, rows = conv_parts(i)
            sp.dma([(cstf[i % 2][:], src_t[rows, :].rearrange("(p r) d -> p r d", r=4))], writes=[t_cstf[i % 2]])

        def emit_conv(n):
            for _ in range(n):
                i = conv_state["i"]
                if i >= NCONV:
                    return
                conv_state["i"] = i + 1
                if i == 0:
                    conv_load(0)
                if i + 1 < NCONV:
                    conv_load(i + 1)
                src_t, hcol, rows = conv_parts(i)
                sg_ = i % 2
                dve.op(lambda e: e.tensor_copy(out=cstg[sg_][:], in_=cstf[sg_][:]), reads=[t_cstf[sg_]], writes=[t_cstg[sg_]])
                pool.dma([(puv_bf[rows, hcol * D:(hcol + 1) * D].rearrange("(p r) d -> p r d", r=4), cstg[sg_][:])],
                         reads=[t_cstg[sg_]], writes=[t_tab])

        n_iter_B = 4 * NG * NT
        conv_per_iter = -(-NCONV // n_iter_B)
        conv_stride = max(1, n_iter_B // NCONV)
        it_B = {"n": 0}
        with contextlib.ExitStack() as pes:
            K.pes = pes
            ps_s = [[K.ps([128, 512], F32) for _ in range(2)] for _ in range(2)]
            t_ps_s = [[Tok(), Tok()], [Tok(), Tok()]]
            ps_acc = K.ps([128, 3, 512], F32)
            t_acc = Tok()
            ps_oT = K.ps([128, 4, 128], BF16)
            t_oT = Tok()

            def acc(m, j):
                a = m * 4 + j
                return ps_acc[:, a // 3, (a % 3) * 129:(a % 3) * 129 + 129]

            for h in range(4):
                for qg in range(NG):
                    qs = slice(qg * 512, (qg + 1) * 512)
                    zq = iq % 2
                    iq += 1
                    pool.op(lambda e: e.tensor_copy(out=Qz[0][zq][0:64, :], in_=QT[0:64, h, qs]), reads=[t_QT], writes=[t_Qz[0][zq]])
                    pool.op(lambda e: e.tensor_copy(out=Qz[1][zq][64:128, :], in_=QT[64:128, h, qs]), reads=[t_QT], writes=[t_Qz[1][zq]])

                    def emit_s(kt):
                        sl = kt % 2
                        for m in range(2):
                            pe.op(lambda e: e.matmul(ps_s[m][sl][:], lhsT=KT[:, h, kt * 128:(kt + 1) * 128],
                                                     rhs=Qz[m][zq][:], start=True, stop=True),
                                  reads=[t_KT, t_Qz[m][zq]], writes=[t_ps_s[m][sl]])

                    emit_s(0)
                    for kt in range(NT):
                        sl = kt % 2
                        if it_B["n"] % conv_stride == 0:
                            emit_conv(conv_per_iter)
                        it_B["n"] += 1
                        if kt + 1 < NT:
                            emit_s(kt + 1)
                        for m in range(2):
                            act.op(lambda e: e.activation(out=pT[m][sl][:], in_=ps_s[m][sl][:], func=AF.Exp),
                                   reads=[t_ps_s[m][sl]], writes=[t_pT[m][sl]])
                        for m in range(2):
                            for j in range(4):
                                a = m * 4 + j
                                pe.op(lambda e: e.matmul(acc(m, j), lhsT=pT[m][sl][:, j * 128:(j + 1) * 128],
                                                         rhs=Vs[:, kt, h, :], start=(kt == 0 and a % 3 == 0),
                                                         stop=(kt == NT - 1)),
                                      reads=[t_pT[m][sl], t_V], writes=[t_acc])
                    for j in range(4):
                        dve.op(lambda e: e.reciprocal(out=rr[:, j:j + 1], in_=acc(0, j)[:, 128:129]),
                               reads=[t_acc], writes=[t_rr])
                        dve.op(lambda e: e.reciprocal(out=rr[:, 4 + j:5 + j], in_=acc(1, j)[:, 128:129]),
                               reads=[t_acc], writes=[t_rr])
                    dve.op(lambda e: e.tensor_tensor(out=rr[:, 4:8], in0=rr[:, 4:8], in1=neglam[:, 0:1].broadcast_to([128, 4]),
                                                     op=ALU.mult), reads=[t_rr, t_nl], writes=[t_rr])
                    for j in range(4):
                        dve.op(lambda e: e.tensor_scalar(out=otmp[:], in0=acc(1, j)[:, 0:128], scalar1=rr[:, 4 + j:5 + j],
                                                         scalar2=None, op0=ALU.mult),
                               reads=[t_acc, t_rr], writes=[t_otmp])
                        dve.op(lambda e: e.scalar_tensor_tensor(out=o_sb[j][:], in0=acc(0, j)[:, 0:128], scalar=rr[:, j:j + 1],
                                                                in1=otmp[:], op0=ALU.mult, op1=ALU.add),
                               reads=[t_acc, t_rr, t_otmp], writes=[t_osb[j]])
                        dve.op(lambda e: e.scalar_tensor_tensor(out=junk[:, 0:128], in0=o_sb[j][:], scalar=1.0, in1=o_sb[j][:],
                                                                op0=ALU.mult, op1=ALU.mult, accum_out=ssB[:, j:j + 1]),
                               reads=[t_osb[j]], writes=[t_ssB, t_junk])
                    rstd_from_ssq(ssB[:], 128, rsB[:], t_ssB, t_rsB, None)
                    for j in range(4):
                        dve.op(lambda e: e.scalar_tensor_tensor(out=on_bf[:, j, :], in0=o_sb[j][:], scalar=rsB[:, j:j + 1],
                                                                in1=gd[:], op0=ALU.mult, op1=ALU.mult),
                               reads=[t_osb[j], t_rsB, t_gd], writes=[t_on])
                    for j in range(4):
                        pe.op(lambda e: e.transpose(out=ps_oT[:, j, :], in_=on_bf[:, j, :], identity=ident[:]),
                              reads=[t_on, t_id], writes=[t_oT])
                    act.op(lambda e: e.activation(out=attnT[:, h, qs].rearrange("p (j q) -> p j q", q=128), in_=ps_oT[:],
                                                  func=AF.Copy), reads=[t_oT], writes=[t_attnT])
        emit_conv(NCONV)
        K.barrier()
        K.release(mA)
        if stop_after == "B":
            return nc

        mC = K.mark()
        w_out_sb = K.sb([128, 8, D], BF16, "w_out")
        t_wout = Tok()
        cast_load(w_out_sb, w_out_d, 8, D, t_wout)
        mC2 = K.mark()
        ubuf = K.sb([128, S], F32, "ubuf")
        gbuf = K.sb([128, S], F32, "gbuf")
        ucf = K.sb([128, S], F32, "ucf")
        ucb = K.sb([128, S], BF16, "ucb")
        ysq = K.sb([128, S], BF16, "ysq")
        t_u, t_g, t_uc, t_ucb, t_ysq, t_hs = Tok(), Tok(), Tok(), Tok(), Tok(), Tok()
        NB = 2
        r_b = [K.sb([128, L], F32) for _ in range(NB)]
        i_b = [K.sb([128, L], F32) for _ in range(NB)]
        a_b = [K.sb([128, L], F32) for _ in range(NB)]
        a2_b = [K.sb([128, L], F32) for _ in range(NB)]
        h_b = [K.sb([128, L], F32) for _ in range(NB)]
        t_r = [Tok() for _ in range(NB)]
        t_i = [Tok() for _ in range(NB)]
        t_a = [Tok() for _ in range(NB)]
        t_a2 = [Tok() for _ in range(NB)]
        t_h = [Tok() for _ in range(NB)]
        carry = K.sb([128, 2], F32, "carry")
        t_carry = Tok()
        with contextlib.ExitStack() as pes:
            K.pes = pes
            ps_ga = [K.ps([128, 512], F32) for _ in range(2)]
            ps_gx = [K.ps([128, 512], F32) for _ in range(2)]
            t_ga = [Tok(), Tok()]
            t_gx = [Tok(), Tok()]
            ps_ssq = K.ps([128, NT], F32)
            t_pssq = Tok()
            ig = 0
            ib = 0
            for c in range(4):
                cp = lambda k: colp[:, c * 12 + k:c * 12 + k + 1]
                sp.dma([(ubuf[:], lru_scr[c * 128:(c + 1) * 128, :])], writes=[t_u, t_hs])
                sp.dma([(gbuf[:], lru_scr[(4 + c) * 128:(5 + c) * 128, :])], writes=[t_g])
                dve.op(lambda e: e.tensor_scalar(out=ucf[:], in0=ubuf[:], scalar1=cp(2), scalar2=cp(4), op0=ALU.mult, op1=ALU.add),
                       reads=[t_u, t_colp], writes=[t_uc])
                dve.op(lambda e: e.scalar_tensor_tensor(out=ucf[:, 2:S], in0=ubuf[:, 0:S - 2], scalar=cp(0), in1=ucf[:, 2:S],
                                                        op0=ALU.mult, op1=ALU.add), reads=[t_u, t_uc, t_colp], writes=[t_uc])
                dve.op(lambda e: e.scalar_tensor_tensor(out=ucf[:, 1:S], in0=ubuf[:, 0:S - 1], scalar=cp(1), in1=ucf[:, 1:S],
                                                        op0=ALU.mult, op1=ALU.add), reads=[t_u, t_uc, t_colp], writes=[t_uc])
                dve.op(lambda e: e.scalar_tensor_tensor(out=ucf[:, 0:S - 1], in0=ubuf[:, 1:S], scalar=cp(3), in1=ucf[:, 0:S - 1],
                                                        op0=ALU.mult, op1=ALU.add), reads=[t_u, t_uc, t_colp], writes=[t_uc])
                act.op(lambda e: e.activation(out=ucb[:], in_=ucf[:], func=AF.Copy), reads=[t_uc], writes=[t_ucb])
                act.op(lambda e: e.activation(out=gbuf[:], in_=gbuf[:], func=AF.Gelu), reads=[t_g], writes=[t_g])
                hs = ubuf
                for d in range(2):
                    segs = list(range(NSEG)) if d == 0 else list(range(NSEG - 1, -1, -1))
                    wia = (0 * 2 + d) * 4 + c
                    wix = (1 * 2 + d) * 4 + c
                    for si, s in enumerate(segs):
                        bi = ib % NB
                        ib += 1
                        for hf in range(L // 512):
                            cs = slice(s * L + hf * 512, s * L + (hf + 1) * 512)
                            ls = slice(hf * 512, (hf + 1) * 512)
                            pg = ig % 2
                            ig += 1
                            pe.op(lambda e: e.matmul(ps_ga[pg][:], lhsT=Wbd[:, wia, :], rhs=ucb[:, cs], start=True, stop=True),
                                  reads=[t_Wbd, t_ucb], writes=[t_ga[pg]])
                            pe.op(lambda e: e.matmul(ps_gx[pg][:], lhsT=Wbd[:, wix, :], rhs=ucb[:, cs], start=True, stop=True),
                                  reads=[t_Wbd, t_ucb], writes=[t_gx[pg]])
                            act.op(lambda e: e.activation(out=r_b[bi][:, ls], in_=ps_ga[pg][:], func=AF.Sigmoid, bias=cp(5 + d), scale=1.0),
                                   reads=[t_ga[pg], t_colp], writes=[t_r[bi]])
                            act.op(lambda e: e.activation(out=i_b[bi][:, ls], in_=ps_gx[pg][:], func=AF.Sigmoid, bias=cp(7 + d), scale=1.0),
                                   reads=[t_gx[pg], t_colp], writes=[t_i[bi]])
                        act.op(lambda e: e.activation(out=a_b[bi][:], in_=r_b[bi][:], func=AF.Exp, scale=scl[:, c, d:d + 1]),
                               reads=[t_r[bi], t_scl], writes=[t_a[bi]])
                        act.op(lambda e: e.activation(out=a2_b[bi][:], in_=r_b[bi][:], func=AF.Exp, scale=scl2[:, c, d:d + 1]),
                               reads=[t_r[bi], t_scl], writes=[t_a2[bi]])
                        act.op(lambda e: e.activation(out=a2_b[bi][:], in_=a2_b[bi][:], func=AF.Sqrt, bias=one_c[:, 0:1], scale=-1.0),
                               reads=[t_a2[bi], t_one], writes=[t_a2[bi]])
                        dve.op(lambda e: e.tensor_tensor(out=i_b[bi][:], in0=i_b[bi][:], in1=ucf[:, s * L:(s + 1) * L], op=ALU.mult),
                               reads=[t_i[bi], t_uc], writes=[t_i[bi]])
                        dve.op(lambda e: e.tensor_tensor(out=i_b[bi][:], in0=i_b[bi][:], in1=a2_b[bi][:], op=ALU.mult),
                               reads=[t_i[bi], t_a2[bi]], writes=[t_i[bi]])
                        init = 0.0 if si == 0 else carry[:, d:d + 1]
                        if d == 0:
                            dve.op(lambda e: e.tensor_tensor_scan(out=hs[:, s * L:(s + 1) * L], data0=a_b[bi][:], data1=i_b[bi][:],
                                                                  initial=init, op0=ALU.mult, op1=ALU.add),
                                   reads=[t_a[bi], t_i[bi], t_carry, t_uc], writes=[t_hs])
                            dve.op(lambda e: e.tensor_copy(out=carry[:, 0:1], in_=hs[:, (s + 1) * L - 1:(s + 1) * L]),
                                   reads=[t_hs], writes=[t_carry])
                        else:
                            dve.op(lambda e: e.tensor_tensor_scan(out=h_b[bi][:, ::-1], data0=a_b[bi][:, ::-1], data1=i_b[bi][:, ::-1],
                                                                  initial=init, op0=ALU.mult, op1=ALU.add),
                                   reads=[t_a[bi], t_i[bi], t_carry], writes=[t_h[bi]])
                            dve.op(lambda e: e.tensor_copy(out=carry[:, 1:2], in_=h_b[bi][:, 0:1]),
                                   reads=[t_h[bi]], writes=[t_carry])
                            pool.op(lambda e: e.tensor_tensor(out=hs[:, s * L:(s + 1) * L], in0=hs[:, s * L:(s + 1) * L],
                                                              in1=h_b[bi][:], op=ALU.add),
                                    reads=[t_hs, t_h[bi]], writes=[t_hs])
                dve.op(lambda e: e.tensor_tensor(out=hs[:], in0=hs[:], in1=gbuf[:], op=ALU.mult), reads=[t_hs, t_g], writes=[t_hs])
                act.op(lambda e: e.activation(out=yT[:, c, :], in_=hs[:], func=AF.Copy, scale=cp(11)),
                       reads=[t_hs, t_colp], writes=[t_yT])
                act.op(lambda e: e.activation(out=ysq[:], in_=hs[:], func=AF.Square), reads=[t_hs], writes=[t_ysq])
                for t in range(NT):
                    pe.op(lambda e: e.matmul(ps_ssq[:, t:t + 1], lhsT=ysq[:, t * 128:(t + 1) * 128], rhs=ones_bf[:, 0:1],
                                             start=(c == 0 and t == 0), stop=(c == 3)),
                          reads=[t_ysq, t_onesbf], writes=[t_pssq])
            rstd_from_ssq(ps_ssq[:], 512, rstd_rec[:], t_pssq, t_rstd_rec, None)
        K.barrier()
        K.release(mC2)
        if stop_after == "C":
            return nc
        hole_off = mC
        wq_sb = K.sb([128, 8, D], BF16, "wq")
        wg_sb = K.sb([128, 8, D], BF16, "wg")
        wp_sb = K.sb([128, 2, D], BF16, "wp")
        t_wq, t_wg, t_wp = Tok(), Tok(), Tok()
        cast_load(wq_sb, wq_d, 8, D, t_wq)
        cast_load(wg_sb, wg_d, 8, D, t_wg)
        cast_load(wp_sb, wp_d, 2, D, t_wp)
        gffn = K.sb([128, D], F32)
        gple = K.sb([128, D], F32)
        gfin = K.sb([128, D], F32)
        t_gffn, t_gple, t_gfin = Tok(), Tok(), Tok()
        bcast_load(gffn[:], gffn_d[0], t_gffn)
        bcast_load(gple[:], gple_d[0], t_gple)
        bcast_load(gfin[:], gfin_d[0], t_gfin)

        mD = K.mark()

        xtd = [K.sb([128, D], F32) for _ in range(2)]
        t_xtd = [Tok(), Tok()]
        h1o = [K.sb([128, D], F32) for _ in range(2)]
        t_h1o = [Tok(), Tok()]
        with contextlib.ExitStack() as pes:
            K.pes = pes
            ps_at = [[K.ps([128, 512], F32) for _ in range(2)] for _ in range(2)]
            ps_rc = [[K.ps([128, 512], F32) for _ in range(2)] for _ in range(2)]
            t_at = [[Tok(), Tok()], [Tok(), Tok()]]
            t_rc = [[Tok(), Tok()], [Tok(), Tok()]]
            for t in range(NT):
                b = t % 2
                ts_ = slice(t * 128, (t + 1) * 128)
                sp.dma([(xtd[b][:], x_d[ts_, :])], writes=[t_xtd[b]])
                for n in range(2):
                    ns = slice(n * 512, (n + 1) * 512)
                    for kc in range(4):
                        pe.op(lambda e: e.matmul(ps_at[b][n][:], lhsT=attnT[:, kc, ts_], rhs=w_out_sb[:, kc, ns],
                                                 start=(kc == 0), stop=(kc == 3)),
                              reads=[t_attnT, t_wout], writes=[t_at[b][n]])
                    for kc in range(4):
                        pe.op(lambda e: e.matmul(ps_rc[b][n][:], lhsT=yT[:, kc, ts_], rhs=w_out_sb[:, 4 + kc, ns],
                                                 start=(kc == 0), stop=(kc == 3)),
                              reads=[t_yT, t_wout], writes=[t_rc[b][n]])
                    dve.op(lambda e: e.scalar_tensor_tensor(out=h1o[b][:, ns], in0=ps_rc[b][n][:], scalar=rstd_rec[:, t:t + 1],
                                                            in1=xtd[b][:, ns], op0=ALU.mult, op1=ALU.add),
                           reads=[t_rc[b][n], t_rstd_rec, t_xtd[b]], writes=[t_h1o[b]])
                    dve.op(lambda e: e.tensor_tensor(out=h1o[b][:, ns], in0=ps_at[b][n][:], in1=h1o[b][:, ns], op=ALU.add),
                           reads=[t_at[b][n], t_h1o[b]], writes=[t_h1o[b]])
                sp.dma([(h1_scr[ts_, :], h1o[b][:])], reads=[t_h1o[b]])
        K.barrier()
        K.release(mD)
        if stop_after == "D":
            return nc

        h1t = [K.sb([128, D], F32) for _ in range(3)]
        ptl = [K.sb([128, 256], F32) for _ in range(3)]
        t_h1t = [Tok() for _ in range(3)]
        t_ptl = [Tok() for _ in range(3)]
        xn2b = [K.sb([128, D], BF16) for _ in range(2)]
        junkb = K.sb([128, D], BF16)
        t_junkb = Tok()
        junk_sq = K.sb([128, D], BF16)
        t_junk_sq = Tok()
        xTF = K.sb([128, 8, 128], BF16)
        xTT = K.sb([128, 8, 128], BF16)
        qTs = K.sb([128, 8, 128], BF16)
        s_sb = K.sb([128, 8, 256], F32)
        t_xn2b = [Tok(), Tok()]
        t_xTF, t_xTT, t_qTs = Tok(), Tok(), Tok()
        t_u = [[Tok(), Tok()] for _ in range(8)]
        t_c = [Tok() for _ in range(8)]
        ssF = K.sb([128, 1], F32)
        rsF = K.sb([128, 1], F32)
        ssT = K.sb([128, 1], F32)
        rsT = K.sb([128, 1], F32)
        t_ssF, t_rsF, t_ssT, t_rsT = Tok(), Tok(), Tok(), Tok()
        v12 = [K.sb([128, 8, 16], F32) for _ in range(2)]
        i12 = [K.sb([128, 8, 16], U32) for _ in range(2)]
        i12f = [K.sb([128, 8, 16], BF16) for _ in range(2)]
        t_tk = Tok()
        cand = K.sb([128, 8, 16, 16], F32)
        scv = K.sb([128, 8, 16], F32)
        posu = K.sb([128, 8, 16], U32)
        pa_i = K.sb([128, 128], I32)
        pa_f = K.sb([128, 2, 128], F32)
        oh = K.sb([128, 128, 16], BF16)
        isel = K.sb([128, 2, 128], BF16)
        idxf = K.sb([128, 128], F32)
        idxi = [K.sb([128, 128], I32) for _ in range(2)]
        t_idx = [Tok(), Tok()]
        gw = [K.sb([128, 8, 16], F32) for _ in range(2)]
        t_gw = [Tok(), Tok()]
        zs = K.sb([128, 8], F32)
        logits = K.sb([128, 128], F32)
        actv = K.sb([128, 128], F32)
        gl4 = K.sb([128, 128], F32)
        NBUF = 10
        GS = 4
        gbufs = [K.sb([128, 2 * D], BF16, at=hole_off + k_ * 4096) for k_ in range(4)] + [K.sb([128, 2 * D], BF16) for _ in range(NBUF - 4)]
        t_gb = [Tok() for _ in range(NBUF)]
        NDG = 4
        diag = [K.sb([128, 128], BF16) for _ in range(NDG)]
        t_diag = [Tok() for _ in range(NDG)]
        t_lg = [Tok() for _ in range(128)]
        t_av = [Tok() for _ in range(128)]
        h2 = [K.sb([128, D], F32) for _ in range(2)]
        t_h2 = [Tok(), Tok()]
        xn3b = K.sb([128, D], BF16)
        gate_sb = K.sb([128, D], F32)
        ptb = K.sb([128, 256], BF16)
        pTs = K.sb([128, 2, 128], BF16)
        h3 = K.sb([128, D], F32)
        outt = [K.sb([128, D], F32) for _ in range(2)]
        t_xn3b, t_gate, t_ptb, t_pTs, t_h3 = Tok(), Tok(), Tok(), Tok(), Tok()
        t_outt = [Tok(), Tok()]

        def gap(n):
            for _ in range(n):
                yield

        def rmsnorm(src, t_src, gain, t_gain, dst, t_dst, ss, rs, t_ss, t_rs):
            act.op(lambda e: e.activation(out=junk_sq[:], in_=src[:], func=AF.Square, accum_out=ss[:, 0:1]),
                   reads=[t_src], writes=[t_ss, t_junk_sq])
            yield from gap(3)
            dve.op(lambda e: e.tensor_scalar(out=rs[:], in0=ss[:], scalar1=1.0 / D, scalar2=EPS, op0=ALU.mult, op1=ALU.add),
                   reads=[t_ss], writes=[t_rs])
            yield from gap(2)
            act.op(lambda e: e.activation(out=rs[:], in_=rs[:], func=AF.Sqrt), reads=[t_rs], writes=[t_rs])
            yield from gap(3)
            dve.op(lambda e: e.reciprocal(out=rs[:], in_=rs[:]), reads=[t_rs], writes=[t_rs])
            yield
            dve.op(lambda e: e.scalar_tensor_tensor(out=dst[:], in0=src[:], scalar=rs[:, 0:1], in1=gain[:],
                                                    op0=ALU.mult, op1=ALU.add if False else ALU.mult),
                   reads=[t_src, t_rs, t_gain], writes=[t_dst])
            yield

        with contextlib.ExitStack() as pes:
            K.pes = pes
            ps_tF = K.ps([128, 8, 128], BF16)
            ps_tT = K.ps([128, 8, 128], BF16)
            t_pstF, t_pstT = Tok(), Tok()
            ps_q = K.ps([128, 4, 128], F32)
            t_psq = Tok()
            ps_sc = K.ps([128, 2, 256], F32)
            t_psc = Tok()
            ps_g = [K.ps([128, 512], F32) for _ in range(2)]
            t_psg = [Tok(), Tok()]
            ps_v = [K.ps([128, 512], F32) for _ in range(2)]
            t_psv = [Tok(), Tok()]

            def front(t):
                b3 = t % 3
                b = t % 2
                ts_ = slice(t * 128, (t + 1) * 128)
                sp.dma([(h1t[b3][:], h1_scr[ts_, :])], writes=[t_h1t[b3]])
                sp.dma([(ptl[b3][:], p_d[ts_, :])], writes=[t_ptl[b3]])
                yield from gap(3)
                yield from rmsnorm(h1t[b3], t_h1t[b3], gffn, t_gffn, xn2b[b], t_xn2b[b], ssF, rsF, t_ssF, t_rsF)
                yield from gap(2)
                for kc in range(8):
                    pe.op(lambda e: e.transpose(out=ps_tF[:, kc, :], in_=xn2b[b][:, kc * 128:(kc + 1) * 128], identity=ident[:]),
                          reads=[t_xn2b[b], t_id], writes=[t_pstF])
                yield from gap(2)
                act.op(lambda e: e.activation(out=xTF[:], in_=ps_tF[:], func=AF.Copy), reads=[t_pstF], writes=[t_xTF])
                yield from gap(2)
                for hh in range(2):
                    for c4 in range(4):
                        cc = hh * 4 + c4
                        for kc in range(8):
                            pe.op(lambda e: e.matmul(ps_q[:, c4, :], lhsT=wq_sb[:, kc, cc * 128:(cc + 1) * 128],
                                                     rhs=xTF[:, kc, :], start=(kc == 0 and c4 == 0), stop=(kc == 7)),
                                  reads=[t_wq, t_xTF], writes=[t_psq])
                        if c4 % 2 == 1:
                            yield
                    yield from gap(2)
                    act.op(lambda e: e.activation(out=qTs[:, hh * 4:(hh + 1) * 4, :], in_=ps_q[:], func=AF.Copy),
                           reads=[t_psq], writes=[t_qTs])
                    yield from gap(2)
                for hp in range(4):
                    for k2 in range(2):
                        hd = hp * 2 + k2
                        pe.op(lambda e: e.matmul(ps_sc[:, k2, :], lhsT=qTs[:, hd, :], rhs=keysbd[:], start=True, stop=True),
                              reads=[t_qTs, t_keys], writes=[t_psc])
                    yield from gap(2)
                    act.op(lambda e: e.activation(out=s_sb[:, hp * 2:(hp + 1) * 2, :], in_=ps_sc[:], func=AF.Copy),
                           reads=[t_psc], writes=[t_u[hp * 2][0], t_u[hp * 2][1], t_u[hp * 2 + 1][0], t_u[hp * 2 + 1][1]])
                    yield
                mode["heavy"] = True
                units = [(hd, sd) for hd in range(8) for sd in range(2)]
                srcu = lambda hd, sd: s_sb[:, hd, sd * 128:(sd + 1) * 128]
                for rnd in range(2):
                    ks = slice(rnd * 8, rnd * 8 + 8)
                    for k_, (hd, sd) in enumerate(units):
                        dve.op(lambda e: e.max(out=v12[sd][:, hd, ks], in_=srcu(hd, sd)), reads=[t_u[hd][sd]], writes=[t_u[hd][sd]])
                        if k_ % 2 == 1:
                            yield
                    for k_, (hd, sd) in enumerate(units):
                        dve.op(lambda e: e.max_index(out=i12[sd][:, hd, ks], in_max=v12[sd][:, hd, ks], in_values=srcu(hd, sd)),
                               reads=[t_u[hd][sd]], writes=[t_u[hd][sd]])
                        if k_ % 2 == 1:
                            yield
                    if rnd == 0:
                        for k_, (hd, sd) in enumerate(units):
                            dve.op(lambda e: e.match_replace(out=srcu(hd, sd), in_to_replace=v12[sd][:, hd, ks], in_values=srcu(hd, sd),
                                                             imm_value=-1e30), reads=[t_u[hd][sd]], writes=[t_u[hd][sd]])
                            if k_ % 2 == 1:
                                yield
                for hd in range(8):
                    dve.op(lambda e: e.tensor_tensor(out=cand[:, hd], in0=v12[0][:, hd, :].unsqueeze(2).broadcast_to([128, 16, 16]),
                                                     in1=v12[1][:, hd, :].unsqueeze(1).broadcast_to([128, 16, 16]), op=ALU.add),
                           reads=[t_u[hd][0], t_u[hd][1]], writes=[t_c[hd]])
                    if hd % 2 == 1:
                        yield
                cfu = lambda hd: cand[:, hd].rearrange("p a b -> p (a b)")
                for rnd in range(2):
                    ks = slice(rnd * 8, rnd * 8 + 8)
                    for hd in range(8):
                        dve.op(lambda e: e.max(out=scv[:, hd, ks], in_=cfu(hd)), reads=[t_c[hd]], writes=[t_c[hd]])
                        if hd % 2 == 1:
                            yield
                    for hd in range(8):
                        dve.op(lambda e: e.max_index(out=posu[:, hd, ks], in_max=scv[:, hd, ks], in_values=cfu(hd)),
                               reads=[t_c[hd]], writes=[t_c[hd]])
                        if hd % 2 == 1:
                            yield
                    if rnd == 0:
                        for hd in range(8):
                            dve.op(lambda e: e.match_replace(out=cfu(hd), in_to_replace=scv[:, hd, ks], in_values=cfu(hd), imm_value=-1e30),
                                   reads=[t_c[hd]], writes=[t_c[hd]])
                            if hd % 2 == 1:
                                yield
                dve.op(lambda e: e.tensor_copy(out=zs[:, 0:1], in_=scv[:, 0, 0:1]), reads=t_c + [t_u[h_][s_] for h_ in range(8) for s_ in range(2)],
                       writes=[t_tk])
                posf_ = posu[:].rearrange("p h k -> p (h k)").bitcast(I32)
                for sd in range(2):
                    if sd == 0:
                        dve.op(lambda e: e.tensor_single_scalar(out=pa_i[:], in_=posf_, scalar=4, op=ALU.arith_shift_right),
                               reads=[t_tk], writes=[t_tk])
                    else:
                        dve.op(lambda e: e.tensor_single_scalar(out=pa_i[:], in_=posf_, scalar=15, op=ALU.bitwise_and),
                               reads=[t_tk], writes=[t_tk])
                    dve.op(lambda e: e.tensor_copy(out=pa_f[:, sd, :], in_=pa_i[:]), reads=[t_tk], writes=[t_tk])
                    dve.op(lambda e: e.tensor_copy(out=i12f[sd][:], in_=i12[sd][:].bitcast(I32)), reads=[t_tk], writes=[t_tk])
                    yield
                    dve.op(lambda e: e.tensor_tensor(out=oh[:], in0=pa_f[:, sd, :].unsqueeze(2).broadcast_to([128, 128, 16]),
                                                     in1=iota16.unsqueeze(1).broadcast_to([128, 128, 16]), op=ALU.is_equal),
                           reads=[t_tk, t_cst], writes=[t_tk])
                    yield
                    oh4 = oh[:].rearrange("p (h s) a -> p h s a", h=8)
                    dve.op(lambda e: e.tensor_tensor(out=oh4, in0=oh4, in1=i12f[sd][:].unsqueeze(2).broadcast_to([128, 8, 16, 16]), op=ALU.mult),
                           reads=[t_tk], writes=[t_tk])
                    yield
                    with nc.allow_low_precision("sum over a one-hot row of an integer id <= 127: exact in bf16"):
                        dve.op(lambda e: e.tensor_reduce(out=isel[:, sd, :], in_=oh[:], axis=AX.X, op=ALU.add), reads=[t_tk], writes=[t_tk])
                    yield
                dve.op(lambda e: e.scalar_tensor_tensor(out=idxf[:], in0=isel[:, 0, :], scalar=128.0, in1=isel[:, 1, :],
                                                        op0=ALU.mult, op1=ALU.add), reads=[t_tk], writes=[t_tk])
                dve.op(lambda e: e.tensor_copy(out=idxi[b][:], in_=idxf[:]), reads=[t_tk], writes=[t_idx[b]])
                mode["heavy"] = False
                yield
                dve.op(lambda e: e.tensor_tensor(out=gw[b][:], in0=scv[:], in1=scv[:, :, 0:1].broadcast_to([128, 8, 16]), op=ALU.subtract),
                       reads=[t_tk], writes=[t_gw[b]])
                act.op(lambda e: e.activation(out=gw[b][:], in_=gw[b][:], func=AF.Exp), reads=[t_gw[b]], writes=[t_gw[b]])
                yield
                dve.op(lambda e: e.tensor_reduce(out=zs[:], in_=gw[b][:], axis=AX.X, op=ALU.add), reads=[t_gw[b]], writes=[t_tk])
                dve.op(lambda e: e.reciprocal(out=zs[:], in_=zs[:]), reads=[t_tk], writes=[t_tk])
                dve.op(lambda e: e.tensor_tensor(out=gw[b][:], in0=gw[b][:], in1=zs[:].unsqueeze(2).broadcast_to([128, 8, 16]), op=ALU.mult),
                       reads=[t_gw[b], t_tk], writes=[t_gw[b]])
                yield

            def tail(t):
                b3 = t % 3
                b = t % 2
                ts_ = slice(t * 128, (t + 1) * 128)
                yield from rmsnorm(h2[b], t_h2[b], gple, t_gple, xn3b, t_xn3b, ssT, rsT, t_ssT, t_rsT)
                yield from gap(1)
                for kc in range(8):
                    pe.op(lambda e: e.transpose(out=ps_tT[:, kc, :], in_=xn3b[:, kc * 128:(kc + 1) * 128], identity=ident[:]),
                          reads=[t_xn3b, t_id], writes=[t_pstT])
                yield from gap(2)
                act.op(lambda e: e.activation(out=xTT[:], in_=ps_tT[:], func=AF.Copy), reads=[t_pstT], writes=[t_xTT])
                yield from gap(2)
                for n in range(2):
                    for kc in range(8):
                        pe.op(lambda e: e.matmul(ps_g[n][:], lhsT=xTT[:, kc, :], rhs=wg_sb[:, kc, n * 512:(n + 1) * 512],
                                                 start=(kc == 0), stop=(kc == 7)), reads=[t_xTT, t_wg], writes=[t_psg[n]])
                    yield
                yield from gap(2)
                for n in range(2):
                    act.op(lambda e: e.activation(out=gate_sb[:, n * 512:(n + 1) * 512], in_=ps_g[n][:], func=AF.Sigmoid),
                           reads=[t_psg[n]], writes=[t_gate])
                act.op(lambda e: e.activation(out=ptb[:], in_=ptl[b3][:], func=AF.Copy), reads=[t_ptl[b3]], writes=[t_ptb])
                yield from gap(2)
                for kc in range(2):
                    pe.op(lambda e: e.transpose(out=ps_tT[:, kc, :], in_=ptb[:, kc * 128:(kc + 1) * 128], identity=ident[:]),
                          reads=[t_ptb, t_id], writes=[t_pstT])
                yield from gap(2)
                act.op(lambda e: e.activation(out=pTs[:], in_=ps_tT[:, 0:2, :], func=AF.Copy), reads=[t_pstT], writes=[t_pTs])
                yield from gap(2)
                for n in range(2):
                    ns = slice(n * 512, (n + 1) * 512)
                    for kc in range(2):
                        pe.op(lambda e: e.matmul(ps_g[n][:], lhsT=pTs[:, kc, :], rhs=wp_sb[:, kc, ns],
                                                 start=(kc == 0), stop=(kc == 1)), reads=[t_pTs, t_wp], writes=[t_psg[n]])
                yield from gap(2)
                for n in range(2):
                    ns = slice(n * 512, (n + 1) * 512)
                    dve.op(lambda e: e.tensor_tensor(out=h3[:, ns], in0=ps_g[n][:], in1=gate_sb[:, ns], op=ALU.mult),
                           reads=[t_psg[n], t_gate], writes=[t_h3])
                    yield
                dve.op(lambda e: e.tensor_tensor(out=h3[:], in0=h3[:], in1=h2[b][:], op=ALU.add), reads=[t_h3, t_h2[b]], writes=[t_h3])
                yield
                yield from rmsnorm(h3, t_h3, gfin, t_gfin, outt[b], t_outt[b], ssT, rsT, t_ssT, t_rsT)
                sp.dma([(out_d[ts_, :], outt[b][:])], reads=[t_outt[b]])
                yield

            gi_state = {"gi": 0}

            TAIL_START = 0

            class _Late:
                _late = True

                def __init__(self, g):
                    self.g = g

                def __iter__(self):
                    return self

                def __next__(self):
                    return next(self.g)

            mode = {"heavy": False}

            def slots(t, gens):
                b = t % 2
                b3 = t % 3
                gwf = gw[b][:].rearrange("p h k -> p (h k)")
                lightq = []

                for sl in range(128):
                    gb = gi_state["gi"] % NBUF
                    gi_state["gi"] += 1
                    dg = sl % NDG
                    pool.dma([(gbufs[gb][:], puv_bf)], reads=[t_idx[b], t_tab], writes=[t_gb[gb]],
                             fn=lambda e, o, i: e.indirect_dma_start(out=o, out_offset=None, in_=i,
                                                                      in_offset=bass.IndirectOffsetOnAxis(ap=idxi[b][:, sl:sl + 1], axis=0)))
                    dve.op(lambda e: e.scalar_tensor_tensor(out=junkb[:], in0=gbufs[gb][:, 0:D], scalar=1.0, in1=xn2b[b][:], op0=ALU.mult, op1=ALU.mult,
                                                            accum_out=logits[:, sl:sl + 1]),
                           reads=[t_gb[gb], t_xn2b[b]], writes=[t_lg[sl], t_junkb])
                    act.op(lambda e: e.activation(out=gl4[:, sl:sl + 1], in_=logits[:, sl:sl + 1], func=AF.Gelu),
                           reads=[t_lg[sl]], writes=[t_av[sl]])

                    def finish_act(s_, gb_):
                        dg_ = s_ % NDG
                        act.op(lambda e: e.activation(out=actv[:, s_:s_ + 1], in_=gl4[:, s_:s_ + 1], func=AF.Copy, scale=gwf[:, s_:s_ + 1]),
                               reads=[t_av[s_], t_gw[b]], writes=[t_av[s_]])
                        act.op(lambda e: e.activation(out=diag[dg_][:], in_=ident[:], func=AF.Copy, scale=actv[:, s_:s_ + 1]),
                               reads=[t_id, t_av[s_]], writes=[t_diag[dg_]])
                        mm(s_, gb_, dg_)

                    def finish_dve(s_, gb_):
                        dg_ = s_ % NDG
                        dve.op(lambda e: e.tensor_scalar(out=diag[dg_][:], in0=ident[:], scalar1=gl4[:, s_:s_ + 1], scalar2=gwf[:, s_:s_ + 1],
                                                         op0=ALU.mult, op1=ALU.mult),
                               reads=[t_id, t_av[s_], t_gw[b]], writes=[t_diag[dg_]])
                        mm(s_, gb_, dg_)

                    def mm(s_, gb_, dg_):
                        for n in range(2):
                            pe.op(lambda e: e.matmul(ps_v[n][:], lhsT=diag[dg_][:], rhs=gbufs[gb_][:, D + n * 512:D + (n + 1) * 512],
                                                     start=(s_ == 0), stop=False),
                                  reads=[t_diag[dg_], t_gb[gb_]], writes=[t_psv[n]])

                    if mode["heavy"]:
                        for p_ in lightq:
                            finish_dve(*p_)
                        del lightq[:]
                        finish_act(sl, gb)
                    else:
                        for p_ in lightq:
                            finish_dve(*p_)
                        del lightq[:]
                        lightq.append((sl, gb))
                    for gen in gens:
                        if getattr(gen, "_late", False) and sl < TAIL_START:
                            continue
                        next(gen, None)
                for p_ in lightq:
                    finish_dve(*p_)
                del lightq[:]
                for gen in gens:
                    for _ in gen:
                        pass
                for n in range(2):
                    ns = slice(n * 512, (n + 1) * 512)
                    dve.op(lambda e: e.tensor_tensor(out=h2[b][:, ns], in0=ps_v[n][:], in1=h1t[b3][:, ns], op=ALU.add),
                           reads=[t_psv[n], t_h1t[b3]], writes=[t_h2[b]])

            for _ in front(0):
                pass
            for t in range(NT):
                gens = []
                if t + 1 < NT:
                    gens.append(front(t + 1))
                if t >= 1:
                    tg = _Late(tail(t - 1))
                    gens.append(tg)
                slots(t, gens)
            for _ in tail(NT - 1):
                pass
        K.barrier()
        print("SBUF peak bytes/partition", K.sb_peak - SB_LO, "of", SB_HI - SB_LO)
    return nc


def _consts():
    c = np.zeros((128, 152), np.float32)
    c[:, 0:128] = np.eye(128, dtype=np.float32)
    c[:, 128:144] = np.arange(16, dtype=np.float32)[None, :]
    inv = (np.float32(500000.0) ** (-np.arange(0, 16, 2, dtype=np.float32) / np.float32(16))).astype(np.float32)
    c[:, 144:152] = inv[None, :]
    return c


def make_in_maps(inputs, S):
    f = lambda a: np.ascontiguousarray(np.asarray(a))
    x = f(inputs["x"])
    B = x.shape[0]
    NT = S // 128
    conv_w = f(inputs["conv_w"])[0]
    cols = []
    for c in range(4):
        sl = slice(c * 128, (c + 1) * 128)
        ks = [conv_w[0, sl], conv_w[1, sl], conv_w[2, sl], conv_w[3, sl], f(inputs["conv_b"])[0, sl],
              f(inputs["lru_ba"])[0, 0, sl], f(inputs["lru_ba"])[0, 1, sl],
              f(inputs["lru_bx"])[0, 0, sl], f(inputs["lru_bx"])[0, 1, sl],
              f(inputs["lru_lambda"])[0, 0, sl], f(inputs["lru_lambda"])[0, 1, sl],
              f(inputs["lru_norm_g"])[0, sl]]
        cols.extend(ks)
    colp = np.ascontiguousarray(np.stack(cols, axis=1).astype(np.float32))
    lam4 = np.ascontiguousarray(np.concatenate([f(inputs["lambda_q1"])[0], f(inputs["lambda_k1"])[0],
                                                f(inputs["lambda_q2"])[0], f(inputs["lambda_k2"])[0]])[None, :])
    shared = {
        "consts": _consts(),
        "w_in": f(inputs["w_in"])[0], "w_out": f(inputs["w_out"])[0], "peer_wq": f(inputs["peer_wq"])[0],
        "ple_w_gate": f(inputs["ple_w_gate"])[0], "ple_w_proj": f(inputs["ple_w_proj"])[0],
        "norm_mix_g": f(inputs["norm_mix_g"]), "norm_ffn_g": f(inputs["norm_ffn_g"]),
        "norm_ple_g": f(inputs["norm_ple_g"]), "final_norm_g": f(inputs["final_norm_g"])[None, :],
        "diff_norm_g": f(inputs["diff_norm_g"]), "lam4": lam4, "colp": colp,
        "lru_wa": f(inputs["lru_wa"])[0], "lru_wx": f(inputs["lru_wx"])[0],
        "keys1T": np.ascontiguousarray(f(inputs["peer_keys1"])[0].T), "keys2T": np.ascontiguousarray(f(inputs["peer_keys2"])[0].T),
        "peer_u": f(inputs["peer_u"])[0], "peer_v": f(inputs["peer_v"])[0],
    }
    maps = []
    for bi in range(B):
        m = dict(shared)
        m["x"] = np.ascontiguousarray(x[bi])
        m["p"] = np.ascontiguousarray(f(inputs["p"])[0, bi])
        m["pos"] = np.ascontiguousarray(f(inputs["positions"])[bi].astype(np.int32).reshape(NT, 128).T)
        maps.append(m)
    return maps


_NC_CACHE = {}


def kernel(**inputs):
    x = np.asarray(inputs["x"])
    B, S, _ = x.shape
    if S not in _NC_CACHE:
        _NC_CACHE[S] = build(S)
    nc = _NC_CACHE[S]
    maps = make_in_maps(inputs, S)
    res = run_bass_kernel_spmd(nc, maps, core_ids=list(range(B)))
    out = np.stack([np.asarray(r["out"]) for r in res.results], axis=0).astype(np.float32)
    return out
```

```python
import math
import contextlib
import numpy as np
import concourse.bass as bass
import concourse.mybir as mybir
from concourse.bass_utils import run_bass_kernel_spmd

F32 = mybir.dt.float32
BF16 = mybir.dt.bfloat16
I32 = mybir.dt.int32
U32 = mybir.dt.uint32
AF = mybir.ActivationFunctionType
ALU = mybir.AluOpType
AX = mybir.AxisListType

SB_LO = 16512
SB_HI = 229344
EPS = 1e-6
D = 1024
NEXP = 16384
PI = math.pi


class Tok:
    __slots__ = ("w", "r")

    def __init__(self):
        self.w = None
        self.r = {}


class Src:
    def __init__(self, sem, name):
        self.sem = sem
        self.name = name
        self.count = 0


class Eng(Src):
    def __init__(self, K, h, sem, name):
        super().__init__(sem, name)
        self.K = K
        self.h = h
        self.seen = {}

    def wait(self, ev):
        if ev is None:
            return
        src, val = ev
        if self.seen.get(src, 0) >= val:
            return
        self.h.wait_ge(src.sem, val)
        self.seen[src] = val

    def _deps(self, reads, writes, extra):
        for t in reads:
            if t.w is not None and t.w[0] is self and self is self.K.pe:
                continue
            self.wait(t.w)
        for t in writes:
            if t.w is not None and t.w[0] is not self:
                self.wait(t.w)
            for s, v in t.r.items():
                if s is not self:
                    self.wait((s, v))
        for e in extra:
            self.wait(e)

    def _mark(self, ev, reads, writes):
        for t in reads:
            if t.r.get(ev[0], 0) < ev[1]:
                t.r[ev[0]] = ev[1]
        for t in writes:
            t.w = ev
            t.r = {}

    def op(self, fn, reads=(), writes=(), extra=()):
        self._deps(reads, writes, extra)
        ins = fn(self.h)
        self.count += 1
        ins.then_inc(self.sem, 1)
        ev = (self, self.count)
        self._mark(ev, reads, writes)
        return ev

    def dma(self, pairs, reads=(), writes=(), extra=(), fn=None):
        K = self.K
        ds = K.next_dsem()
        if ds.count:
            self.wait((ds, ds.count))
        self._deps(reads, writes, extra)
        for (o, i) in pairs:
            if fn is not None:
                ins = fn(self.h, o, i)
            else:
                ins = self.h.dma_start(out=o, in_=i)
            ds.count += 16
            ins.then_inc(ds.sem, 16)
        ev = (ds, ds.count)
        self._mark(ev, reads, writes)
        return ev


class Kern:
    def __init__(self, nc, es, n_dsem=56):
        self.nc = nc
        self.es = es
        mk = lambda n: es.enter_context(nc.semaphore(n))
        self.pe = Eng(self, nc.tensor, mk("s_pe"), "pe")
        self.act = Eng(self, nc.scalar, mk("s_act"), "act")
        self.dve = Eng(self, nc.vector, mk("s_dve"), "dve")
        self.pool = Eng(self, nc.gpsimd, mk("s_pool"), "pool")
        self.sp = Eng(self, nc.sync, mk("s_sp"), "sp")
        self.engs = [self.pe, self.act, self.dve, self.pool, self.sp]
        self.dsems = [Src(mk(f"s_d{i}"), f"d{i}") for i in range(n_dsem)]
        self.di = 0
        self.sb_off = SB_LO
        self.sb_peak = SB_LO
        self.n_alloc = 0
        self.pes = None

    def next_dsem(self):
        d = self.dsems[self.di]
        self.di = (self.di + 1) % len(self.dsems)
        return d

    def sb(self, shape, dt, name=None, at=None):
        esz = {F32: 4, BF16: 2, I32: 4, U32: 4}[dt]
        n = 1
        for s in shape[1:]:
            n *= s
        nbytes = (n * esz + 63) // 64 * 64
        if at is not None:
            self.n_alloc += 1
            return self.nc.alloc_sbuf_tensor_at(f"{name or 't'}_{self.n_alloc}", list(shape), dt, offset=at)
        off = self.sb_off
        assert off + nbytes <= SB_HI, f"SBUF overflow {name} {off}+{nbytes}"
        self.sb_off += nbytes
        self.sb_peak = max(self.sb_peak, self.sb_off)
        self.n_alloc += 1
        return self.nc.alloc_sbuf_tensor_at(f"{name or 't'}_{self.n_alloc}", list(shape), dt, offset=off)

    def mark(self):
        return self.sb_off

    def release(self, m):
        self.sb_off = m

    def ps(self, shape, dt=F32, name=None):
        self.n_alloc += 1
        return self.pes.enter_context(self.nc.psum_tensor(f"{name or 'ps'}_{self.n_alloc}", list(shape), dt))

    def barrier(self):
        srcs = [(e, e.count) for e in self.engs if e.count] + [(d, d.count) for d in self.dsems if d.count]
        for e in self.engs:
            for ev in srcs:
                e.wait(ev)


def build(S, debug=False, stop_after=None):
    NT = S // 128
    NG = S // 512
    L = min(1024, S)
    NSEG = S // L
    nc = bass.Bass("TRN2", target_bir_lowering=False)

    def din(n, s, d=F32):
        return nc.dram_tensor(n, list(s), d, kind="ExternalInput").ap()

    x_d = din("x", [S, D])
    p_d = din("p", [S, 256])
    pos_d = din("pos", [128, NT], I32)
    cst_d = din("consts", [128, 152])
    w_in_d = din("w_in", [D, 2560])
    w_out_d = din("w_out", [D, D])
    wq_d = din("peer_wq", [D, D])
    wg_d = din("ple_w_gate", [D, D])
    wp_d = din("ple_w_proj", [256, D])
    gmix_d = din("norm_mix_g", [1, D])
    gffn_d = din("norm_ffn_g", [1, D])
    gple_d = din("norm_ple_g", [1, D])
    gfin_d = din("final_norm_g", [1, D])
    gdiff_d = din("diff_norm_g", [1, 128])
    lam_d = din("lam4", [1, 256])
    colp_d = din("colp", [128, 48])
    wa_d = din("lru_wa", [2, 8, 64, 64])
    wx_d = din("lru_wx", [2, 8, 64, 64])
    k1T_d = din("keys1T", [64, 128])
    k2T_d = din("keys2T", [64, 128])
    pu_d = din("peer_u", [NEXP, D])
    pv_d = din("peer_v", [NEXP, D])
    out_d = nc.dram_tensor("out", [S, D], F32, kind="ExternalOutput").ap()
    lru_scr = nc.dram_tensor("lru_scr", [1024, S], F32, kind="Internal").ap()
    h1_scr = nc.dram_tensor("h1_scr", [S, D], F32, kind="Internal").ap()
    puv_bf = nc.dram_tensor("puv_bf", [NEXP, 2 * D], BF16, kind="Internal").ap()

    with contextlib.ExitStack() as es:
        K = Kern(nc, es)
        pe, act, dve, pool, sp = K.pe, K.act, K.dve, K.pool, K.sp

        def cast_load(dst3, src2, nk, ncol, tok):
            pairs = []
            for kc in range(nk):
                for c0 in range(0, ncol, 1024):
                    c1 = min(ncol, c0 + 1024)
                    pairs.append((dst3[:, kc, c0:c1], src2[kc * 128:(kc + 1) * 128, c0:c1]))
            return pool.dma(pairs, writes=[tok])

        def bcast_load(dst, src_row, tok):
            return sp.dma([(dst, src_row.partition_broadcast(128))], writes=[tok])

        cst = K.sb([128, 152], F32, "cst")
        t_cst = Tok()
        sp.dma([(cst[:], cst_d)], writes=[t_cst])
        ident = K.sb([128, 128], BF16, "ident")
        t_id = Tok()
        dve.op(lambda e: e.tensor_copy(out=ident[:], in_=cst[:, 0:128]), reads=[t_cst], writes=[t_id])
        iota16 = cst[:, 128:144]
        invf = cst[:, 144:152]
        one_c = K.sb([128, 1], F32, "one")
        t_one = Tok()
        dve.op(lambda e: e.memset(one_c[:], 1.0), writes=[t_one])
        ones_bf = K.sb([128, 1], BF16, "onesbf")
        t_onesbf = Tok()
        dve.op(lambda e: e.memset(ones_bf[:], 1.0), writes=[t_onesbf])
        junk = K.sb([128, 1024], F32, "junk")
        t_junk = Tok()
        rstd_rec = K.sb([128, NT], F32, "rstdrec")
        t_rstd_rec = Tok()

        def rstd_from_ssq(ssq_ap, n, out_ap, tok_in, tok_out, shape_tmp):
            dve.op(lambda e: e.tensor_scalar(out=out_ap, in0=ssq_ap, scalar1=1.0 / n, scalar2=EPS,
                                             op0=ALU.mult, op1=ALU.add), reads=[tok_in], writes=[tok_out])
            act.op(lambda e: e.activation(out=out_ap, in_=out_ap, func=AF.Sqrt), reads=[tok_out], writes=[tok_out])
            dve.op(lambda e: e.reciprocal(out=out_ap, in_=out_ap), reads=[tok_out], writes=[tok_out])

        lamt = K.sb([128, 256], F32, "lamt")
        t_lam = Tok()
        bcast_load(lamt[:], lam_d[0], t_lam)
        lsum = K.sb([128, 2], F32, "lsum")
        neglam = K.sb([128, 1], F32, "neglam")
        t_nl = Tok()
        dve.op(lambda e: e.scalar_tensor_tensor(out=junk[:, 0:64], in0=lamt[:, 0:64], scalar=1.0, in1=lamt[:, 64:128],
                                                op0=ALU.mult, op1=ALU.mult, accum_out=lsum[:, 0:1]),
               reads=[t_lam], writes=[t_nl, t_junk])
        dve.op(lambda e: e.scalar_tensor_tensor(out=junk[:, 0:64], in0=lamt[:, 128:192], scalar=1.0, in1=lamt[:, 192:256],
                                                op0=ALU.mult, op1=ALU.mult, accum_out=lsum[:, 1:2]),
               reads=[t_lam], writes=[t_nl, t_junk])
        act.op(lambda e: e.activation(out=lsum[:], in_=lsum[:], func=AF.Exp), reads=[t_nl], writes=[t_nl])
        dve.op(lambda e: e.tensor_tensor(out=neglam[:], in0=lsum[:, 1:2], in1=lsum[:, 0:1], op=ALU.subtract),
               reads=[t_nl], writes=[t_nl])
        dve.op(lambda e: e.tensor_scalar(out=neglam[:], in0=neglam[:], scalar1=-0.2, scalar2=None, op0=ALU.add),
               reads=[t_nl], writes=[t_nl])

        posi = K.sb([128, NT], I32, "posi")
        t_pos = Tok()
        sp.dma([(posi[:], pos_d)], writes=[t_pos])
        posf = K.sb([128, NT], F32, "posf")
        ang = K.sb([128, NT, 8], F32, "ang")
        t_ang = Tok()
        dve.op(lambda e: e.tensor_copy(out=posf[:], in_=posi[:]), reads=[t_pos], writes=[t_ang])
        dve.op(lambda e: e.tensor_tensor(out=ang[:], in0=posf[:].unsqueeze(2).broadcast_to([128, NT, 8]),
                                         in1=invf.unsqueeze(1).broadcast_to([128, NT, 8]), op=ALU.mult),
               reads=[t_ang, t_cst], writes=[t_ang])
        cosk = K.sb([128, NT, 8], F32, "cosk")
        sink = K.sb([128, NT, 8], F32, "sink")
        cosq = K.sb([128, NT, 8], F32, "cosq")
        sinq = K.sb([128, NT, 8], F32, "sinq")
        t_trig = Tok()
        colp = K.sb([128, 48], F32, "colp")
        scl = K.sb([128, 4, 2], F32, "scl")
        scl2 = K.sb([128, 4, 2], F32, "scl2")
        Wbd = K.sb([128, 16, 128], BF16, "Wbd")
        keysbd = K.sb([128, 256], BF16, "keysbd")
        m_trig = K.mark()
        wst = K.sb([128, 16, 128], F32, "wst")
        kst = K.sb([128, 256], F32, "kst")
        tr_a = K.sb([128, NT, 8], F32)
        tr_n = K.sb([128, NT, 8], F32)
        tr_i = K.sb([128, NT, 8], I32)
        tr_m = K.sb([128, NT, 8], F32)
        t_tr = Tok()
        for dst, shift in ((sink, 0.0), (cosk, PI / 2)):
            dve.op(lambda e: e.tensor_scalar(out=tr_a[:], in0=ang[:], scalar1=shift, scalar2=None, op0=ALU.add),
                   reads=[t_ang], writes=[t_tr])
            dve.op(lambda e: e.tensor_scalar(out=tr_n[:], in0=tr_a[:], scalar1=1.0 / (2 * PI), scalar2=None, op0=ALU.mult),
                   reads=[t_tr], writes=[t_tr])
            dve.op(lambda e: e.tensor_copy(out=tr_i[:], in_=tr_n[:]), reads=[t_tr], writes=[t_tr])
            dve.op(lambda e: e.tensor_copy(out=tr_n[:], in_=tr_i[:]), reads=[t_tr], writes=[t_tr])
            dve.op(lambda e: e.scalar_tensor_tensor(out=tr_a[:], in0=tr_n[:], scalar=-2 * PI, in1=tr_a[:],
                                                    op0=ALU.mult, op1=ALU.add), reads=[t_tr], writes=[t_tr])
            dve.op(lambda e: e.tensor_single_scalar(out=tr_m[:], in_=tr_a[:], scalar=PI, op=ALU.is_gt),
                   reads=[t_tr], writes=[t_tr])
            dve.op(lambda e: e.scalar_tensor_tensor(out=tr_a[:], in0=tr_m[:], scalar=-2 * PI, in1=tr_a[:],
                                                    op0=ALU.mult, op1=ALU.add), reads=[t_tr], writes=[t_tr])
            dve.op(lambda e: e.tensor_single_scalar(out=tr_m[:], in_=tr_a[:], scalar=-PI, op=ALU.is_lt),
                   reads=[t_tr], writes=[t_tr])
            dve.op(lambda e: e.scalar_tensor_tensor(out=tr_a[:], in0=tr_m[:], scalar=2 * PI, in1=tr_a[:],
                                                    op0=ALU.mult, op1=ALU.add), reads=[t_tr], writes=[t_tr])
            dve.op(lambda e: e.tensor_scalar(out=tr_a[:], in0=tr_a[:], scalar1=3.1415925, scalar2=-3.1415925,
                                             op0=ALU.min, op1=ALU.max), reads=[t_tr], writes=[t_tr])
            act.op(lambda e: e.activation(out=dst[:], in_=tr_a[:], func=AF.Sin), reads=[t_tr], writes=[t_trig])
        dve.op(lambda e: e.tensor_scalar(out=cosq[:], in0=cosk[:], scalar1=0.125, scalar2=None, op0=ALU.mult),
               reads=[t_trig], writes=[t_trig])
        dve.op(lambda e: e.tensor_scalar(out=sinq[:], in0=sink[:], scalar1=0.125, scalar2=None, op0=ALU.mult),
               reads=[t_trig], writes=[t_trig])
        t_colp = Tok()
        sp.dma([(colp[:], colp_d)], writes=[t_colp])
        colp3 = colp[:].rearrange("p (c k) -> p c k", k=12)
        t_scl = Tok()
        act.op(lambda e: e.activation(out=scl[:], in_=colp3[:, :, 9:11], func=AF.Exp, scale=-1.0), reads=[t_colp], writes=[t_scl])
        dve.op(lambda e: e.tensor_scalar(out=scl[:], in0=scl[:], scalar1=1.0, scalar2=None, op0=ALU.add), reads=[t_scl], writes=[t_scl])
        act.op(lambda e: e.activation(out=scl[:], in_=scl[:], func=AF.Ln), reads=[t_scl], writes=[t_scl])
        dve.op(lambda e: e.tensor_scalar(out=scl2[:], in0=scl[:], scalar1=-16.0, scalar2=None, op0=ALU.mult), reads=[t_scl], writes=[t_scl])
        dve.op(lambda e: e.tensor_scalar(out=scl[:], in0=scl[:], scalar1=-8.0, scalar2=None, op0=ALU.mult), reads=[t_scl], writes=[t_scl])
        t_wst, t_Wbd = Tok(), Tok()
        dve.op(lambda e: e.memset(wst[:], 0.0), writes=[t_wst])
        pairs = []
        for typ, wd in ((0, wa_d), (1, wx_d)):
            for d in range(2):
                base = (typ * 2 + d) * 4
                for ehalf in range(2):
                    src = wd[d, ehalf::2].rearrange("n i o -> i n o")
                    pairs.append((wst[ehalf * 64:(ehalf + 1) * 64, base:base + 4, ehalf * 64:(ehalf + 1) * 64], src))
        sp.dma(pairs, writes=[t_wst])
        dve.op(lambda e: e.tensor_copy(out=Wbd[:], in_=wst[:]), reads=[t_wst], writes=[t_Wbd])

        t_kst, t_keys = Tok(), Tok()
        dve.op(lambda e: e.memset(kst[:], 0.0), writes=[t_kst])
        sp.dma([(kst[0:64, 0:128], k1T_d), (kst[64:128, 128:256], k2T_d)], writes=[t_kst])
        dve.op(lambda e: e.tensor_copy(out=keysbd[:], in_=kst[:]), reads=[t_kst], writes=[t_keys])

        K.barrier()
        K.release(m_trig)
        if stop_after == "setup":
            return nc

        attnT = K.sb([128, 4, S], BF16, "attnT", at=SB_HI - 8 * S)
        yT = K.sb([128, 4, S], BF16, "yT", at=SB_HI - 16 * S)
        t_attnT = Tok()
        t_yT = Tok()

        t_xr = Tok()

        def rope(ps3, dst3, ct, st, t, t_ps, t_dst, tmp):
            C = ct[:, t, :].unsqueeze(1).broadcast_to([128, 8, 8])
            Sn = st[:, t, :].unsqueeze(1).broadcast_to([128, 8, 8])
            t1, t2, t3, t4, xr = tmp
            tt = Tok()
            act.op(lambda e: e.activation(out=xr[:], in_=ps3[:, :, 0:16], func=AF.Copy), reads=[t_ps], writes=[t_xr])
            t_ps = t_xr
            x1 = xr[:, :, 0:8]
            x2 = xr[:, :, 8:16]
            dve.op(lambda e: e.tensor_tensor(out=t1[:], in0=x1, in1=C, op=ALU.mult), reads=[t_ps, t_trig], writes=[tt])
            dve.op(lambda e: e.tensor_tensor(out=t2[:], in0=x2, in1=Sn, op=ALU.mult), reads=[t_ps, t_trig], writes=[tt])
            dve.op(lambda e: e.tensor_tensor(out=t3[:], in0=x2, in1=C, op=ALU.mult), reads=[t_ps, t_trig], writes=[tt])
            dve.op(lambda e: e.tensor_tensor(out=t4[:], in0=x1, in1=Sn, op=ALU.mult), reads=[t_ps, t_trig], writes=[tt])
            dve.op(lambda e: e.tensor_tensor(out=dst3[:, :, 0:8], in0=t1[:], in1=t2[:], op=ALU.subtract),
                   reads=[tt], writes=[t_dst])
            dve.op(lambda e: e.tensor_tensor(out=dst3[:, :, 8:16], in0=t3[:], in1=t4[:], op=ALU.add),
                   reads=[tt], writes=[t_dst])

        mA = K.mark()
        QT = K.sb([128, 4, S], BF16, "QT")
        KT = K.sb([128, 4, S], BF16, "KT")
        Vs = K.sb([128, NT, 4, 129], BF16, "V")
        t_QT, t_KT, t_V = Tok(), Tok(), Tok()
        dve.op(lambda e: e.memset(Vs[:, :, :, 128:129], 1.0), writes=[t_V])
        mA2 = K.mark()
        w_in_sb = K.sb([128, 8, 2560], BF16, "w_in")
        t_win = Tok()
        t_win_l = Tok()
        pool.dma([(w_in_sb[:, kc, c0:c0 + 768], w_in_d[kc * 128:(kc + 1) * 128, c0:c0 + 768])
                  for kc in range(8) for c0 in (0, 768)], writes=[t_win])
        pool.dma([(w_in_sb[:, kc, 1536:2560], w_in_d[kc * 128:(kc + 1) * 128, 1536:2560]) for kc in range(8)], writes=[t_win_l])
        gmix = K.sb([128, D], F32, "gmix")
        t_gmix = Tok()
        bcast_load(gmix[:], gmix_d[0], t_gmix)
        xts = [K.sb([128, D], F32, f"xt{i}") for i in range(4)]
        t_xt = [Tok() for _ in range(4)]
        xnb = [K.sb([128, D], BF16, f"xnb{i}") for i in range(2)]
        t_xnb = [Tok() for _ in range(2)]
        xnT = [K.sb([128, 8, 512], BF16, f"xnT{i}") for i in range(2)]
        t_xnT = [Tok() for _ in range(2)]
        ssqA = K.sb([128, 4], F32, "ssqA")
        rstdA = K.sb([128, 4], F32, "rstdA")
        t_ssqA, t_rstdA = Tok(), Tok()
        q_sb = K.sb([128, 8, 64], BF16, "q_sb")
        k_sb = K.sb([128, 8, 64], BF16, "k_sb")
        t_qsb, t_ksb = Tok(), Tok()
        rtmp = [K.sb([128, 8, 8], F32) for _ in range(4)] + [K.sb([128, 8, 16], F32)]
        stage = [K.sb([128, 512], F32, f"stg{i}") for i in range(3)]
        t_stage = [Tok() for _ in range(3)]
        if stop_after == "A0":
            K.barrier()
            return nc
        with contextlib.ExitStack() as pes:
            K.pes = pes
            ps_tr = [K.ps([128, 8, 128], BF16) for _ in range(2)]
            t_ps_tr = [Tok(), Tok()]
            ps_qkv = [K.ps([128, 512], F32) for _ in range(3)]
            t_ps_qkv = [Tok() for _ in range(3)]
            ps_qk = K.ps([128, 8, 128], BF16)
            t_ps_qk = Tok()
            ps_l = [K.ps([128, 512], F32) for _ in range(2)]
            t_ps_l = [Tok(), Tok()]
            itr = 0
            il = 0
            for g in range(NG):
                b = g % 2
                for j in range(4):
                    t = 4 * g + j
                    sp.dma([(xts[j][:], x_d[t * 128:(t + 1) * 128, :])], writes=[t_xt[j]])
                    dve.op(lambda e: e.scalar_tensor_tensor(out=junk[:], in0=xts[j][:], scalar=1.0, in1=xts[j][:],
                                                            op0=ALU.mult, op1=ALU.mult, accum_out=ssqA[:, j:j + 1]),
                           reads=[t_xt[j]], writes=[t_ssqA, t_junk])
                rstd_from_ssq(ssqA[:], D, rstdA[:], t_ssqA, t_rstdA, None)
                for j in range(4):
                    xb = j % 2
                    dve.op(lambda e: e.scalar_tensor_tensor(out=xnb[xb][:], in0=xts[j][:], scalar=rstdA[:, j:j + 1],
                                                            in1=gmix[:], op0=ALU.mult, op1=ALU.mult),
                           reads=[t_xt[j], t_rstdA, t_gmix], writes=[t_xnb[xb]])
                    pt = itr % 2
                    itr += 1
                    for kc in range(8):
                        pe.op(lambda e: e.transpose(out=ps_tr[pt][:, kc, :], in_=xnb[xb][:, kc * 128:(kc + 1) * 128],
                                                    identity=ident[:]),
                              reads=[t_xnb[xb], t_id], writes=[t_ps_tr[pt]])
                    act.op(lambda e: e.activation(out=xnT[b][:, :, j * 128:(j + 1) * 128], in_=ps_tr[pt][:], func=AF.Copy),
                           reads=[t_ps_tr[pt]], writes=[t_xnT[b]])
                if stop_after == "A1":
                    K.barrier()
                    return nc
                for j in range(4):
                    t = 4 * g + j
                    for cg in range(3):
                        for kc in range(8):
                            pe.op(lambda e: e.matmul(ps_qkv[cg][:], lhsT=xnT[b][:, kc, j * 128:(j + 1) * 128],
                                                     rhs=w_in_sb[:, kc, cg * 512:(cg + 1) * 512],
                                                     start=(kc == 0), stop=(kc == 7)),
                                  reads=[t_xnT[b], t_win], writes=[t_ps_qkv[cg]])
                    psq3 = ps_qkv[0][:].rearrange("p (g d) -> p g d", d=64)
                    psk3 = ps_qkv[1][:].rearrange("p (g d) -> p g d", d=64)
                    act.op(lambda e: e.activation(out=q_sb[:, :, 16:64], in_=psq3[:, :, 16:64], func=AF.Copy, scale=0.125),
                           reads=[t_ps_qkv[0]], writes=[t_qsb])
                    if stop_after == "A2a":
                        K.barrier()
                        return nc
                    rope(psq3, q_sb, cosq, sinq, t, t_ps_qkv[0], t_qsb, rtmp)
                    if stop_after == "A2b":
                        K.barrier()
                        return nc
                    act.op(lambda e: e.activation(out=k_sb[:, :, 16:64], in_=psk3[:, :, 16:64], func=AF.Copy),
                           reads=[t_ps_qkv[1]], writes=[t_ksb])
                    rope(psk3, k_sb, cosk, sink, t, t_ps_qkv[1], t_ksb, rtmp)
                    act.op(lambda e: e.activation(out=Vs[:, t, :, 0:128],
                                                  in_=ps_qkv[2][:].rearrange("p (h d) -> p h d", d=128), func=AF.Copy),
                           reads=[t_ps_qkv[2]], writes=[t_V])
                    if stop_after == "A2c":
                        K.barrier()
                        return nc
                    qf = q_sb[:].rearrange("p g d -> p (g d)")
                    kf = k_sb[:].rearrange("p g d -> p (g d)")
                    for h in range(4):
                        pe.op(lambda e: e.transpose(out=ps_qk[:, h, :], in_=qf[:, h * 128:(h + 1) * 128], identity=ident[:]),
                              reads=[t_qsb, t_id], writes=[t_ps_qk])
                    for h in range(4):
                        pe.op(lambda e: e.transpose(out=ps_qk[:, 4 + h, :], in_=kf[:, h * 128:(h + 1) * 128], identity=ident[:]),
                              reads=[t_ksb, t_id], writes=[t_ps_qk])
                    if stop_after == "A2d":
                        K.barrier()
                        return nc
                    act.op(lambda e: e.activation(out=QT[:, :, t * 128:(t + 1) * 128], in_=ps_qk[:, 0:4, :], func=AF.Copy),
                           reads=[t_ps_qk], writes=[t_QT])
                    act.op(lambda e: e.activation(out=KT[:, :, t * 128:(t + 1) * 128], in_=ps_qk[:, 4:8, :], func=AF.Copy),
                           reads=[t_ps_qk], writes=[t_KT])
                if stop_after == "A2":
                    K.barrier()
                    return nc
                for cc in range(8):
                    pl = il % 2
                    sg = il % 3
                    il += 1
                    for kc in range(8):
                        pe.op(lambda e: e.matmul(ps_l[pl][:], lhsT=w_in_sb[:, kc, 1536 + cc * 128:1536 + (cc + 1) * 128],
                                                 rhs=xnT[b][:, kc, :], start=(kc == 0), stop=(kc == 7)),
                              reads=[t_xnT[b], t_win_l], writes=[t_ps_l[pl]])
                    act.op(lambda e: e.activation(out=stage[sg][:], in_=ps_l[pl][:], func=AF.Copy),
                           reads=[t_ps_l[pl]], writes=[t_stage[sg]])
                    sp.dma([(lru_scr[cc * 128:(cc + 1) * 128, g * 512:(g + 1) * 512], stage[sg][:])], reads=[t_stage[sg]])
        K.barrier()
        K.release(mA2)
        if stop_after == "A":
            return nc

        gd = K.sb([128, 128], F32, "gdiff")
        t_gd = Tok()
        bcast_load(gd[:], gdiff_d[0], t_gd)
        dve.op(lambda e: e.tensor_scalar(out=gd[:], in0=gd[:], scalar1=0.8, scalar2=None, op0=ALU.mult),
               reads=[t_gd], writes=[t_gd])
        Qz = [[K.sb([128, 512], BF16) for _ in range(2)] for _ in range(2)]
        t_Qz = [[Tok(), Tok()], [Tok(), Tok()]]
        for sl_ in range(2):
            pool.op(lambda e: e.memset(Qz[0][sl_][:], 0.0), writes=[t_Qz[0][sl_]])
            pool.op(lambda e: e.memset(Qz[1][sl_][:], 0.0), writes=[t_Qz[1][sl_]])
        iq = 0
        pT = [[K.sb([128, 512], BF16) for _ in range(2)] for _ in range(2)]
        t_pT = [[Tok(), Tok()], [Tok(), Tok()]]
        o_sb = [K.sb([128, 128], F32) for _ in range(4)]
        t_osb = [Tok() for _ in range(4)]
        otmp = K.sb([128, 128], F32)
        rr = K.sb([128, 8], F32)
        ssB = K.sb([128, 4], F32)
        rsB = K.sb([128, 4], F32)
        t_rr, t_ssB, t_rsB, t_otmp = Tok(), Tok(), Tok(), Tok()
        on_bf = K.sb([128, 4, 128], BF16)
        t_on = Tok()
        cstg = [K.sb([128, 4, D], BF16) for _ in range(2)]
        t_cstg = [Tok(), Tok()]
        cstf = [K.sb([128, 4, D], F32) for _ in range(2)]
        t_cstf = [Tok(), Tok()]
        t_tab = Tok()
        conv_state = {"i": 0}
        NCONV = 2 * (NEXP // 512)

        def conv_parts(i):
            src_t, hcol = (pu_d, 0) if i < NCONV // 2 else (pv_d, 1)
            ci = i % (NCONV // 2)
            rows = slice(ci * 512, (ci + 1) * 512)
            return src_t, hcol, rows

        def conv_load(i):
            src_t, hcol, rows = conv_parts(i)
            sp.dma([(cstf[i % 2][:], src_t[rows, :].rearrange("(p r) d -> p r d", r=4))], writes=[t_cstf[i % 2]])

        def emit_conv(n):
            for _ in range(n):
                i = conv_state["i"]
                if i >= NCONV:
                    return
                conv_state["i"] = i + 1
                if i == 0:
                    conv_load(0)
                if i + 1 < NCONV:
                    conv_load(i + 1)
                src_t, hcol, rows = conv_parts(i)
                sg_ = i % 2
                dve.op(lambda e: e.tensor_copy(out=cstg[sg_][:], in_=cstf[sg_][:]), reads=[t_cstf[sg_]], writes=[t_cstg[sg_]])
                pool.dma([(puv_bf[rows, hcol * D:(hcol + 1) * D].rearrange("(p r) d -> p r d", r=4), cstg[sg_][:])],
                         reads=[t_cstg[sg_]], writes=[t_tab])

        n_iter_B = 4 * NG * NT
        conv_per_iter = -(-NCONV // n_iter_B)
        conv_stride = max(1, n_iter_B // NCONV)
        it_B = {"n": 0}
        with contextlib.ExitStack() as pes:
            K.pes = pes
            ps_s = [[K.ps([128, 512], F32) for _ in range(2)] for _ in range(2)]
            t_ps_s = [[Tok(), Tok()], [Tok(), Tok()]]
            ps_acc = K.ps([128, 3, 512], F32)
            t_acc = Tok()
            ps_oT = K.ps([128, 4, 128], BF16)
            t_oT = Tok()

            def acc(m, j):
                a = m * 4 + j
                return ps_acc[:, a // 3, (a % 3) * 129:(a % 3) * 129 + 129]

            for h in range(4):
                for qg in range(NG):
                    qs = slice(qg * 512, (qg + 1) * 512)
                    zq = iq % 2
                    iq += 1
                    pool.op(lambda e: e.tensor_copy(out=Qz[0][zq][0:64, :], in_=QT[0:64, h, qs]), reads=[t_QT], writes=[t_Qz[0][zq]])
                    pool.op(lambda e: e.tensor_copy(out=Qz[1][zq][64:128, :], in_=QT[64:128, h, qs]), reads=[t_QT], writes=[t_Qz[1][zq]])

                    def emit_s(kt):
                        sl = kt % 2
                        for m in range(2):
                            pe.op(lambda e: e.matmul(ps_s[m][sl][:], lhsT=KT[:, h, kt * 128:(kt + 1) * 128],
                                                     rhs=Qz[m][zq][:], start=True, stop=True),
                                  reads=[t_KT, t_Qz[m][zq]], writes=[t_ps_s[m][sl]])

                    emit_s(0)
                    for kt in range(NT):
                        sl = kt % 2
                        if it_B["n"] % conv_stride == 0:
                            emit_conv(conv_per_iter)
                        it_B["n"] += 1
                        if kt + 1 < NT:
                            emit_s(kt + 1)
                        for m in range(2):
                            act.op(lambda e: e.activation(out=pT[m][sl][:], in_=ps_s[m][sl][:], func=AF.Exp),
                                   reads=[t_ps_s[m][sl]], writes=[t_pT[m][sl]])
                        for m in range(2):
                            for j in range(4):
                                a = m * 4 + j
                                pe.op(lambda e: e.matmul(acc(m, j), lhsT=pT[m][sl][:, j * 128:(j + 1) * 128],
                                                         rhs=Vs[:, kt, h, :], start=(kt == 0 and a % 3 == 0),
                                                         stop=(kt == NT - 1)),
                                      reads=[t_pT[m][sl], t_V], writes=[t_acc])
                    for j in range(4):
                        dve.op(lambda e: e.reciprocal(out=rr[:, j:j + 1], in_=acc(0, j)[:, 128:129]),
                               reads=[t_acc], writes=[t_rr])
                        dve.op(lambda e: e.reciprocal(out=rr[:, 4 + j:5 + j], in_=acc(1, j)[:, 128:129]),
                               reads=[t_acc], writes=[t_rr])
                    dve.op(lambda e: e.tensor_tensor(out=rr[:, 4:8], in0=rr[:, 4:8], in1=neglam[:, 0:1].broadcast_to([128, 4]),
                                                     op=ALU.mult), reads=[t_rr, t_nl], writes=[t_rr])
                    for j in range(4):
                        dve.op(lambda e: e.tensor_scalar(out=otmp[:], in0=acc(1, j)[:, 0:128], scalar1=rr[:, 4 + j:5 + j],
                                                         scalar2=None, op0=ALU.mult),
                               reads=[t_acc, t_rr], writes=[t_otmp])
                        dve.op(lambda e: e.scalar_tensor_tensor(out=o_sb[j][:], in0=acc(0, j)[:, 0:128], scalar=rr[:, j:j + 1],
                                                                in1=otmp[:], op0=ALU.mult, op1=ALU.add),
                               reads=[t_acc, t_rr, t_otmp], writes=[t_osb[j]])
                        dve.op(lambda e: e.scalar_tensor_tensor(out=junk[:, 0:128], in0=o_sb[j][:], scalar=1.0, in1=o_sb[j][:],
                                                                op0=ALU.mult, op1=ALU.mult, accum_out=ssB[:, j:j + 1]),
                               reads=[t_osb[j]], writes=[t_ssB, t_junk])
                    rstd_from_ssq(ssB[:], 128, rsB[:], t_ssB, t_rsB, None)
                    for j in range(4):
                        dve.op(lambda e: e.scalar_tensor_tensor(out=on_bf[:, j, :], in0=o_sb[j][:], scalar=rsB[:, j:j + 1],
                                                                in1=gd[:], op0=ALU.mult, op1=ALU.mult),
                               reads=[t_osb[j], t_rsB, t_gd], writes=[t_on])
                    for j in range(4):
                        pe.op(lambda e: e.transpose(out=ps_oT[:, j, :], in_=on_bf[:, j, :], identity=ident[:]),
                              reads=[t_on, t_id], writes=[t_oT])
                    act.op(lambda e: e.activation(out=attnT[:, h, qs].rearrange("p (j q) -> p j q", q=128), in_=ps_oT[:],
                                                  func=AF.Copy), reads=[t_oT], writes=[t_attnT])
        emit_conv(NCONV)
        K.barrier()
        K.release(mA)
        if stop_after == "B":
            return nc

        mC = K.mark()
        w_out_sb = K.sb([128, 8, D], BF16, "w_out")
        t_wout = Tok()
        cast_load(w_out_sb, w_out_d, 8, D, t_wout)
        mC2 = K.mark()
        ubuf = K.sb([128, S], F32, "ubuf")
        gbuf = K.sb([128, S], F32, "gbuf")
        ucf = K.sb([128, S], F32, "ucf")
        ucb = K.sb([128, S], BF16, "ucb")
        ysq = K.sb([128, S], BF16, "ysq")
        t_u, t_g, t_uc, t_ucb, t_ysq, t_hs = Tok(), Tok(), Tok(), Tok(), Tok(), Tok()
        NB = 2
        r_b = [K.sb([128, L], F32) for _ in range(NB)]
        i_b = [K.sb([128, L], F32) for _ in range(NB)]
        a_b = [K.sb([128, L], F32) for _ in range(NB)]
        a2_b = [K.sb([128, L], F32) for _ in range(NB)]
        h_b = [K.sb([128, L], F32) for _ in range(NB)]
        t_r = [Tok() for _ in range(NB)]
        t_i = [Tok() for _ in range(NB)]
        t_a = [Tok() for _ in range(NB)]
        t_a2 = [Tok() for _ in range(NB)]
        t_h = [Tok() for _ in range(NB)]
        carry = K.sb([128, 2], F32, "carry")
        t_carry = Tok()
        with contextlib.ExitStack() as pes:
            K.pes = pes
            ps_ga = [K.ps([128, 512], F32) for _ in range(2)]
            ps_gx = [K.ps([128, 512], F32) for _ in range(2)]
            t_ga = [Tok(), Tok()]
            t_gx = [Tok(), Tok()]
            ps_ssq = K.ps([128, NT], F32)
            t_pssq = Tok()
            ig = 0
            ib = 0
            for c in range(4):
                cp = lambda k: colp[:, c * 12 + k:c * 12 + k + 1]
                sp.dma([(ubuf[:], lru_scr[c * 128:(c + 1) * 128, :])], writes=[t_u, t_hs])
                sp.dma([(gbuf[:], lru_scr[(4 + c) * 128:(5 + c) * 128, :])], writes=[t_g])
                dve.op(lambda e: e.tensor_scalar(out=ucf[:], in0=ubuf[:], scalar1=cp(2), scalar2=cp(4), op0=ALU.mult, op1=ALU.add),
                       reads=[t_u, t_colp], writes=[t_uc])
                dve.op(lambda e: e.scalar_tensor_tensor(out=ucf[:, 2:S], in0=ubuf[:, 0:S - 2], scalar=cp(0), in1=ucf[:, 2:S],
                                                        op0=ALU.mult, op1=ALU.add), reads=[t_u, t_uc, t_colp], writes=[t_uc])
                dve.op(lambda e: e.scalar_tensor_tensor(out=ucf[:, 1:S], in0=ubuf[:, 0:S - 1], scalar=cp(1), in1=ucf[:, 1:S],
                                                        op0=ALU.mult, op1=ALU.add), reads=[t_u, t_uc, t_colp], writes=[t_uc])
                dve.op(lambda e: e.scalar_tensor_tensor(out=ucf[:, 0:S - 1], in0=ubuf[:, 1:S], scalar=cp(3), in1=ucf[:, 0:S - 1],
                                                        op0=ALU.mult, op1=ALU.add), reads=[t_u, t_uc, t_colp], writes=[t_uc])
                act.op(lambda e: e.activation(out=ucb[:], in_=ucf[:], func=AF.Copy), reads=[t_uc], writes=[t_ucb])
                act.op(lambda e: e.activation(out=gbuf[:], in_=gbuf[:], func=AF.Gelu), reads=[t_g], writes=[t_g])
                hs = ubuf
                for d in range(2):
                    segs = list(range(NSEG)) if d == 0 else list(range(NSEG - 1, -1, -1))
                    wia = (0 * 2 + d) * 4 + c
                    wix = (1 * 2 + d) * 4 + c
                    for si, s in enumerate(segs):
                        bi = ib % NB
                        ib += 1
                        for hf in range(L // 512):
                            cs = slice(s * L + hf * 512, s * L + (hf + 1) * 512)
                            ls = slice(hf * 512, (hf + 1) * 512)
                            pg = ig % 2
                            ig += 1
                            pe.op(lambda e: e.matmul(ps_ga[pg][:], lhsT=Wbd[:, wia, :], rhs=ucb[:, cs], start=True, stop=True),
                                  reads=[t_Wbd, t_ucb], writes=[t_ga[pg]])
                            pe.op(lambda e: e.matmul(ps_gx[pg][:], lhsT=Wbd[:, wix, :], rhs=ucb[:, cs], start=True, stop=True),
                                  reads=[t_Wbd, t_ucb], writes=[t_gx[pg]])
                            act.op(lambda e: e.activation(out=r_b[bi][:, ls], in_=ps_ga[pg][:], func=AF.Sigmoid, bias=cp(5 + d), scale=1.0),
                                   reads=[t_ga[pg], t_colp], writes=[t_r[bi]])
                            act.op(lambda e: e.activation(out=i_b[bi][:, ls], in_=ps_gx[pg][:], func=AF.Sigmoid, bias=cp(7 + d), scale=1.0),
                                   reads=[t_gx[pg], t_colp], writes=[t_i[bi]])
                        act.op(lambda e: e.activation(out=a_b[bi][:], in_=r_b[bi][:], func=AF.Exp, scale=scl[:, c, d:d + 1]),
                               reads=[t_r[bi], t_scl], writes=[t_a[bi]])
                        act.op(lambda e: e.activation(out=a2_b[bi][:], in_=r_b[bi][:], func=AF.Exp, scale=scl2[:, c, d:d + 1]),
                               reads=[t_r[bi], t_scl], writes=[t_a2[bi]])
                        act.op(lambda e: e.activation(out=a2_b[bi][:], in_=a2_b[bi][:], func=AF.Sqrt, bias=one_c[:, 0:1], scale=-1.0),
                               reads=[t_a2[bi], t_one], writes=[t_a2[bi]])
                        dve.op(lambda e: e.tensor_tensor(out=i_b[bi][:], in0=i_b[bi][:], in1=ucf[:, s * L:(s + 1) * L], op=ALU.mult),
                               reads=[t_i[bi], t_uc], writes=[t_i[bi]])
                        dve.op(lambda e: e.tensor_tensor(out=i_b[bi][:], in0=i_b[bi][:], in1=a2_b[bi][:], op=ALU.mult),
                               reads=[t_i[bi], t_a2[bi]], writes=[t_i[bi]])
                        init = 0.0 if si == 0 else carry[:, d:d + 1]
                        if d == 0:
                            dve.op(lambda e: e.tensor_tensor_scan(out=hs[:, s * L:(s + 1) * L], data0=a_b[bi][:], data1=i_b[bi][:],
                                                                  initial=init, op0=ALU.mult, op1=ALU.add),
                                   reads=[t_a[bi], t_i[bi], t_carry, t_uc], writes=[t_hs])
                            dve.op(lambda e: e.tensor_copy(out=carry[:, 0:1], in_=hs[:, (s + 1) * L - 1:(s + 1) * L]),
                                   reads=[t_hs], writes=[t_carry])
                        else:
                            dve.op(lambda e: e.tensor_tensor_scan(out=h_b[bi][:, ::-1], data0=a_b[bi][:, ::-1], data1=i_b[bi][:, ::-1],
                                                                  initial=init, op0=ALU.mult, op1=ALU.add),
                                   reads=[t_a[bi], t_i[bi], t_carry], writes=[t_h[bi]])
                            dve.op(lambda e: e.tensor_copy(out=carry[:, 1:2], in_=h_b[bi][:, 0:1]),
                                   reads=[t_h[bi]], writes=[t_carry])
                            pool.op(lambda e: e.tensor_tensor(out=hs[:, s * L:(s + 1) * L], in0=hs[:, s * L:(s + 1) * L],
                                                              in1=h_b[bi][:], op=ALU.add),
                                    reads=[t_hs, t_h[bi]], writes=[t_hs])
                dve.op(lambda e: e.tensor_tensor(out=hs[:], in0=hs[:], in1=gbuf[:], op=ALU.mult), reads=[t_hs, t_g], writes=[t_hs])
                act.op(lambda e: e.activation(out=yT[:, c, :], in_=hs[:], func=AF.Copy, scale=cp(11)),
                       reads=[t_hs, t_colp], writes=[t_yT])
                act.op(lambda e: e.activation(out=ysq[:], in_=hs[:], func=AF.Square), reads=[t_hs], writes=[t_ysq])
                for t in range(NT):
                    pe.op(lambda e: e.matmul(ps_ssq[:, t:t + 1], lhsT=ysq[:, t * 128:(t + 1) * 128], rhs=ones_bf[:, 0:1],
                                             start=(c == 0 and t == 0), stop=(c == 3)),
                          reads=[t_ysq, t_onesbf], writes=[t_pssq])
            rstd_from_ssq(ps_ssq[:], 512, rstd_rec[:], t_pssq, t_rstd_rec, None)
        K.barrier()
        K.release(mC2)
        if stop_after == "C":
            return nc
        hole_off = mC
        wq_sb = K.sb([128, 8, D], BF16, "wq")
        wg_sb = K.sb([128, 8, D], BF16, "wg")
        wp_sb = K.sb([128, 2, D], BF16, "wp")
        t_wq, t_wg, t_wp = Tok(), Tok(), Tok()
        cast_load(wq_sb, wq_d, 8, D, t_wq)
        cast_load(wg_sb, wg_d, 8, D, t_wg)
        cast_load(wp_sb, wp_d, 2, D, t_wp)
        gffn = K.sb([128, D], F32)
        gple = K.sb([128, D], F32)
        gfin = K.sb([128, D], F32)
        t_gffn, t_gple, t_gfin = Tok(), Tok(), Tok()
        bcast_load(gffn[:], gffn_d[0], t_gffn)
        bcast_load(gple[:], gple_d[0], t_gple)
        bcast_load(gfin[:], gfin_d[0], t_gfin)

        mD = K.mark()

        xtd = [K.sb([128, D], F32) for _ in range(2)]
        t_xtd = [Tok(), Tok()]
        h1o = [K.sb([128, D], F32) for _ in range(2)]
        t_h1o = [Tok(), Tok()]
        with contextlib.ExitStack() as pes:
            K.pes = pes
            ps_at = [[K.ps([128, 512], F32) for _ in range(2)] for _ in range(2)]
            ps_rc = [[K.ps([128, 512], F32) for _ in range(2)] for _ in range(2)]
            t_at = [[Tok(), Tok()], [Tok(), Tok()]]
            t_rc = [[Tok(), Tok()], [Tok(), Tok()]]
            for t in range(NT):
                b = t % 2
                ts_ = slice(t * 128, (t + 1) * 128)
                sp.dma([(xtd[b][:], x_d[ts_, :])], writes=[t_xtd[b]])
                for n in range(2):
                    ns = slice(n * 512, (n + 1) * 512)
                    for kc in range(4):
                        pe.op(lambda e: e.matmul(ps_at[b][n][:], lhsT=attnT[:, kc, ts_], rhs=w_out_sb[:, kc, ns],
                                                 start=(kc == 0), stop=(kc == 3)),
                              reads=[t_attnT, t_wout], writes=[t_at[b][n]])
                    for kc in range(4):
                        pe.op(lambda e: e.matmul(ps_rc[b][n][:], lhsT=yT[:, kc, ts_], rhs=w_out_sb[:, 4 + kc, ns],
                                                 start=(kc == 0), stop=(kc == 3)),
                              reads=[t_yT, t_wout], writes=[t_rc[b][n]])
                    dve.op(lambda e: e.scalar_tensor_tensor(out=h1o[b][:, ns], in0=ps_rc[b][n][:], scalar=rstd_rec[:, t:t + 1],
                                                            in1=xtd[b][:, ns], op0=ALU.mult, op1=ALU.add),
                           reads=[t_rc[b][n], t_rstd_rec, t_xtd[b]], writes=[t_h1o[b]])
                    dve.op(lambda e: e.tensor_tensor(out=h1o[b][:, ns], in0=ps_at[b][n][:], in1=h1o[b][:, ns], op=ALU.add),
                           reads=[t_at[b][n], t_h1o[b]], writes=[t_h1o[b]])
                sp.dma([(h1_scr[ts_, :], h1o[b][:])], reads=[t_h1o[b]])
        K.barrier()
        K.release(mD)
        if stop_after == "D":
            return nc

        h1t = [K.sb([128, D], F32) for _ in range(3)]
        ptl = [K.sb([128, 256], F32) for _ in range(3)]
        t_h1t = [Tok() for _ in range(3)]
        t_ptl = [Tok() for _ in range(3)]
        xn2b = [K.sb([128, D], BF16) for _ in range(2)]
        junkb = K.sb([128, D], BF16)
        t_junkb = Tok()
        junk_sq = K.sb([128, D], BF16)
        t_junk_sq = Tok()
        xTF = K.sb([128, 8, 128], BF16)
        xTT = K.sb([128, 8, 128], BF16)
        qTs = K.sb([128, 8, 128], BF16)
        s_sb = K.sb([128, 8, 256], F32)
        t_xn2b = [Tok(), Tok()]
        t_xTF, t_xTT, t_qTs = Tok(), Tok(), Tok()
        t_u = [[Tok(), Tok()] for _ in range(8)]
        t_c = [Tok() for _ in range(8)]
        ssF = K.sb([128, 1], F32)
        rsF = K.sb([128, 1], F32)
        ssT = K.sb([128, 1], F32)
        rsT = K.sb([128, 1], F32)
        t_ssF, t_rsF, t_ssT, t_rsT = Tok(), Tok(), Tok(), Tok()
        v12 = [K.sb([128, 8, 16], F32) for _ in range(2)]
        i12 = [K.sb([128, 8, 16], U32) for _ in range(2)]
        i12f = [K.sb([128, 8, 16], BF16) for _ in range(2)]
        t_tk = Tok()
        cand = K.sb([128, 8, 16, 16], F32)
        scv = K.sb([128, 8, 16], F32)
        posu = K.sb([128, 8, 16], U32)
        pa_i = K.sb([128, 128], I32)
        pa_f = K.sb([128, 2, 128], F32)
        oh = K.sb([128, 128, 16], BF16)
        isel = K.sb([128, 2, 128], BF16)
        idxf = K.sb([128, 128], F32)
        idxi = [K.sb([128, 128], I32) for _ in range(2)]
        t_idx = [Tok(), Tok()]
        gw = [K.sb([128, 8, 16], F32) for _ in range(2)]
        t_gw = [Tok(), Tok()]
        zs = K.sb([128, 8], F32)
        logits = K.sb([128, 128], F32)
        actv = K.sb([128, 128], F32)
        gl4 = K.sb([128, 128], F32)
        NBUF = 14
        GS = 4
        gbufs = [K.sb([128, 2 * D], BF16, at=hole_off + k_ * 4096) for k_ in range(4)] + [K.sb([128, 2 * D], BF16) for _ in range(NBUF - 4)]
        t_gb = [Tok() for _ in range(NBUF)]
        NDG = 4
        diag = [K.sb([128, 128], BF16) for _ in range(NDG)]
        t_diag = [Tok() for _ in range(NDG)]
        t_lg = [Tok() for _ in range(128)]
        t_av = [Tok() for _ in range(128)]
        h2 = [K.sb([128, D], F32) for _ in range(2)]
        t_h2 = [Tok(), Tok()]
        xn3b = K.sb([128, D], BF16)
        gate_sb = K.sb([128, D], F32)
        ptb = K.sb([128, 256], BF16)
        pTs = K.sb([128, 2, 128], BF16)
        h3 = K.sb([128, D], F32)
        outt = [K.sb([128, D], F32) for _ in range(2)]
        t_xn3b, t_gate, t_ptb, t_pTs, t_h3 = Tok(), Tok(), Tok(), Tok(), Tok()
        t_outt = [Tok(), Tok()]

        def gap(n):
            for _ in range(n):
                yield

        def rmsnorm(src, t_src, gain, t_gain, dst, t_dst, ss, rs, t_ss, t_rs):
            act.op(lambda e: e.activation(out=junk_sq[:], in_=src[:], func=AF.Square, accum_out=ss[:, 0:1]),
                   reads=[t_src], writes=[t_ss, t_junk_sq])
            yield from gap(3)
            dve.op(lambda e: e.tensor_scalar(out=rs[:], in0=ss[:], scalar1=1.0 / D, scalar2=EPS, op0=ALU.mult, op1=ALU.add),
                   reads=[t_ss], writes=[t_rs])
            yield from gap(2)
            act.op(lambda e: e.activation(out=rs[:], in_=rs[:], func=AF.Sqrt), reads=[t_rs], writes=[t_rs])
            yield from gap(3)
            dve.op(lambda e: e.reciprocal(out=rs[:], in_=rs[:]), reads=[t_rs], writes=[t_rs])
            yield
            dve.op(lambda e: e.scalar_tensor_tensor(out=dst[:], in0=src[:], scalar=rs[:, 0:1], in1=gain[:],
                                                    op0=ALU.mult, op1=ALU.add if False else ALU.mult),
                   reads=[t_src, t_rs, t_gain], writes=[t_dst])
            yield

        with contextlib.ExitStack() as pes:
            K.pes = pes
            ps_tF = K.ps([128, 8, 128], BF16)
            ps_tT = K.ps([128, 8, 128], BF16)
            t_pstF, t_pstT = Tok(), Tok()
            ps_q = K.ps([128, 4, 128], F32)
            t_psq = Tok()
            ps_sc = K.ps([128, 2, 256], F32)
            t_psc = Tok()
            ps_g = [K.ps([128, 512], F32) for _ in range(2)]
            t_psg = [Tok(), Tok()]
            ps_v = [K.ps([128, 512], F32) for _ in range(2)]
            t_psv = [Tok(), Tok()]

            def front(t):
                b3 = t % 3
                b = t % 2
                ts_ = slice(t * 128, (t + 1) * 128)
                sp.dma([(h1t[b3][:], h1_scr[ts_, :])], writes=[t_h1t[b3]])
                sp.dma([(ptl[b3][:], p_d[ts_, :])], writes=[t_ptl[b3]])
                yield from gap(3)
                yield from rmsnorm(h1t[b3], t_h1t[b3], gffn, t_gffn, xn2b[b], t_xn2b[b], ssF, rsF, t_ssF, t_rsF)
                yield from gap(2)
                for kc in range(8):
                    pe.op(lambda e: e.transpose(out=ps_tF[:, kc, :], in_=xn2b[b][:, kc * 128:(kc + 1) * 128], identity=ident[:]),
                          reads=[t_xn2b[b], t_id], writes=[t_pstF])
                yield from gap(2)
                act.op(lambda e: e.activation(out=xTF[:], in_=ps_tF[:], func=AF.Copy), reads=[t_pstF], writes=[t_xTF])
                yield from gap(2)
                for hh in range(2):
                    for c4 in range(4):
                        cc = hh * 4 + c4
                        for kc in range(8):
                            pe.op(lambda e: e.matmul(ps_q[:, c4, :], lhsT=wq_sb[:, kc, cc * 128:(cc + 1) * 128],
                                                     rhs=xTF[:, kc, :], start=(kc == 0 and c4 == 0), stop=(kc == 7)),
                                  reads=[t_wq, t_xTF], writes=[t_psq])
                        if c4 % 2 == 1:
                            yield
                    yield from gap(2)
                    act.op(lambda e: e.activation(out=qTs[:, hh * 4:(hh + 1) * 4, :], in_=ps_q[:], func=AF.Copy),
                           reads=[t_psq], writes=[t_qTs])
                    yield from gap(2)
                for hp in range(4):
                    for k2 in range(2):
                        hd = hp * 2 + k2
                        pe.op(lambda e: e.matmul(ps_sc[:, k2, :], lhsT=qTs[:, hd, :], rhs=keysbd[:], start=True, stop=True),
                              reads=[t_qTs, t_keys], writes=[t_psc])
                    yield from gap(2)
                    act.op(lambda e: e.activation(out=s_sb[:, hp * 2:(hp + 1) * 2, :], in_=ps_sc[:], func=AF.Copy),
                           reads=[t_psc], writes=[t_u[hp * 2][0], t_u[hp * 2][1], t_u[hp * 2 + 1][0], t_u[hp * 2 + 1][1]])
                    yield
                mode["heavy"] = True
                units = [(hd, sd) for hd in range(8) for sd in range(2)]
                srcu = lambda hd, sd: s_sb[:, hd, sd * 128:(sd + 1) * 128]
                for rnd in range(2):
                    ks = slice(rnd * 8, rnd * 8 + 8)
                    for k_, (hd, sd) in enumerate(units):
                        dve.op(lambda e: e.max(out=v12[sd][:, hd, ks], in_=srcu(hd, sd)), reads=[t_u[hd][sd]], writes=[t_u[hd][sd]])
                        if k_ % 2 == 1:
                            yield
                    for k_, (hd, sd) in enumerate(units):
                        dve.op(lambda e: e.max_index(out=i12[sd][:, hd, ks], in_max=v12[sd][:, hd, ks], in_values=srcu(hd, sd)),
                               reads=[t_u[hd][sd]], writes=[t_u[hd][sd]])
                        if k_ % 2 == 1:
                            yield
                    if rnd == 0:
                        for k_, (hd, sd) in enumerate(units):
                            dve.op(lambda e: e.match_replace(out=srcu(hd, sd), in_to_replace=v12[sd][:, hd, ks], in_values=srcu(hd, sd),
                                                             imm_value=-1e30), reads=[t_u[hd][sd]], writes=[t_u[hd][sd]])
                            if k_ % 2 == 1:
                                yield
                for hd in range(8):
                    dve.op(lambda e: e.tensor_tensor(out=cand[:, hd], in0=v12[0][:, hd, :].unsqueeze(2).broadcast_to([128, 16, 16]),
                                                     in1=v12[1][:, hd, :].unsqueeze(1).broadcast_to([128, 16, 16]), op=ALU.add),
                           reads=[t_u[hd][0], t_u[hd][1]], writes=[t_c[hd]])
                    if hd % 2 == 1:
                        yield
                cfu = lambda hd: cand[:, hd].rearrange("p a b -> p (a b)")
                for rnd in range(2):
                    ks = slice(rnd * 8, rnd * 8 + 8)
                    for hd in range(8):
                        dve.op(lambda e: e.max(out=scv[:, hd, ks], in_=cfu(hd)), reads=[t_c[hd]], writes=[t_c[hd]])
                        if hd % 2 == 1:
                            yield
                    for hd in range(8):
                        dve.op(lambda e: e.max_index(out=posu[:, hd, ks], in_max=scv[:, hd, ks], in_values=cfu(hd)),
                               reads=[t_c[hd]], writes=[t_c[hd]])
                        if hd % 2 == 1:
                            yield
                    if rnd == 0:
                        for hd in range(8):
                            dve.op(lambda e: e.match_replace(out=cfu(hd), in_to_replace=scv[:, hd, ks], in_values=cfu(hd), imm_value=-1e30),
                                   reads=[t_c[hd]], writes=[t_c[hd]])
                            if hd % 2 == 1:
                                yield
                dve.op(lambda e: e.tensor_copy(out=zs[:, 0:1], in_=scv[:, 0, 0:1]), reads=t_c + [t_u[h_][s_] for h_ in range(8) for s_ in range(2)],
                       writes=[t_tk])
                posf_ = posu[:].rearrange("p h k -> p (h k)").bitcast(I32)
                for sd in range(2):
                    if sd == 0:
                        dve.op(lambda e: e.tensor_single_scalar(out=pa_i[:], in_=posf_, scalar=4, op=ALU.arith_shift_right),
                               reads=[t_tk], writes=[t_tk])
                    else:
                        dve.op(lambda e: e.tensor_single_scalar(out=pa_i[:], in_=posf_, scalar=15, op=ALU.bitwise_and),
                               reads=[t_tk], writes=[t_tk])
                    dve.op(lambda e: e.tensor_copy(out=pa_f[:, sd, :], in_=pa_i[:]), reads=[t_tk], writes=[t_tk])
                    dve.op(lambda e: e.tensor_copy(out=i12f[sd][:], in_=i12[sd][:].bitcast(I32)), reads=[t_tk], writes=[t_tk])
                    yield
                    dve.op(lambda e: e.tensor_tensor(out=oh[:], in0=pa_f[:, sd, :].unsqueeze(2).broadcast_to([128, 128, 16]),
                                                     in1=iota16.unsqueeze(1).broadcast_to([128, 128, 16]), op=ALU.is_equal),
                           reads=[t_tk, t_cst], writes=[t_tk])
                    yield
                    oh4 = oh[:].rearrange("p (h s) a -> p h s a", h=8)
                    dve.op(lambda e: e.tensor_tensor(out=oh4, in0=oh4, in1=i12f[sd][:].unsqueeze(2).broadcast_to([128, 8, 16, 16]), op=ALU.mult),
                           reads=[t_tk], writes=[t_tk])
                    yield
                    with nc.allow_low_precision("sum over a one-hot row of an integer id <= 127: exact in bf16"):
                        dve.op(lambda e: e.tensor_reduce(out=isel[:, sd, :], in_=oh[:], axis=AX.X, op=ALU.add), reads=[t_tk], writes=[t_tk])
                    yield
                dve.op(lambda e: e.scalar_tensor_tensor(out=idxf[:], in0=isel[:, 0, :], scalar=128.0, in1=isel[:, 1, :],
                                                        op0=ALU.mult, op1=ALU.add), reads=[t_tk], writes=[t_tk])
                dve.op(lambda e: e.tensor_copy(out=idxi[b][:], in_=idxf[:]), reads=[t_tk], writes=[t_idx[b]])
                mode["heavy"] = False
                yield
                dve.op(lambda e: e.tensor_tensor(out=gw[b][:], in0=scv[:], in1=scv[:, :, 0:1].broadcast_to([128, 8, 16]), op=ALU.subtract),
                       reads=[t_tk], writes=[t_gw[b]])
                act.op(lambda e: e.activation(out=gw[b][:], in_=gw[b][:], func=AF.Exp), reads=[t_gw[b]], writes=[t_gw[b]])
                yield
                dve.op(lambda e: e.tensor_reduce(out=zs[:], in_=gw[b][:], axis=AX.X, op=ALU.add), reads=[t_gw[b]], writes=[t_tk])
                dve.op(lambda e: e.reciprocal(out=zs[:], in_=zs[:]), reads=[t_tk], writes=[t_tk])
                dve.op(lambda e: e.tensor_tensor(out=gw[b][:], in0=gw[b][:], in1=zs[:].unsqueeze(2).broadcast_to([128, 8, 16]), op=ALU.mult),
                       reads=[t_gw[b], t_tk], writes=[t_gw[b]])
                yield

            def tail(t):
                b3 = t % 3
                b = t % 2
                ts_ = slice(t * 128, (t + 1) * 128)
                yield from rmsnorm(h2[b], t_h2[b], gple, t_gple, xn3b, t_xn3b, ssT, rsT, t_ssT, t_rsT)
                yield from gap(1)
                for kc in range(8):
                    pe.op(lambda e: e.transpose(out=ps_tT[:, kc, :], in_=xn3b[:, kc * 128:(kc + 1) * 128], identity=ident[:]),
                          reads=[t_xn3b, t_id], writes=[t_pstT])
                yield from gap(2)
                act.op(lambda e: e.activation(out=xTT[:], in_=ps_tT[:], func=AF.Copy), reads=[t_pstT], writes=[t_xTT])
                yield from gap(2)
                for n in range(2):
                    for kc in range(8):
                        pe.op(lambda e: e.matmul(ps_g[n][:], lhsT=xTT[:, kc, :], rhs=wg_sb[:, kc, n * 512:(n + 1) * 512],
                                                 start=(kc == 0), stop=(kc == 7)), reads=[t_xTT, t_wg], writes=[t_psg[n]])
                    yield
                yield from gap(2)
                for n in range(2):
                    act.op(lambda e: e.activation(out=gate_sb[:, n * 512:(n + 1) * 512], in_=ps_g[n][:], func=AF.Sigmoid),
                           reads=[t_psg[n]], writes=[t_gate])
                act.op(lambda e: e.activation(out=ptb[:], in_=ptl[b3][:], func=AF.Copy), reads=[t_ptl[b3]], writes=[t_ptb])
                yield from gap(2)
                for kc in range(2):
                    pe.op(lambda e: e.transpose(out=ps_tT[:, kc, :], in_=ptb[:, kc * 128:(kc + 1) * 128], identity=ident[:]),
                          reads=[t_ptb, t_id], writes=[t_pstT])
                yield from gap(2)
                act.op(lambda e: e.activation(out=pTs[:], in_=ps_tT[:, 0:2, :], func=AF.Copy), reads=[t_pstT], writes=[t_pTs])
                yield from gap(2)
                for n in range(2):
                    ns = slice(n * 512, (n + 1) * 512)
                    for kc in range(2):
                        pe.op(lambda e: e.matmul(ps_g[n][:], lhsT=pTs[:, kc, :], rhs=wp_sb[:, kc, ns],
                                                 start=(kc == 0), stop=(kc == 1)), reads=[t_pTs, t_wp], writes=[t_psg[n]])
                yield from gap(2)
                for n in range(2):
                    ns = slice(n * 512, (n + 1) * 512)
                    dve.op(lambda e: e.tensor_tensor(out=h3[:, ns], in0=ps_g[n][:], in1=gate_sb[:, ns], op=ALU.mult),
                           reads=[t_psg[n], t_gate], writes=[t_h3])
                    yield
                dve.op(lambda e: e.tensor_tensor(out=h3[:], in0=h3[:], in1=h2[b][:], op=ALU.add), reads=[t_h3, t_h2[b]], writes=[t_h3])
                yield
                yield from rmsnorm(h3, t_h3, gfin, t_gfin, outt[b], t_outt[b], ssT, rsT, t_ssT, t_rsT)
                sp.dma([(out_d[ts_, :], outt[b][:])], reads=[t_outt[b]])
                yield

            gi_state = {"gi": 0}

            TAIL_START = 0

            class _Late:
                _late = True

                def __init__(self, g):
                    self.g = g

                def __iter__(self):
                    return self

                def __next__(self):
                    return next(self.g)

            mode = {"heavy": False}

            def slots(t, gens):
                b = t % 2
                b3 = t % 3
                gwf = gw[b][:].rearrange("p h k -> p (h k)")
                lightq = []

                for sl in range(128):
                    gb = gi_state["gi"] % NBUF
                    gi_state["gi"] += 1
                    dg = sl % NDG
                    pool.dma([(gbufs[gb][:], puv_bf)], reads=[t_idx[b], t_tab], writes=[t_gb[gb]],
                             fn=lambda e, o, i: e.indirect_dma_start(out=o, out_offset=None, in_=i,
                                                                      in_offset=bass.IndirectOffsetOnAxis(ap=idxi[b][:, sl:sl + 1], axis=0)))
                    dve.op(lambda e: e.scalar_tensor_tensor(out=junkb[:], in0=gbufs[gb][:, 0:D], scalar=1.0, in1=xn2b[b][:], op0=ALU.mult, op1=ALU.mult,
                                                            accum_out=logits[:, sl:sl + 1]),
                           reads=[t_gb[gb], t_xn2b[b]], writes=[t_lg[sl], t_junkb])
                    act.op(lambda e: e.activation(out=gl4[:, sl:sl + 1], in_=logits[:, sl:sl + 1], func=AF.Gelu),
                           reads=[t_lg[sl]], writes=[t_av[sl]])

                    def finish_act(s_, gb_):
                        dg_ = s_ % NDG
                        act.op(lambda e: e.activation(out=actv[:, s_:s_ + 1], in_=gl4[:, s_:s_ + 1], func=AF.Copy, scale=gwf[:, s_:s_ + 1]),
                               reads=[t_av[s_], t_gw[b]], writes=[t_av[s_]])
                        act.op(lambda e: e.activation(out=diag[dg_][:], in_=ident[:], func=AF.Copy, scale=actv[:, s_:s_ + 1]),
                               reads=[t_id, t_av[s_]], writes=[t_diag[dg_]])
                        mm(s_, gb_, dg_)

                    def finish_dve(s_, gb_):
                        dg_ = s_ % NDG
                        dve.op(lambda e: e.tensor_scalar(out=diag[dg_][:], in0=ident[:], scalar1=gl4[:, s_:s_ + 1], scalar2=gwf[:, s_:s_ + 1],
                                                         op0=ALU.mult, op1=ALU.mult),
                               reads=[t_id, t_av[s_], t_gw[b]], writes=[t_diag[dg_]])
                        mm(s_, gb_, dg_)

                    def mm(s_, gb_, dg_):
                        for n in range(2):
                            pe.op(lambda e: e.matmul(ps_v[n][:], lhsT=diag[dg_][:], rhs=gbufs[gb_][:, D + n * 512:D + (n + 1) * 512],
                                                     start=(s_ == 0), stop=False),
                                  reads=[t_diag[dg_], t_gb[gb_]], writes=[t_psv[n]])

                    if mode["heavy"]:
                        for p_ in lightq:
                            finish_dve(*p_)
                        del lightq[:]
                        finish_act(sl, gb)
                    else:
                        for p_ in lightq:
                            finish_dve(*p_)
                        del lightq[:]
                        lightq.append((sl, gb))
                    for gen in gens:
                        if getattr(gen, "_late", False) and sl < TAIL_START:
                            continue
                        next(gen, None)
                for p_ in lightq:
                    finish_dve(*p_)
                del lightq[:]
                for gen in gens:
                    for _ in gen:
                        pass
                for n in range(2):
                    ns = slice(n * 512, (n + 1) * 512)
                    dve.op(lambda e: e.tensor_tensor(out=h2[b][:, ns], in0=ps_v[n][:], in1=h1t[b3][:, ns], op=ALU.add),
                           reads=[t_psv[n], t_h1t[b3]], writes=[t_h2[b]])

            for _ in front(0):
                pass
            for t in range(NT):
                gens = []
                if t + 1 < NT:
                    gens.append(front(t + 1))
                if t >= 1:
                    tg = _Late(tail(t - 1))
                    gens.append(tg)
                slots(t, gens)
            for _ in tail(NT - 1):
                pass
        K.barrier()
        print("SBUF peak bytes/partition", K.sb_peak - SB_LO, "of", SB_HI - SB_LO)
    return nc


def _consts():
    c = np.zeros((128, 152), np.float32)
    c[:, 0:128] = np.eye(128, dtype=np.float32)
    c[:, 128:144] = np.arange(16, dtype=np.float32)[None, :]
    inv = (np.float32(500000.0) ** (-np.arange(0, 16, 2, dtype=np.float32) / np.float32(16))).astype(np.float32)
    c[:, 144:152] = inv[None, :]
    return c


def make_in_maps(inputs, S):
    f = lambda a: np.ascontiguousarray(np.asarray(a))
    x = f(inputs["x"])
    B = x.shape[0]
    NT = S // 128
    conv_w = f(inputs["conv_w"])[0]
    cols = []
    for c in range(4):
        sl = slice(c * 128, (c + 1) * 128)
        ks = [conv_w[0, sl], conv_w[1, sl], conv_w[2, sl], conv_w[3, sl], f(inputs["conv_b"])[0, sl],
              f(inputs["lru_ba"])[0, 0, sl], f(inputs["lru_ba"])[0, 1, sl],
              f(inputs["lru_bx"])[0, 0, sl], f(inputs["lru_bx"])[0, 1, sl],
              f(inputs["lru_lambda"])[0, 0, sl], f(inputs["lru_lambda"])[0, 1, sl],
              f(inputs["lru_norm_g"])[0, sl]]
        cols.extend(ks)
    colp = np.ascontiguousarray(np.stack(cols, axis=1).astype(np.float32))
    lam4 = np.ascontiguousarray(np.concatenate([f(inputs["lambda_q1"])[0], f(inputs["lambda_k1"])[0],
                                                f(inputs["lambda_q2"])[0], f(inputs["lambda_k2"])[0]])[None, :])
    shared = {
        "consts": _consts(),
        "w_in": f(inputs["w_in"])[0], "w_out": f(inputs["w_out"])[0], "peer_wq": f(inputs["peer_wq"])[0],
        "ple_w_gate": f(inputs["ple_w_gate"])[0], "ple_w_proj": f(inputs["ple_w_proj"])[0],
        "norm_mix_g": f(inputs["norm_mix_g"]), "norm_ffn_g": f(inputs["norm_ffn_g"]),
        "norm_ple_g": f(inputs["norm_ple_g"]), "final_norm_g": f(inputs["final_norm_g"])[None, :],
        "diff_norm_g": f(inputs["diff_norm_g"]), "lam4": lam4, "colp": colp,
        "lru_wa": f(inputs["lru_wa"])[0], "lru_wx": f(inputs["lru_wx"])[0],
        "keys1T": np.ascontiguousarray(f(inputs["peer_keys1"])[0].T), "keys2T": np.ascontiguousarray(f(inputs["peer_keys2"])[0].T),
        "peer_u": f(inputs["peer_u"])[0], "peer_v": f(inputs["peer_v"])[0],
    }
    maps = []
    for bi in range(B):
        m = dict(shared)
        m["x"] = np.ascontiguousarray(x[bi])
        m["p"] = np.ascontiguousarray(f(inputs["p"])[0, bi])
        m["pos"] = np.ascontiguousarray(f(inputs["positions"])[bi].astype(np.int32).reshape(NT, 128).T)
        maps.append(m)
    return maps


_NC_CACHE = {}


def kernel(**inputs):
    x = np.asarray(inputs["x"])
    B, S, _ = x.shape
    if S not in _NC_CACHE:
        _NC_CACHE[S] = build(S)
    nc = _NC_CACHE[S]
    maps = make_in_maps(inputs, S)
    res = run_bass_kernel_spmd(nc, maps, core_ids=list(range(B)))
    out = np.stack([np.asarray(r["out"]) for r in res.results], axis=0).astype(np.float32)
    return out
```

```python
import math
import contextlib
import numpy as np
import concourse.bass as bass
import concourse.mybir as mybir
from concourse.bass_utils import run_bass_kernel_spmd

F32 = mybir.dt.float32
BF16 = mybir.dt.bfloat16
I32 = mybir.dt.int32
U32 = mybir.dt.uint32
AF = mybir.ActivationFunctionType
ALU = mybir.AluOpType
AX = mybir.AxisListType

SB_LO = 16512
SB_HI = 229344
EPS = 1e-6
D = 1024
NEXP = 16384
PI = math.pi


class Tok:
    __slots__ = ("w", "r")

    def __init__(self):
        self.w = None
        self.r = {}


class Src:
    def __init__(self, sem, name):
        self.sem = sem
        self.name = name
        self.count = 0


class Eng(Src):
    def __init__(self, K, h, sem, name):
        super().__init__(sem, name)
        self.K = K
        self.h = h
        self.seen = {}

    def wait(self, ev):
        if ev is None:
            return
        src, val = ev
        if self.seen.get(src, 0) >= val:
            return
        self.h.wait_ge(src.sem, val)
        self.seen[src] = val

    def _deps(self, reads, writes, extra):
        for t in reads:
            if t.w is not None and t.w[0] is self and self is self.K.pe:
                continue
            self.wait(t.w)
        for t in writes:
            if t.w is not None and t.w[0] is not self:
                self.wait(t.w)
            for s, v in t.r.items():
                if s is not self:
                    self.wait((s, v))
        for e in extra:
            self.wait(e)

    def _mark(self, ev, reads, writes):
        for t in reads:
            if t.r.get(ev[0], 0) < ev[1]:
                t.r[ev[0]] = ev[1]
        for t in writes:
            t.w = ev
            t.r = {}

    def op(self, fn, reads=(), writes=(), extra=()):
        self._deps(reads, writes, extra)
        ins = fn(self.h)
        self.count += 1
        ins.then_inc(self.sem, 1)
        ev = (self, self.count)
        self._mark(ev, reads, writes)
        return ev

    def dma(self, pairs, reads=(), writes=(), extra=(), fn=None):
        K = self.K
        ds = K.next_dsem()
        if ds.count:
            self.wait((ds, ds.count))
        self._deps(reads, writes, extra)
        for (o, i) in pairs:
            if fn is not None:
                ins = fn(self.h, o, i)
            else:
                ins = self.h.dma_start(out=o, in_=i)
            ds.count += 16
            ins.then_inc(ds.sem, 16)
        ev = (ds, ds.count)
        self._mark(ev, reads, writes)
        return ev


class Kern:
    def __init__(self, nc, es, n_dsem=56):
        self.nc = nc
        self.es = es
        mk = lambda n: es.enter_context(nc.semaphore(n))
        self.pe = Eng(self, nc.tensor, mk("s_pe"), "pe")
        self.act = Eng(self, nc.scalar, mk("s_act"), "act")
        self.dve = Eng(self, nc.vector, mk("s_dve"), "dve")
        self.pool = Eng(self, nc.gpsimd, mk("s_pool"), "pool")
        self.sp = Eng(self, nc.sync, mk("s_sp"), "sp")
        self.engs = [self.pe, self.act, self.dve, self.pool, self.sp]
        self.dsems = [Src(mk(f"s_d{i}"), f"d{i}") for i in range(n_dsem)]
        self.di = 0
        self.sb_off = SB_LO
        self.sb_peak = SB_LO
        self.n_alloc = 0
        self.pes = None

    def next_dsem(self):
        d = self.dsems[self.di]
        self.di = (self.di + 1) % len(self.dsems)
        return d

    def sb(self, shape, dt, name=None, at=None):
        esz = {F32: 4, BF16: 2, I32: 4, U32: 4}[dt]
        n = 1
        for s in shape[1:]:
            n *= s
        nbytes = (n * esz + 63) // 64 * 64
        if at is not None:
            self.n_alloc += 1
            return self.nc.alloc_sbuf_tensor_at(f"{name or 't'}_{self.n_alloc}", list(shape), dt, offset=at)
        off = self.sb_off
        assert off + nbytes <= SB_HI, f"SBUF overflow {name} {off}+{nbytes}"
        self.sb_off += nbytes
        self.sb_peak = max(self.sb_peak, self.sb_off)
        self.n_alloc += 1
        return self.nc.alloc_sbuf_tensor_at(f"{name or 't'}_{self.n_alloc}", list(shape), dt, offset=off)

    def mark(self):
        return self.sb_off

    def release(self, m):
        self.sb_off = m

    def ps(self, shape, dt=F32, name=None):
        self.n_alloc += 1
        return self.pes.enter_context(self.nc.psum_tensor(f"{name or 'ps'}_{self.n_alloc}", list(shape), dt))

    def barrier(self):
        srcs = [(e, e.count) for e in self.engs if e.count] + [(d, d.count) for d in self.dsems if d.count]
        for e in self.engs:
            for ev in srcs:
                e.wait(ev)


def build(S, debug=False, stop_after=None):
    NT = S // 128
    NG = S // 512
    L = min(1024, S)
    NSEG = S // L
    nc = bass.Bass("TRN2", target_bir_lowering=False)

    def din(n, s, d=F32):
        return nc.dram_tensor(n, list(s), d, kind="ExternalInput").ap()

    x_d = din("x", [S, D])
    p_d = din("p", [S, 256])
    pos_d = din("pos", [128, NT], I32)
    cst_d = din("consts", [128, 152])
    w_in_d = din("w_in", [D, 2560])
    w_out_d = din("w_out", [D, D])
    wq_d = din("peer_wq", [D, D])
    wg_d = din("ple_w_gate", [D, D])
    wp_d = din("ple_w_proj", [256, D])
    gmix_d = din("norm_mix_g", [1, D])
    gffn_d = din("norm_ffn_g", [1, D])
    gple_d = din("norm_ple_g", [1, D])
    gfin_d = din("final_norm_g", [1, D])
    gdiff_d = din("diff_norm_g", [1, 128])
    lam_d = din("lam4", [1, 256])
    colp_d = din("colp", [128, 48])
    wa_d = din("lru_wa", [2, 8, 64, 64])
    wx_d = din("lru_wx", [2, 8, 64, 64])
    k1T_d = din("keys1T", [64, 128])
    k2T_d = din("keys2T", [64, 128])
    pu_d = din("peer_u", [NEXP, D])
    pv_d = din("peer_v", [NEXP, D])
    out_d = nc.dram_tensor("out", [S, D], F32, kind="ExternalOutput").ap()
    lru_scr = nc.dram_tensor("lru_scr", [1024, S], F32, kind="Internal").ap()
    h1_scr = nc.dram_tensor("h1_scr", [S, D], F32, kind="Internal").ap()
    puv_bf = nc.dram_tensor("puv_bf", [NEXP, 2 * D], BF16, kind="Internal").ap()

    with contextlib.ExitStack() as es:
        K = Kern(nc, es)
        pe, act, dve, pool, sp = K.pe, K.act, K.dve, K.pool, K.sp

        def cast_load(dst3, src2, nk, ncol, tok):
            pairs = []
            for kc in range(nk):
                for c0 in range(0, ncol, 1024):
                    c1 = min(ncol, c0 + 1024)
                    pairs.append((dst3[:, kc, c0:c1], src2[kc * 128:(kc + 1) * 128, c0:c1]))
            return pool.dma(pairs, writes=[tok])

        def bcast_load(dst, src_row, tok):
            return sp.dma([(dst, src_row.partition_broadcast(128))], writes=[tok])

        cst = K.sb([128, 152], F32, "cst")
        t_cst = Tok()
        sp.dma([(cst[:], cst_d)], writes=[t_cst])
        ident = K.sb([128, 128], BF16, "ident")
        t_id = Tok()
        dve.op(lambda e: e.tensor_copy(out=ident[:], in_=cst[:, 0:128]), reads=[t_cst], writes=[t_id])
        iota16 = cst[:, 128:144]
        invf = cst[:, 144:152]
        one_c = K.sb([128, 1], F32, "one")
        t_one = Tok()
        dve.op(lambda e: e.memset(one_c[:], 1.0), writes=[t_one])
        ones_bf = K.sb([128, 1], BF16, "onesbf")
        t_onesbf = Tok()
        dve.op(lambda e: e.memset(ones_bf[:], 1.0), writes=[t_onesbf])
        junk = K.sb([128, 1024], F32, "junk")
        t_junk = Tok()
        rstd_rec = K.sb([128, NT], F32, "rstdrec")
        t_rstd_rec = Tok()

        def rstd_from_ssq(ssq_ap, n, out_ap, tok_in, tok_out, shape_tmp):
            dve.op(lambda e: e.tensor_scalar(out=out_ap, in0=ssq_ap, scalar1=1.0 / n, scalar2=EPS,
                                             op0=ALU.mult, op1=ALU.add), reads=[tok_in], writes=[tok_out])
            act.op(lambda e: e.activation(out=out_ap, in_=out_ap, func=AF.Sqrt), reads=[tok_out], writes=[tok_out])
            dve.op(lambda e: e.reciprocal(out=out_ap, in_=out_ap), reads=[tok_out], writes=[tok_out])

        lamt = K.sb([128, 256], F32, "lamt")
        t_lam = Tok()
        bcast_load(lamt[:], lam_d[0], t_lam)
        lsum = K.sb([128, 2], F32, "lsum")
        neglam = K.sb([128, 1], F32, "neglam")
        t_nl = Tok()
        dve.op(lambda e: e.scalar_tensor_tensor(out=junk[:, 0:64], in0=lamt[:, 0:64], scalar=1.0, in1=lamt[:, 64:128],
                                                op0=ALU.mult, op1=ALU.mult, accum_out=lsum[:, 0:1]),
               reads=[t_lam], writes=[t_nl, t_junk])
        dve.op(lambda e: e.scalar_tensor_tensor(out=junk[:, 0:64], in0=lamt[:, 128:192], scalar=1.0, in1=lamt[:, 192:256],
                                                op0=ALU.mult, op1=ALU.mult, accum_out=lsum[:, 1:2]),
               reads=[t_lam], writes=[t_nl, t_junk])
        act.op(lambda e: e.activation(out=lsum[:], in_=lsum[:], func=AF.Exp), reads=[t_nl], writes=[t_nl])
        dve.op(lambda e: e.tensor_tensor(out=neglam[:], in0=lsum[:, 1:2], in1=lsum[:, 0:1], op=ALU.subtract),
               reads=[t_nl], writes=[t_nl])
        dve.op(lambda e: e.tensor_scalar(out=neglam[:], in0=neglam[:], scalar1=-0.2, scalar2=None, op0=ALU.add),
               reads=[t_nl], writes=[t_nl])

        posi = K.sb([128, NT], I32, "posi")
        t_pos = Tok()
        sp.dma([(posi[:], pos_d)], writes=[t_pos])
        posf = K.sb([128, NT], F32, "posf")
        ang = K.sb([128, NT, 8], F32, "ang")
        t_ang = Tok()
        dve.op(lambda e: e.tensor_copy(out=posf[:], in_=posi[:]), reads=[t_pos], writes=[t_ang])
        dve.op(lambda e: e.tensor_tensor(out=ang[:], in0=posf[:].unsqueeze(2).broadcast_to([128, NT, 8]),
                                         in1=invf.unsqueeze(1).broadcast_to([128, NT, 8]), op=ALU.mult),
               reads=[t_ang, t_cst], writes=[t_ang])
        cosk = K.sb([128, NT, 8], F32, "cosk")
        sink = K.sb([128, NT, 8], F32, "sink")
        cosq = K.sb([128, NT, 8], F32, "cosq")
        sinq = K.sb([128, NT, 8], F32, "sinq")
        t_trig = Tok()
        colp = K.sb([128, 48], F32, "colp")
        scl = K.sb([128, 4, 2], F32, "scl")
        scl2 = K.sb([128, 4, 2], F32, "scl2")
        Wbd = K.sb([128, 16, 128], BF16, "Wbd")
        keysbd = K.sb([128, 256], BF16, "keysbd")
        m_trig = K.mark()
        wst = K.sb([128, 16, 128], F32, "wst")
        kst = K.sb([128, 256], F32, "kst")
        tr_a = K.sb([128, NT, 8], F32)
        tr_n = K.sb([128, NT, 8], F32)
        tr_i = K.sb([128, NT, 8], I32)
        tr_m = K.sb([128, NT, 8], F32)
        t_tr = Tok()
        for dst, shift in ((sink, 0.0), (cosk, PI / 2)):
            dve.op(lambda e: e.tensor_scalar(out=tr_a[:], in0=ang[:], scalar1=shift, scalar2=None, op0=ALU.add),
                   reads=[t_ang], writes=[t_tr])
            dve.op(lambda e: e.tensor_scalar(out=tr_n[:], in0=tr_a[:], scalar1=1.0 / (2 * PI), scalar2=None, op0=ALU.mult),
                   reads=[t_tr], writes=[t_tr])
            dve.op(lambda e: e.tensor_copy(out=tr_i[:], in_=tr_n[:]), reads=[t_tr], writes=[t_tr])
            dve.op(lambda e: e.tensor_copy(out=tr_n[:], in_=tr_i[:]), reads=[t_tr], writes=[t_tr])
            dve.op(lambda e: e.scalar_tensor_tensor(out=tr_a[:], in0=tr_n[:], scalar=-2 * PI, in1=tr_a[:],
                                                    op0=ALU.mult, op1=ALU.add), reads=[t_tr], writes=[t_tr])
            dve.op(lambda e: e.tensor_single_scalar(out=tr_m[:], in_=tr_a[:], scalar=PI, op=ALU.is_gt),
                   reads=[t_tr], writes=[t_tr])
            dve.op(lambda e: e.scalar_tensor_tensor(out=tr_a[:], in0=tr_m[:], scalar=-2 * PI, in1=tr_a[:],
                                                    op0=ALU.mult, op1=ALU.add), reads=[t_tr], writes=[t_tr])
            dve.op(lambda e: e.tensor_single_scalar(out=tr_m[:], in_=tr_a[:], scalar=-PI, op=ALU.is_lt),
                   reads=[t_tr], writes=[t_tr])
            dve.op(lambda e: e.scalar_tensor_tensor(out=tr_a[:], in0=tr_m[:], scalar=2 * PI, in1=tr_a[:],
                                                    op0=ALU.mult, op1=ALU.add), reads=[t_tr], writes=[t_tr])
            dve.op(lambda e: e.tensor_scalar(out=tr_a[:], in0=tr_a[:], scalar1=3.1415925, scalar2=-3.1415925,
                                             op0=ALU.min, op1=ALU.max), reads=[t_tr], writes=[t_tr])
            act.op(lambda e: e.activation(out=dst[:], in_=tr_a[:], func=AF.Sin), reads=[t_tr], writes=[t_trig])
        dve.op(lambda e: e.tensor_scalar(out=cosq[:], in0=cosk[:], scalar1=0.125, scalar2=None, op0=ALU.mult),
               reads=[t_trig], writes=[t_trig])
        dve.op(lambda e: e.tensor_scalar(out=sinq[:], in0=sink[:], scalar1=0.125, scalar2=None, op0=ALU.mult),
               reads=[t_trig], writes=[t_trig])
        t_colp = Tok()
        sp.dma([(colp[:], colp_d)], writes=[t_colp])
        colp3 = colp[:].rearrange("p (c k) -> p c k", k=12)
        t_scl = Tok()
        act.op(lambda e: e.activation(out=scl[:], in_=colp3[:, :, 9:11], func=AF.Exp, scale=-1.0), reads=[t_colp], writes=[t_scl])
        dve.op(lambda e: e.tensor_scalar(out=scl[:], in0=scl[:], scalar1=1.0, scalar2=None, op0=ALU.add), reads=[t_scl], writes=[t_scl])
        act.op(lambda e: e.activation(out=scl[:], in_=scl[:], func=AF.Ln), reads=[t_scl], writes=[t_scl])
        dve.op(lambda e: e.tensor_scalar(out=scl2[:], in0=scl[:], scalar1=-16.0, scalar2=None, op0=ALU.mult), reads=[t_scl], writes=[t_scl])
        dve.op(lambda e: e.tensor_scalar(out=scl[:], in0=scl[:], scalar1=-8.0, scalar2=None, op0=ALU.mult), reads=[t_scl], writes=[t_scl])
        t_wst, t_Wbd = Tok(), Tok()
        dve.op(lambda e: e.memset(wst[:], 0.0), writes=[t_wst])
        pairs = []
        for typ, wd in ((0, wa_d), (1, wx_d)):
            for d in range(2):
                base = (typ * 2 + d) * 4
                for ehalf in range(2):
                    src = wd[d, ehalf::2].rearrange("n i o -> i n o")
                    pairs.append((wst[ehalf * 64:(ehalf + 1) * 64, base:base + 4, ehalf * 64:(ehalf + 1) * 64], src))
        sp.dma(pairs, writes=[t_wst])
        dve.op(lambda e: e.tensor_copy(out=Wbd[:], in_=wst[:]), reads=[t_wst], writes=[t_Wbd])

        t_kst, t_keys = Tok(), Tok()
        dve.op(lambda e: e.memset(kst[:], 0.0), writes=[t_kst])
        sp.dma([(kst[0:64, 0:128], k1T_d), (kst[64:128, 128:256], k2T_d)], writes=[t_kst])
        dve.op(lambda e: e.tensor_copy(out=keysbd[:], in_=kst[:]), reads=[t_kst], writes=[t_keys])

        K.barrier()
        K.release(m_trig)
        if stop_after == "setup":
            return nc

        attnT = K.sb([128, 4, S], BF16, "attnT", at=SB_HI - 8 * S)
        yT = K.sb([128, 4, S], BF16, "yT", at=SB_HI - 16 * S)
        t_attnT = Tok()
        t_yT = Tok()

        t_xr = Tok()

        def rope(ps3, dst3, ct, st, t, t_ps, t_dst, tmp):
            C = ct[:, t, :].unsqueeze(1).broadcast_to([128, 8, 8])
            Sn = st[:, t, :].unsqueeze(1).broadcast_to([128, 8, 8])
            t1, t2, t3, t4, xr = tmp
            tt = Tok()
            act.op(lambda e: e.activation(out=xr[:], in_=ps3[:, :, 0:16], func=AF.Copy), reads=[t_ps], writes=[t_xr])
            t_ps = t_xr
            x1 = xr[:, :, 0:8]
            x2 = xr[:, :, 8:16]
            dve.op(lambda e: e.tensor_tensor(out=t1[:], in0=x1, in1=C, op=ALU.mult), reads=[t_ps, t_trig], writes=[tt])
            dve.op(lambda e: e.tensor_tensor(out=t2[:], in0=x2, in1=Sn, op=ALU.mult), reads=[t_ps, t_trig], writes=[tt])
            dve.op(lambda e: e.tensor_tensor(out=t3[:], in0=x2, in1=C, op=ALU.mult), reads=[t_ps, t_trig], writes=[tt])
            dve.op(lambda e: e.tensor_tensor(out=t4[:], in0=x1, in1=Sn, op=ALU.mult), reads=[t_ps, t_trig], writes=[tt])
            dve.op(lambda e: e.tensor_tensor(out=dst3[:, :, 0:8], in0=t1[:], in1=t2[:], op=ALU.subtract),
                   reads=[tt], writes=[t_dst])
            dve.op(lambda e: e.tensor_tensor(out=dst3[:, :, 8:16], in0=t3[:], in1=t4[:], op=ALU.add),
                   reads=[tt], writes=[t_dst])

        mA = K.mark()
        QT = K.sb([128, 4, S], BF16, "QT")
        KT = K.sb([128, 4, S], BF16, "KT")
        Vs = K.sb([128, NT, 4, 129], BF16, "V")
        t_QT, t_KT, t_V = Tok(), Tok(), Tok()
        dve.op(lambda e: e.memset(Vs[:, :, :, 128:129], 1.0), writes=[t_V])
        mA2 = K.mark()
        w_in_sb = K.sb([128, 8, 2560], BF16, "w_in")
        t_win = Tok()
        t_win_l = Tok()
        pool.dma([(w_in_sb[:, kc, c0:c0 + 768], w_in_d[kc * 128:(kc + 1) * 128, c0:c0 + 768])
                  for kc in range(8) for c0 in (0, 768)], writes=[t_win])
        pool.dma([(w_in_sb[:, kc, 1536:2560], w_in_d[kc * 128:(kc + 1) * 128, 1536:2560]) for kc in range(8)], writes=[t_win_l])
        gmix = K.sb([128, D], F32, "gmix")
        t_gmix = Tok()
        bcast_load(gmix[:], gmix_d[0], t_gmix)
        xts = [K.sb([128, D], F32, f"xt{i}") for i in range(4)]
        t_xt = [Tok() for _ in range(4)]
        xnb = [K.sb([128, D], BF16, f"xnb{i}") for i in range(2)]
        t_xnb = [Tok() for _ in range(2)]
        xnT = [K.sb([128, 8, 512], BF16, f"xnT{i}") for i in range(2)]
        t_xnT = [Tok() for _ in range(2)]
        ssqA = K.sb([128, 4], F32, "ssqA")
        rstdA = K.sb([128, 4], F32, "rstdA")
        t_ssqA, t_rstdA = Tok(), Tok()
        q_sb = K.sb([128, 8, 64], BF16, "q_sb")
        k_sb = K.sb([128, 8, 64], BF16, "k_sb")
        t_qsb, t_ksb = Tok(), Tok()
        rtmp = [K.sb([128, 8, 8], F32) for _ in range(4)] + [K.sb([128, 8, 16], F32)]
        stage = [K.sb([128, 512], F32, f"stg{i}") for i in range(3)]
        t_stage = [Tok() for _ in range(3)]
        if stop_after == "A0":
            K.barrier()
            return nc
        with contextlib.ExitStack() as pes:
            K.pes = pes
            ps_tr = [K.ps([128, 8, 128], BF16) for _ in range(2)]
            t_ps_tr = [Tok(), Tok()]
            ps_qkv = [K.ps([128, 512], F32) for _ in range(3)]
            t_ps_qkv = [Tok() for _ in range(3)]
            ps_qk = K.ps([128, 8, 128], BF16)
            t_ps_qk = Tok()
            ps_l = [K.ps([128, 512], F32) for _ in range(2)]
            t_ps_l = [Tok(), Tok()]
            itr = 0
            il = 0
            for g in range(NG):
                b = g % 2
                for j in range(4):
                    t = 4 * g + j
                    if g == 0:
                        sp.dma([(xts[j][:], x_d[t * 128:(t + 1) * 128, :])], writes=[t_xt[j]])
                    dve.op(lambda e: e.scalar_tensor_tensor(out=junk[:], in0=xts[j][:], scalar=1.0, in1=xts[j][:],
                                                            op0=ALU.mult, op1=ALU.mult, accum_out=ssqA[:, j:j + 1]),
                           reads=[t_xt[j]], writes=[t_ssqA, t_junk])
                rstd_from_ssq(ssqA[:], D, rstdA[:], t_ssqA, t_rstdA, None)
                for j in range(4):
                    xb = j % 2
                    dve.op(lambda e: e.scalar_tensor_tensor(out=xnb[xb][:], in0=xts[j][:], scalar=rstdA[:, j:j + 1],
                                                            in1=gmix[:], op0=ALU.mult, op1=ALU.mult),
                           reads=[t_xt[j], t_rstdA, t_gmix], writes=[t_xnb[xb]])
                    pt = itr % 2
                    itr += 1
                    for kc in range(8):
                        pe.op(lambda e: e.transpose(out=ps_tr[pt][:, kc, :], in_=xnb[xb][:, kc * 128:(kc + 1) * 128],
                                                    identity=ident[:]),
                              reads=[t_xnb[xb], t_id], writes=[t_ps_tr[pt]])
                    act.op(lambda e: e.activation(out=xnT[b][:, :, j * 128:(j + 1) * 128], in_=ps_tr[pt][:], func=AF.Copy),
                           reads=[t_ps_tr[pt]], writes=[t_xnT[b]])
                if stop_after == "A1":
                    K.barrier()
                    return nc
                for j in range(4):
                    t = 4 * g + j
                    for cg in range(3):
                        for kc in range(8):
                            pe.op(lambda e: e.matmul(ps_qkv[cg][:], lhsT=xnT[b][:, kc, j * 128:(j + 1) * 128],
                                                     rhs=w_in_sb[:, kc, cg * 512:(cg + 1) * 512],
                                                     start=(kc == 0), stop=(kc == 7)),
                                  reads=[t_xnT[b], t_win], writes=[t_ps_qkv[cg]])
                    psq3 = ps_qkv[0][:].rearrange("p (g d) -> p g d", d=64)
                    psk3 = ps_qkv[1][:].rearrange("p (g d) -> p g d", d=64)
                    act.op(lambda e: e.activation(out=q_sb[:, :, 16:64], in_=psq3[:, :, 16:64], func=AF.Copy, scale=0.125),
                           reads=[t_ps_qkv[0]], writes=[t_qsb])
                    if stop_after == "A2a":
                        K.barrier()
                        return nc
                    rope(psq3, q_sb, cosq, sinq, t, t_ps_qkv[0], t_qsb, rtmp)
                    if stop_after == "A2b":
                        K.barrier()
                        return nc
                    act.op(lambda e: e.activation(out=k_sb[:, :, 16:64], in_=psk3[:, :, 16:64], func=AF.Copy),
                           reads=[t_ps_qkv[1]], writes=[t_ksb])
                    rope(psk3, k_sb, cosk, sink, t, t_ps_qkv[1], t_ksb, rtmp)
                    act.op(lambda e: e.activation(out=Vs[:, t, :, 0:128],
                                                  in_=ps_qkv[2][:].rearrange("p (h d) -> p h d", d=128), func=AF.Copy),
                           reads=[t_ps_qkv[2]], writes=[t_V])
                    if stop_after == "A2c":
                        K.barrier()
                        return nc
                    qf = q_sb[:].rearrange("p g d -> p (g d)")
                    kf = k_sb[:].rearrange("p g d -> p (g d)")
                    for h in range(4):
                        pe.op(lambda e: e.transpose(out=ps_qk[:, h, :], in_=qf[:, h * 128:(h + 1) * 128], identity=ident[:]),
                              reads=[t_qsb, t_id], writes=[t_ps_qk])
                    for h in range(4):
                        pe.op(lambda e: e.transpose(out=ps_qk[:, 4 + h, :], in_=kf[:, h * 128:(h + 1) * 128], identity=ident[:]),
                              reads=[t_ksb, t_id], writes=[t_ps_qk])
                    if stop_after == "A2d":
                        K.barrier()
                        return nc
                    act.op(lambda e: e.activation(out=QT[:, :, t * 128:(t + 1) * 128], in_=ps_qk[:, 0:4, :], func=AF.Copy),
                           reads=[t_ps_qk], writes=[t_QT])
                    act.op(lambda e: e.activation(out=KT[:, :, t * 128:(t + 1) * 128], in_=ps_qk[:, 4:8, :], func=AF.Copy),
                           reads=[t_ps_qk], writes=[t_KT])
                if stop_after == "A2":
                    K.barrier()
                    return nc
                if g + 1 < NG:
                    for j in range(4):
                        tn = 4 * (g + 1) + j
                        sp.dma([(xts[j][:], x_d[tn * 128:(tn + 1) * 128, :])], writes=[t_xt[j]])
                for cc in range(8):
                    pl = il % 2
                    sg = il % 3
                    il += 1
                    for kc in range(8):
                        pe.op(lambda e: e.matmul(ps_l[pl][:], lhsT=w_in_sb[:, kc, 1536 + cc * 128:1536 + (cc + 1) * 128],
                                                 rhs=xnT[b][:, kc, :], start=(kc == 0), stop=(kc == 7)),
                              reads=[t_xnT[b], t_win_l], writes=[t_ps_l[pl]])
                    act.op(lambda e: e.activation(out=stage[sg][:], in_=ps_l[pl][:], func=AF.Copy),
                           reads=[t_ps_l[pl]], writes=[t_stage[sg]])
                    sp.dma([(lru_scr[cc * 128:(cc + 1) * 128, g * 512:(g + 1) * 512], stage[sg][:])], reads=[t_stage[sg]])
        K.barrier()
        K.release(mA2)
        if stop_after == "A":
            return nc

        gd = K.sb([128, 128], F32, "gdiff")
        t_gd = Tok()
        bcast_load(gd[:], gdiff_d[0], t_gd)
        dve.op(lambda e: e.tensor_scalar(out=gd[:], in0=gd[:], scalar1=0.8, scalar2=None, op0=ALU.mult),
               reads=[t_gd], writes=[t_gd])
        Qz = [[K.sb([128, 512], BF16) for _ in range(2)] for _ in range(2)]
        t_Qz = [[Tok(), Tok()], [Tok(), Tok()]]
        for sl_ in range(2):
            pool.op(lambda e: e.memset(Qz[0][sl_][:], 0.0), writes=[t_Qz[0][sl_]])
            pool.op(lambda e: e.memset(Qz[1][sl_][:], 0.0), writes=[t_Qz[1][sl_]])
        iq = 0
        pT = [[K.sb([128, 512], BF16) for _ in range(2)] for _ in range(2)]
        t_pT = [[Tok(), Tok()], [Tok(), Tok()]]
        o_sb = [K.sb([128, 128], F32) for _ in range(4)]
        t_osb = [Tok() for _ in range(4)]
        otmp = K.sb([128, 128], F32)
        rr = K.sb([128, 8], F32)
        ssB = K.sb([128, 4], F32)
        rsB = K.sb([128, 4], F32)
        t_rr, t_ssB, t_rsB, t_otmp = Tok(), Tok(), Tok(), Tok()
        on_bf = K.sb([128, 4, 128], BF16)
        t_on = Tok()
        cstg = [K.sb([128, 4, D], BF16) for _ in range(2)]
        t_cstg = [Tok(), Tok()]
        cstf = [K.sb([128, 4, D], F32) for _ in range(2)]
        t_cstf = [Tok(), Tok()]
        t_tab = Tok()
        conv_state = {"i": 0}
        NCONV = 2 * (NEXP // 512)

        def conv_parts(i):
            src_t, hcol = (pu_d, 0) if i < NCONV // 2 else (pv_d, 1)
            ci = i % (NCONV // 2)
            rows = slice(ci * 512, (ci + 1) * 512)
            return src_t, hcol, rows

        def conv_load(i):
            src_t, hcol, rows = conv_parts(i)
            sp.dma([(cstf[i % 2][:], src_t[rows, :].rearrange("(p r) d -> p r d", r=4))], writes=[t_cstf[i % 2]])

        def emit_conv(n):
            for _ in range(n):
                i = conv_state["i"]
                if i >= NCONV:
                    return
                conv_state["i"] = i + 1
                if i == 0:
                    conv_load(0)
                if i + 1 < NCONV:
                    conv_load(i + 1)
                src_t, hcol, rows = conv_parts(i)
                sg_ = i % 2
                dve.op(lambda e: e.tensor_copy(out=cstg[sg_][:], in_=cstf[sg_][:]), reads=[t_cstf[sg_]], writes=[t_cstg[sg_]])
                pool.dma([(puv_bf[rows, hcol * D:(hcol + 1) * D].rearrange("(p r) d -> p r d", r=4), cstg[sg_][:])],
                         reads=[t_cstg[sg_]], writes=[t_tab])

        n_iter_B = 4 * NG * NT
        conv_per_iter = -(-NCONV // n_iter_B)
        conv_stride = max(1, n_iter_B // NCONV)
        it_B = {"n": 0}
        with contextlib.ExitStack() as pes:
            K.pes = pes
            ps_s = [[K.ps([128, 512], F32) for _ in range(2)] for _ in range(2)]
            t_ps_s = [[Tok(), Tok()], [Tok(), Tok()]]
            ps_acc = K.ps([128, 3, 512], F32)
            t_acc = Tok()
            ps_oT = K.ps([128, 4, 128], BF16)
            t_oT = Tok()

            def acc(m, j):
                a = m * 4 + j
                return ps_acc[:, a // 3, (a % 3) * 129:(a % 3) * 129 + 129]

            for h in range(4):
                for qg in range(NG):
                    qs = slice(qg * 512, (qg + 1) * 512)
                    zq = iq % 2
                    iq += 1
                    pool.op(lambda e: e.tensor_copy(out=Qz[0][zq][0:64, :], in_=QT[0:64, h, qs]), reads=[t_QT], writes=[t_Qz[0][zq]])
                    pool.op(lambda e: e.tensor_copy(out=Qz[1][zq][64:128, :], in_=QT[64:128, h, qs]), reads=[t_QT], writes=[t_Qz[1][zq]])

                    def emit_s(kt):
                        sl = kt % 2
                        for m in range(2):
                            pe.op(lambda e: e.matmul(ps_s[m][sl][:], lhsT=KT[:, h, kt * 128:(kt + 1) * 128],
                                                     rhs=Qz[m][zq][:], start=True, stop=True),
                                  reads=[t_KT, t_Qz[m][zq]], writes=[t_ps_s[m][sl]])

                    emit_s(0)
                    for kt in range(NT):
                        sl = kt % 2
                        if it_B["n"] % conv_stride == 0:
                            emit_conv(conv_per_iter)
                        it_B["n"] += 1
                        if kt + 1 < NT:
                            emit_s(kt + 1)
                        for m in range(2):
                            act.op(lambda e: e.activation(out=pT[m][sl][:], in_=ps_s[m][sl][:], func=AF.Exp),
                                   reads=[t_ps_s[m][sl]], writes=[t_pT[m][sl]])
                        for m in range(2):
                            for j in range(4):
                                a = m * 4 + j
                                pe.op(lambda e: e.matmul(acc(m, j), lhsT=pT[m][sl][:, j * 128:(j + 1) * 128],
                                                         rhs=Vs[:, kt, h, :], start=(kt == 0 and a % 3 == 0),
                                                         stop=(kt == NT - 1)),
                                      reads=[t_pT[m][sl], t_V], writes=[t_acc])
                    for j in range(4):
                        dve.op(lambda e: e.reciprocal(out=rr[:, j:j + 1], in_=acc(0, j)[:, 128:129]),
                               reads=[t_acc], writes=[t_rr])
                        dve.op(lambda e: e.reciprocal(out=rr[:, 4 + j:5 + j], in_=acc(1, j)[:, 128:129]),
                               reads=[t_acc], writes=[t_rr])
                    dve.op(lambda e: e.tensor_tensor(out=rr[:, 4:8], in0=rr[:, 4:8], in1=neglam[:, 0:1].broadcast_to([128, 4]),
                                                     op=ALU.mult), reads=[t_rr, t_nl], writes=[t_rr])
                    for j in range(4):
                        dve.op(lambda e: e.tensor_scalar(out=otmp[:], in0=acc(1, j)[:, 0:128], scalar1=rr[:, 4 + j:5 + j],
                                                         scalar2=None, op0=ALU.mult),
                               reads=[t_acc, t_rr], writes=[t_otmp])
                        dve.op(lambda e: e.scalar_tensor_tensor(out=o_sb[j][:], in0=acc(0, j)[:, 0:128], scalar=rr[:, j:j + 1],
                                                                in1=otmp[:], op0=ALU.mult, op1=ALU.add),
                               reads=[t_acc, t_rr, t_otmp], writes=[t_osb[j]])
                        dve.op(lambda e: e.scalar_tensor_tensor(out=junk[:, 0:128], in0=o_sb[j][:], scalar=1.0, in1=o_sb[j][:],
                                                                op0=ALU.mult, op1=ALU.mult, accum_out=ssB[:, j:j + 1]),
                               reads=[t_osb[j]], writes=[t_ssB, t_junk])
                    rstd_from_ssq(ssB[:], 128, rsB[:], t_ssB, t_rsB, None)
                    for j in range(4):
                        dve.op(lambda e: e.scalar_tensor_tensor(out=on_bf[:, j, :], in0=o_sb[j][:], scalar=rsB[:, j:j + 1],
                                                                in1=gd[:], op0=ALU.mult, op1=ALU.mult),
                               reads=[t_osb[j], t_rsB, t_gd], writes=[t_on])
                    for j in range(4):
                        pe.op(lambda e: e.transpose(out=ps_oT[:, j, :], in_=on_bf[:, j, :], identity=ident[:]),
                              reads=[t_on, t_id], writes=[t_oT])
                    act.op(lambda e: e.activation(out=attnT[:, h, qs].rearrange("p (j q) -> p j q", q=128), in_=ps_oT[:],
                                                  func=AF.Copy), reads=[t_oT], writes=[t_attnT])
        emit_conv(NCONV)
        K.barrier()
        K.release(mA)
        if stop_after == "B":
            return nc

        mC = K.mark()
        w_out_sb = K.sb([128, 8, D], BF16, "w_out")
        t_wout = Tok()
        cast_load(w_out_sb, w_out_d, 8, D, t_wout)
        mC2 = K.mark()
        ubuf = K.sb([128, S], F32, "ubuf")
        gbuf = K.sb([128, S], F32, "gbuf")
        ucf = K.sb([128, S], F32, "ucf")
        ucb = K.sb([128, S], BF16, "ucb")
        ysq = K.sb([128, S], BF16, "ysq")
        t_u, t_g, t_uc, t_ucb, t_ysq, t_hs = Tok(), Tok(), Tok(), Tok(), Tok(), Tok()
        NB = 2
        r_b = [K.sb([128, L], F32) for _ in range(NB)]
        i_b = [K.sb([128, L], F32) for _ in range(NB)]
        a_b = [K.sb([128, L], F32) for _ in range(NB)]
        a2_b = [K.sb([128, L], F32) for _ in range(NB)]
        h_b = [K.sb([128, L], F32) for _ in range(NB)]
        t_r = [Tok() for _ in range(NB)]
        t_i = [Tok() for _ in range(NB)]
        t_a = [Tok() for _ in range(NB)]
        t_a2 = [Tok() for _ in range(NB)]
        t_h = [Tok() for _ in range(NB)]
        carry = K.sb([128, 2], F32, "carry")
        t_carry = Tok()
        with contextlib.ExitStack() as pes:
            K.pes = pes
            ps_ga = [K.ps([128, 512], F32) for _ in range(2)]
            ps_gx = [K.ps([128, 512], F32) for _ in range(2)]
            t_ga = [Tok(), Tok()]
            t_gx = [Tok(), Tok()]
            ps_ssq = K.ps([128, NT], F32)
            t_pssq = Tok()
            ig = 0
            ib = 0
            for c in range(4):
                cp = lambda k: colp[:, c * 12 + k:c * 12 + k + 1]
                sp.dma([(ubuf[:], lru_scr[c * 128:(c + 1) * 128, :])], writes=[t_u, t_hs])
                sp.dma([(gbuf[:], lru_scr[(4 + c) * 128:(5 + c) * 128, :])], writes=[t_g])
                dve.op(lambda e: e.tensor_scalar(out=ucf[:], in0=ubuf[:], scalar1=cp(2), scalar2=cp(4), op0=ALU.mult, op1=ALU.add),
                       reads=[t_u, t_colp], writes=[t_uc])
                dve.op(lambda e: e.scalar_tensor_tensor(out=ucf[:, 2:S], in0=ubuf[:, 0:S - 2], scalar=cp(0), in1=ucf[:, 2:S],
                                                        op0=ALU.mult, op1=ALU.add), reads=[t_u, t_uc, t_colp], writes=[t_uc])
                dve.op(lambda e: e.scalar_tensor_tensor(out=ucf[:, 1:S], in0=ubuf[:, 0:S - 1], scalar=cp(1), in1=ucf[:, 1:S],
                                                        op0=ALU.mult, op1=ALU.add), reads=[t_u, t_uc, t_colp], writes=[t_uc])
                dve.op(lambda e: e.scalar_tensor_tensor(out=ucf[:, 0:S - 1], in0=ubuf[:, 1:S], scalar=cp(3), in1=ucf[:, 0:S - 1],
                                                        op0=ALU.mult, op1=ALU.add), reads=[t_u, t_uc, t_colp], writes=[t_uc])
                act.op(lambda e: e.activation(out=ucb[:], in_=ucf[:], func=AF.Copy), reads=[t_uc], writes=[t_ucb])
                act.op(lambda e: e.activation(out=gbuf[:], in_=gbuf[:], func=AF.Gelu), reads=[t_g], writes=[t_g])
                hs = ubuf
                for d in range(2):
                    segs = list(range(NSEG)) if d == 0 else list(range(NSEG - 1, -1, -1))
                    wia = (0 * 2 + d) * 4 + c
                    wix = (1 * 2 + d) * 4 + c
                    for si, s in enumerate(segs):
                        bi = ib % NB
                        ib += 1
                        for hf in range(L // 512):
                            cs = slice(s * L + hf * 512, s * L + (hf + 1) * 512)
                            ls = slice(hf * 512, (hf + 1) * 512)
                            pg = ig % 2
                            ig += 1
                            pe.op(lambda e: e.matmul(ps_ga[pg][:], lhsT=Wbd[:, wia, :], rhs=ucb[:, cs], start=True, stop=True),
                                  reads=[t_Wbd, t_ucb], writes=[t_ga[pg]])
                            pe.op(lambda e: e.matmul(ps_gx[pg][:], lhsT=Wbd[:, wix, :], rhs=ucb[:, cs], start=True, stop=True),
                                  reads=[t_Wbd, t_ucb], writes=[t_gx[pg]])
                            act.op(lambda e: e.activation(out=r_b[bi][:, ls], in_=ps_ga[pg][:], func=AF.Sigmoid, bias=cp(5 + d), scale=1.0),
                                   reads=[t_ga[pg], t_colp], writes=[t_r[bi]])
                            act.op(lambda e: e.activation(out=i_b[bi][:, ls], in_=ps_gx[pg][:], func=AF.Sigmoid, bias=cp(7 + d), scale=1.0),
                                   reads=[t_gx[pg], t_colp], writes=[t_i[bi]])
                        act.op(lambda e: e.activation(out=a_b[bi][:], in_=r_b[bi][:], func=AF.Exp, scale=scl[:, c, d:d + 1]),
                               reads=[t_r[bi], t_scl], writes=[t_a[bi]])
                        act.op(lambda e: e.activation(out=a2_b[bi][:], in_=r_b[bi][:], func=AF.Exp, scale=scl2[:, c, d:d + 1]),
                               reads=[t_r[bi], t_scl], writes=[t_a2[bi]])
                        act.op(lambda e: e.activation(out=a2_b[bi][:], in_=a2_b[bi][:], func=AF.Sqrt, bias=one_c[:, 0:1], scale=-1.0),
                               reads=[t_a2[bi], t_one], writes=[t_a2[bi]])
                        dve.op(lambda e: e.tensor_tensor(out=i_b[bi][:], in0=i_b[bi][:], in1=ucf[:, s * L:(s + 1) * L], op=ALU.mult),
                               reads=[t_i[bi], t_uc], writes=[t_i[bi]])
                        dve.op(lambda e: e.tensor_tensor(out=i_b[bi][:], in0=i_b[bi][:], in1=a2_b[bi][:], op=ALU.mult),
                               reads=[t_i[bi], t_a2[bi]], writes=[t_i[bi]])
                        init = 0.0 if si == 0 else carry[:, d:d + 1]
                        if d == 0:
                            dve.op(lambda e: e.tensor_tensor_scan(out=hs[:, s * L:(s + 1) * L], data0=a_b[bi][:], data1=i_b[bi][:],
                                                                  initial=init, op0=ALU.mult, op1=ALU.add),
                                   reads=[t_a[bi], t_i[bi], t_carry, t_uc], writes=[t_hs])
                            dve.op(lambda e: e.tensor_copy(out=carry[:, 0:1], in_=hs[:, (s + 1) * L - 1:(s + 1) * L]),
                                   reads=[t_hs], writes=[t_carry])
                        else:
                            dve.op(lambda e: e.tensor_tensor_scan(out=h_b[bi][:, ::-1], data0=a_b[bi][:, ::-1], data1=i_b[bi][:, ::-1],
                                                                  initial=init, op0=ALU.mult, op1=ALU.add),
                                   reads=[t_a[bi], t_i[bi], t_carry], writes=[t_h[bi]])
                            dve.op(lambda e: e.tensor_copy(out=carry[:, 1:2], in_=h_b[bi][:, 0:1]),
                                   reads=[t_h[bi]], writes=[t_carry])
                            pool.op(lambda e: e.tensor_tensor(out=hs[:, s * L:(s + 1) * L], in0=hs[:, s * L:(s + 1) * L],
                                                              in1=h_b[bi][:], op=ALU.add),
                                    reads=[t_hs, t_h[bi]], writes=[t_hs])
                dve.op(lambda e: e.tensor_tensor(out=hs[:], in0=hs[:], in1=gbuf[:], op=ALU.mult), reads=[t_hs, t_g], writes=[t_hs])
                act.op(lambda e: e.activation(out=yT[:, c, :], in_=hs[:], func=AF.Copy, scale=cp(11)),
                       reads=[t_hs, t_colp], writes=[t_yT])
                act.op(lambda e: e.activation(out=ysq[:], in_=hs[:], func=AF.Square), reads=[t_hs], writes=[t_ysq])
                for t in range(NT):
                    pe.op(lambda e: e.matmul(ps_ssq[:, t:t + 1], lhsT=ysq[:, t * 128:(t + 1) * 128], rhs=ones_bf[:, 0:1],
                                             start=(c == 0 and t == 0), stop=(c == 3)),
                          reads=[t_ysq, t_onesbf], writes=[t_pssq])
            rstd_from_ssq(ps_ssq[:], 512, rstd_rec[:], t_pssq, t_rstd_rec, None)
        K.barrier()
        K.release(mC2)
        if stop_after == "C":
            return nc
        hole_off = mC
        wq_sb = K.sb([128, 8, D], BF16, "wq")
        wg_sb = K.sb([128, 8, D], BF16, "wg")
        wp_sb = K.sb([128, 2, D], BF16, "wp")
        t_wq, t_wg, t_wp = Tok(), Tok(), Tok()
        cast_load(wq_sb, wq_d, 8, D, t_wq)
        cast_load(wg_sb, wg_d, 8, D, t_wg)
        cast_load(wp_sb, wp_d, 2, D, t_wp)
        gffn = K.sb([128, D], F32)
        gple = K.sb([128, D], F32)
        gfin = K.sb([128, D], F32)
        t_gffn, t_gple, t_gfin = Tok(), Tok(), Tok()
        bcast_load(gffn[:], gffn_d[0], t_gffn)
        bcast_load(gple[:], gple_d[0], t_gple)
        bcast_load(gfin[:], gfin_d[0], t_gfin)

        mD = K.mark()

        xtd = [K.sb([128, D], F32) for _ in range(2)]
        t_xtd = [Tok(), Tok()]
        h1o = [K.sb([128, D], F32) for _ in range(2)]
        t_h1o = [Tok(), Tok()]
        with contextlib.ExitStack() as pes:
            K.pes = pes
            ps_at = [[K.ps([128, 512], F32) for _ in range(2)] for _ in range(2)]
            ps_rc = [[K.ps([128, 512], F32) for _ in range(2)] for _ in range(2)]
            t_at = [[Tok(), Tok()], [Tok(), Tok()]]
            t_rc = [[Tok(), Tok()], [Tok(), Tok()]]
            for t in range(NT):
                b = t % 2
                ts_ = slice(t * 128, (t + 1) * 128)
                if t == 0:
                    sp.dma([(xtd[0][:], x_d[0:128, :])], writes=[t_xtd[0]])
                if t + 1 < NT:
                    sp.dma([(xtd[1 - b][:], x_d[(t + 1) * 128:(t + 2) * 128, :])], writes=[t_xtd[1 - b]])
                for n in range(2):
                    ns = slice(n * 512, (n + 1) * 512)
                    for kc in range(4):
                        pe.op(lambda e: e.matmul(ps_at[b][n][:], lhsT=attnT[:, kc, ts_], rhs=w_out_sb[:, kc, ns],
                                                 start=(kc == 0), stop=(kc == 3)),
                              reads=[t_attnT, t_wout], writes=[t_at[b][n]])
                    for kc in range(4):
                        pe.op(lambda e: e.matmul(ps_rc[b][n][:], lhsT=yT[:, kc, ts_], rhs=w_out_sb[:, 4 + kc, ns],
                                                 start=(kc == 0), stop=(kc == 3)),
                              reads=[t_yT, t_wout], writes=[t_rc[b][n]])
                    dve.op(lambda e: e.scalar_tensor_tensor(out=h1o[b][:, ns], in0=ps_rc[b][n][:], scalar=rstd_rec[:, t:t + 1],
                                                            in1=xtd[b][:, ns], op0=ALU.mult, op1=ALU.add),
                           reads=[t_rc[b][n], t_rstd_rec, t_xtd[b]], writes=[t_h1o[b]])
                    dve.op(lambda e: e.tensor_tensor(out=h1o[b][:, ns], in0=ps_at[b][n][:], in1=h1o[b][:, ns], op=ALU.add),
                           reads=[t_at[b][n], t_h1o[b]], writes=[t_h1o[b]])
                sp.dma([(h1_scr[ts_, :], h1o[b][:])], reads=[t_h1o[b]])
        K.barrier()
        K.release(mD)
        if stop_after == "D":
            return nc

        h1t = [K.sb([128, D], F32) for _ in range(3)]
        ptl = [K.sb([128, 256], F32) for _ in range(3)]
        t_h1t = [Tok() for _ in range(3)]
        t_ptl = [Tok() for _ in range(3)]
        xn2b = [K.sb([128, D], BF16) for _ in range(2)]
        junkb = K.sb([128, D], BF16)
        t_junkb = Tok()
        junk_sq = K.sb([128, D], BF16)
        t_junk_sq = Tok()
        xTF = K.sb([128, 8, 128], BF16)
        xTT = K.sb([128, 8, 128], BF16)
        qTs = K.sb([128, 8, 128], BF16)
        s_sb = K.sb([128, 8, 256], F32)
        t_xn2b = [Tok(), Tok()]
        t_xTF, t_xTT, t_qTs = Tok(), Tok(), Tok()
        t_u = [[Tok(), Tok()] for _ in range(8)]
        t_c = [Tok() for _ in range(8)]
        ssF = K.sb([128, 1], F32)
        rsF = K.sb([128, 1], F32)
        ssT = K.sb([128, 1], F32)
        rsT = K.sb([128, 1], F32)
        t_ssF, t_rsF, t_ssT, t_rsT = Tok(), Tok(), Tok(), Tok()
        v12 = [K.sb([128, 8, 16], F32) for _ in range(2)]
        i12 = [K.sb([128, 8, 16], U32) for _ in range(2)]
        i12f = [K.sb([128, 8, 16], BF16) for _ in range(2)]
        t_tk = Tok()
        cand = K.sb([128, 8, 16, 16], F32)
        scv = K.sb([128, 8, 16], F32)
        posu = K.sb([128, 8, 16], U32)
        pa_i = K.sb([128, 128], I32)
        pa_f = K.sb([128, 2, 128], F32)
        oh = K.sb([128, 128, 16], BF16)
        isel = K.sb([128, 2, 128], BF16)
        idxf = K.sb([128, 128], F32)
        idxi = [K.sb([128, 128], I32) for _ in range(2)]
        t_idx = [Tok(), Tok()]
        gw = [K.sb([128, 8, 16], F32) for _ in range(2)]
        t_gw = [Tok(), Tok()]
        zs = K.sb([128, 8], F32)
        logits = K.sb([128, 128], F32)
        actv = K.sb([128, 128], F32)
        gl4 = K.sb([128, 128], F32)
        NBUF = 14
        GS = 4
        gbufs = [K.sb([128, 2 * D], BF16, at=hole_off + k_ * 4096) for k_ in range(4)] + [K.sb([128, 2 * D], BF16) for _ in range(NBUF - 4)]
        t_gb = [Tok() for _ in range(NBUF)]
        NDG = 4
        diag = [K.sb([128, 128], BF16) for _ in range(NDG)]
        t_diag = [Tok() for _ in range(NDG)]
        t_lg = [Tok() for _ in range(128)]
        t_av = [Tok() for _ in range(128)]
        h2 = [K.sb([128, D], F32) for _ in range(2)]
        t_h2 = [Tok(), Tok()]
        xn3b = K.sb([128, D], BF16)
        gate_sb = K.sb([128, D], F32)
        ptb = K.sb([128, 256], BF16)
        pTs = K.sb([128, 2, 128], BF16)
        h3 = K.sb([128, D], F32)
        outt = [K.sb([128, D], F32) for _ in range(2)]
        t_xn3b, t_gate, t_ptb, t_pTs, t_h3 = Tok(), Tok(), Tok(), Tok(), Tok()
        t_outt = [Tok(), Tok()]

        def gap(n):
            for _ in range(n):
                yield

        def rmsnorm(src, t_src, gain, t_gain, dst, t_dst, ss, rs, t_ss, t_rs):
            act.op(lambda e: e.activation(out=junk_sq[:], in_=src[:], func=AF.Square, accum_out=ss[:, 0:1]),
                   reads=[t_src], writes=[t_ss, t_junk_sq])
            yield from gap(3)
            dve.op(lambda e: e.tensor_scalar(out=rs[:], in0=ss[:], scalar1=1.0 / D, scalar2=EPS, op0=ALU.mult, op1=ALU.add),
                   reads=[t_ss], writes=[t_rs])
            yield from gap(2)
            act.op(lambda e: e.activation(out=rs[:], in_=rs[:], func=AF.Sqrt), reads=[t_rs], writes=[t_rs])
            yield from gap(3)
            dve.op(lambda e: e.reciprocal(out=rs[:], in_=rs[:]), reads=[t_rs], writes=[t_rs])
            yield
            dve.op(lambda e: e.scalar_tensor_tensor(out=dst[:], in0=src[:], scalar=rs[:, 0:1], in1=gain[:],
                                                    op0=ALU.mult, op1=ALU.add if False else ALU.mult),
                   reads=[t_src, t_rs, t_gain], writes=[t_dst])
            yield

        with contextlib.ExitStack() as pes:
            K.pes = pes
            ps_tF = K.ps([128, 8, 128], BF16)
            ps_tT = K.ps([128, 8, 128], BF16)
            t_pstF, t_pstT = Tok(), Tok()
            ps_q = K.ps([128, 4, 128], F32)
            t_psq = Tok()
            ps_sc = K.ps([128, 2, 256], F32)
            t_psc = Tok()
            ps_g = [K.ps([128, 512], F32) for _ in range(2)]
            t_psg = [Tok(), Tok()]
            ps_v = [K.ps([128, 512], F32) for _ in range(2)]
            t_psv = [Tok(), Tok()]

            def front(t):
                b3 = t % 3
                b = t % 2
                ts_ = slice(t * 128, (t + 1) * 128)
                sp.dma([(h1t[b3][:], h1_scr[ts_, :])], writes=[t_h1t[b3]])
                sp.dma([(ptl[b3][:], p_d[ts_, :])], writes=[t_ptl[b3]])
                yield from gap(3)
                yield from rmsnorm(h1t[b3], t_h1t[b3], gffn, t_gffn, xn2b[b], t_xn2b[b], ssF, rsF, t_ssF, t_rsF)
                yield from gap(2)
                for kc in range(8):
                    pe.op(lambda e: e.transpose(out=ps_tF[:, kc, :], in_=xn2b[b][:, kc * 128:(kc + 1) * 128], identity=ident[:]),
                          reads=[t_xn2b[b], t_id], writes=[t_pstF])
                yield from gap(2)
                act.op(lambda e: e.activation(out=xTF[:], in_=ps_tF[:], func=AF.Copy), reads=[t_pstF], writes=[t_xTF])
                yield from gap(2)
                for hh in range(2):
                    for c4 in range(4):
                        cc = hh * 4 + c4
                        for kc in range(8):
                            pe.op(lambda e: e.matmul(ps_q[:, c4, :], lhsT=wq_sb[:, kc, cc * 128:(cc + 1) * 128],
                                                     rhs=xTF[:, kc, :], start=(kc == 0 and c4 == 0), stop=(kc == 7)),
                                  reads=[t_wq, t_xTF], writes=[t_psq])
                        if c4 % 2 == 1:
                            yield
                    yield from gap(2)
                    act.op(lambda e: e.activation(out=qTs[:, hh * 4:(hh + 1) * 4, :], in_=ps_q[:], func=AF.Copy),
                           reads=[t_psq], writes=[t_qTs])
                    yield from gap(2)
                for hp in range(4):
                    for k2 in range(2):
                        hd = hp * 2 + k2
                        pe.op(lambda e: e.matmul(ps_sc[:, k2, :], lhsT=qTs[:, hd, :], rhs=keysbd[:], start=True, stop=True),
                              reads=[t_qTs, t_keys], writes=[t_psc])
                    yield from gap(2)
                    act.op(lambda e: e.activation(out=s_sb[:, hp * 2:(hp + 1) * 2, :], in_=ps_sc[:], func=AF.Copy),
                           reads=[t_psc], writes=[t_u[hp * 2][0], t_u[hp * 2][1], t_u[hp * 2 + 1][0], t_u[hp * 2 + 1][1]])
                    yield
                mode["heavy"] = True
                units = [(hd, sd) for hd in range(8) for sd in range(2)]
                srcu = lambda hd, sd: s_sb[:, hd, sd * 128:(sd + 1) * 128]
                for rnd in range(2):
                    ks = slice(rnd * 8, rnd * 8 + 8)
                    for k_, (hd, sd) in enumerate(units):
                        dve.op(lambda e: e.max(out=v12[sd][:, hd, ks], in_=srcu(hd, sd)), reads=[t_u[hd][sd]], writes=[t_u[hd][sd]])
                        if k_ % 2 == 1:
                            yield
                    for k_, (hd, sd) in enumerate(units):
                        dve.op(lambda e: e.max_index(out=i12[sd][:, hd, ks], in_max=v12[sd][:, hd, ks], in_values=srcu(hd, sd)),
                               reads=[t_u[hd][sd]], writes=[t_u[hd][sd]])
                        if k_ % 2 == 1:
                            yield
                    if rnd == 0:
                        for k_, (hd, sd) in enumerate(units):
                            dve.op(lambda e: e.match_replace(out=srcu(hd, sd), in_to_replace=v12[sd][:, hd, ks], in_values=srcu(hd, sd),
                                                             imm_value=-1e30), reads=[t_u[hd][sd]], writes=[t_u[hd][sd]])
                            if k_ % 2 == 1:
                                yield
                for hd in range(8):
                    dve.op(lambda e: e.tensor_tensor(out=cand[:, hd], in0=v12[0][:, hd, :].unsqueeze(2).broadcast_to([128, 16, 16]),
                                                     in1=v12[1][:, hd, :].unsqueeze(1).broadcast_to([128, 16, 16]), op=ALU.add),
                           reads=[t_u[hd][0], t_u[hd][1]], writes=[t_c[hd]])
                    if hd % 2 == 1:
                        yield
                cfu = lambda hd: cand[:, hd].rearrange("p a b -> p (a b)")
                for rnd in range(2):
                    ks = slice(rnd * 8, rnd * 8 + 8)
                    for hd in range(8):
                        dve.op(lambda e: e.max(out=scv[:, hd, ks], in_=cfu(hd)), reads=[t_c[hd]], writes=[t_c[hd]])
                        if hd % 2 == 1:
                            yield
                    for hd in range(8):
                        dve.op(lambda e: e.max_index(out=posu[:, hd, ks], in_max=scv[:, hd, ks], in_values=cfu(hd)),
                               reads=[t_c[hd]], writes=[t_c[hd]])
                        if hd % 2 == 1:
                            yield
                    if rnd == 0:
                        for hd in range(8):
                            dve.op(lambda e: e.match_replace(out=cfu(hd), in_to_replace=scv[:, hd, ks], in_values=cfu(hd), imm_value=-1e30),
                                   reads=[t_c[hd]], writes=[t_c[hd]])
                            if hd % 2 == 1:
                                yield
                dve.op(lambda e: e.tensor_copy(out=zs[:, 0:1], in_=scv[:, 0, 0:1]), reads=t_c + [t_u[h_][s_] for h_ in range(8) for s_ in range(2)],
                       writes=[t_tk])
                posf_ = posu[:].rearrange("p h k -> p (h k)").bitcast(I32)
                for sd in range(2):
                    if sd == 0:
                        dve.op(lambda e: e.tensor_single_scalar(out=pa_i[:], in_=posf_, scalar=4, op=ALU.arith_shift_right),
                               reads=[t_tk], writes=[t_tk])
                    else:
                        dve.op(lambda e: e.tensor_single_scalar(out=pa_i[:], in_=posf_, scalar=15, op=ALU.bitwise_and),
                               reads=[t_tk], writes=[t_tk])
                    dve.op(lambda e: e.tensor_copy(out=pa_f[:, sd, :], in_=pa_i[:]), reads=[t_tk], writes=[t_tk])
                    dve.op(lambda e: e.tensor_copy(out=i12f[sd][:], in_=i12[sd][:].bitcast(I32)), reads=[t_tk], writes=[t_tk])
                    yield
                    dve.op(lambda e: e.tensor_tensor(out=oh[:], in0=pa_f[:, sd, :].unsqueeze(2).broadcast_to([128, 128, 16]),
                                                     in1=iota16.unsqueeze(1).broadcast_to([128, 128, 16]), op=ALU.is_equal),
                           reads=[t_tk, t_cst], writes=[t_tk])
                    yield
                    oh4 = oh[:].rearrange("p (h s) a -> p h s a", h=8)
                    dve.op(lambda e: e.tensor_tensor(out=oh4, in0=oh4, in1=i12f[sd][:].unsqueeze(2).broadcast_to([128, 8, 16, 16]), op=ALU.mult),
                           reads=[t_tk], writes=[t_tk])
                    yield
                    with nc.allow_low_precision("sum over a one-hot row of an integer id <= 127: exact in bf16"):
                        dve.op(lambda e: e.tensor_reduce(out=isel[:, sd, :], in_=oh[:], axis=AX.X, op=ALU.add), reads=[t_tk], writes=[t_tk])
                    yield
                dve.op(lambda e: e.scalar_tensor_tensor(out=idxf[:], in0=isel[:, 0, :], scalar=128.0, in1=isel[:, 1, :],
                                                        op0=ALU.mult, op1=ALU.add), reads=[t_tk], writes=[t_tk])
                dve.op(lambda e: e.tensor_copy(out=idxi[b][:], in_=idxf[:]), reads=[t_tk], writes=[t_idx[b]])
                mode["heavy"] = False
                yield
                dve.op(lambda e: e.tensor_tensor(out=gw[b][:], in0=scv[:], in1=scv[:, :, 0:1].broadcast_to([128, 8, 16]), op=ALU.subtract),
                       reads=[t_tk], writes=[t_gw[b]])
                act.op(lambda e: e.activation(out=gw[b][:], in_=gw[b][:], func=AF.Exp), reads=[t_gw[b]], writes=[t_gw[b]])
                yield
                dve.op(lambda e: e.tensor_reduce(out=zs[:], in_=gw[b][:], axis=AX.X, op=ALU.add), reads=[t_gw[b]], writes=[t_tk])
                dve.op(lambda e: e.reciprocal(out=zs[:], in_=zs[:]), reads=[t_tk], writes=[t_tk])
                dve.op(lambda e: e.tensor_tensor(out=gw[b][:], in0=gw[b][:], in1=zs[:].unsqueeze(2).broadcast_to([128, 8, 16]), op=ALU.mult),
                       reads=[t_gw[b], t_tk], writes=[t_gw[b]])
                yield

            def tail(t):
                b3 = t % 3
                b = t % 2
                ts_ = slice(t * 128, (t + 1) * 128)
                yield from rmsnorm(h2[b], t_h2[b], gple, t_gple, xn3b, t_xn3b, ssT, rsT, t_ssT, t_rsT)
                yield from gap(1)
                for kc in range(8):
                    pe.op(lambda e: e.transpose(out=ps_tT[:, kc, :], in_=xn3b[:, kc * 128:(kc + 1) * 128], identity=ident[:]),
                          reads=[t_xn3b, t_id], writes=[t_pstT])
                yield from gap(2)
                act.op(lambda e: e.activation(out=xTT[:], in_=ps_tT[:], func=AF.Copy), reads=[t_pstT], writes=[t_xTT])
                yield from gap(2)
                for n in range(2):
                    for kc in range(8):
                        pe.op(lambda e: e.matmul(ps_g[n][:], lhsT=xTT[:, kc, :], rhs=wg_sb[:, kc, n * 512:(n + 1) * 512],
                                                 start=(kc == 0), stop=(kc == 7)), reads=[t_xTT, t_wg], writes=[t_psg[n]])
                    yield
                yield from gap(2)
                for n in range(2):
                    act.op(lambda e: e.activation(out=gate_sb[:, n * 512:(n + 1) * 512], in_=ps_g[n][:], func=AF.Sigmoid),
                           reads=[t_psg[n]], writes=[t_gate])
                act.op(lambda e: e.activation(out=ptb[:], in_=ptl[b3][:], func=AF.Copy), reads=[t_ptl[b3]], writes=[t_ptb])
                yield from gap(2)
                for kc in range(2):
                    pe.op(lambda e: e.transpose(out=ps_tT[:, kc, :], in_=ptb[:, kc * 128:(kc + 1) * 128], identity=ident[:]),
                          reads=[t_ptb, t_id], writes=[t_pstT])
                yield from gap(2)
                act.op(lambda e: e.activation(out=pTs[:], in_=ps_tT[:, 0:2, :], func=AF.Copy), reads=[t_pstT], writes=[t_pTs])
                yield from gap(2)
                for n in range(2):
                    ns = slice(n * 512, (n + 1) * 512)
                    for kc in range(2):
                        pe.op(lambda e: e.matmul(ps_g[n][:], lhsT=pTs[:, kc, :], rhs=wp_sb[:, kc, ns],
                                                 start=(kc == 0), stop=(kc == 1)), reads=[t_pTs, t_wp], writes=[t_psg[n]])
                yield from gap(2)
                for n in range(2):
                    ns = slice(n * 512, (n + 1) * 512)
                    dve.op(lambda e: e.tensor_tensor(out=h3[:, ns], in0=ps_g[n][:], in1=gate_sb[:, ns], op=ALU.mult),
                           reads=[t_psg[n], t_gate], writes=[t_h3])
                    yield
                dve.op(lambda e: e.tensor_tensor(out=h3[:], in0=h3[:], in1=h2[b][:], op=ALU.add), reads=[t_h3, t_h2[b]], writes=[t_h3])
                yield
                yield from rmsnorm(h3, t_h3, gfin, t_gfin, outt[b], t_outt[b], ssT, rsT, t_ssT, t_rsT)
                sp.dma([(out_d[ts_, :], outt[b][:])], reads=[t_outt[b]])
                yield

            gi_state = {"gi": 0}

            TAIL_START = 0

            class _Late:
                _late = True

                def __init__(self, g):
                    self.g = g

                def __iter__(self):
                    return self

                def __next__(self):
                    return next(self.g)

            mode = {"heavy": False}

            def slots(t, gens):
                b = t % 2
                b3 = t % 3
                gwf = gw[b][:].rearrange("p h k -> p (h k)")
                lightq = []

                for sl in range(128):
                    gb = gi_state["gi"] % NBUF
                    gi_state["gi"] += 1
                    dg = sl % NDG
                    pool.dma([(gbufs[gb][:], puv_bf)], reads=[t_idx[b], t_tab], writes=[t_gb[gb]],
                             fn=lambda e, o, i: e.indirect_dma_start(out=o, out_offset=None, in_=i,
                                                                      in_offset=bass.IndirectOffsetOnAxis(ap=idxi[b][:, sl:sl + 1], axis=0)))
                    dve.op(lambda e: e.scalar_tensor_tensor(out=junkb[:], in0=gbufs[gb][:, 0:D], scalar=1.0, in1=xn2b[b][:], op0=ALU.mult, op1=ALU.mult,
                                                            accum_out=logits[:, sl:sl + 1]),
                           reads=[t_gb[gb], t_xn2b[b]], writes=[t_lg[sl], t_junkb])
                    act.op(lambda e: e.activation(out=gl4[:, sl:sl + 1], in_=logits[:, sl:sl + 1], func=AF.Gelu),
                           reads=[t_lg[sl]], writes=[t_av[sl]])

                    def finish_act(s_, gb_):
                        dg_ = s_ % NDG
                        act.op(lambda e: e.activation(out=actv[:, s_:s_ + 1], in_=gl4[:, s_:s_ + 1], func=AF.Copy, scale=gwf[:, s_:s_ + 1]),
                               reads=[t_av[s_], t_gw[b]], writes=[t_av[s_]])
                        act.op(lambda e: e.activation(out=diag[dg_][:], in_=ident[:], func=AF.Copy, scale=actv[:, s_:s_ + 1]),
                               reads=[t_id, t_av[s_]], writes=[t_diag[dg_]])
                        mm(s_, gb_, dg_)

                    def finish_dve(s_, gb_):
                        dg_ = s_ % NDG
                        dve.op(lambda e: e.tensor_scalar(out=diag[dg_][:], in0=ident[:], scalar1=gl4[:, s_:s_ + 1], scalar2=gwf[:, s_:s_ + 1],
                                                         op0=ALU.mult, op1=ALU.mult),
                               reads=[t_id, t_av[s_], t_gw[b]], writes=[t_diag[dg_]])
                        mm(s_, gb_, dg_)

                    def mm(s_, gb_, dg_):
                        for n in range(2):
                            pe.op(lambda e: e.matmul(ps_v[n][:], lhsT=diag[dg_][:], rhs=gbufs[gb_][:, D + n * 512:D + (n + 1) * 512],
                                                     start=(s_ == 0), stop=False),
                                  reads=[t_diag[dg_], t_gb[gb_]], writes=[t_psv[n]])

                    if mode["heavy"]:
                        for p_ in lightq:
                            finish_dve(*p_)
                        del lightq[:]
                        finish_act(sl, gb)
                    else:
                        for p_ in lightq:
                            finish_dve(*p_)
                        del lightq[:]
                        lightq.append((sl, gb))
                    for gen in gens:
                        if getattr(gen, "_late", False) and sl < TAIL_START:
                            continue
                        next(gen, None)
                for p_ in lightq:
                    finish_dve(*p_)
                del lightq[:]
                for gen in gens:
                    for _ in gen:
                        pass
                for n in range(2):
                    ns = slice(n * 512, (n + 1) * 512)
                    dve.op(lambda e: e.tensor_tensor(out=h2[b][:, ns], in0=ps_v[n][:], in1=h1t[b3][:, ns], op=ALU.add),
                           reads=[t_psv[n], t_h1t[b3]], writes=[t_h2[b]])

            for _ in front(0):
                pass
            for t in range(NT):
                gens = []
                if t + 1 < NT:
                    gens.append(front(t + 1))
                if t >= 1:
                    tg = _Late(tail(t - 1))
                    gens.append(tg)
                slots(t, gens)
            for _ in tail(NT - 1):
                pass
        K.barrier()
        print("SBUF peak bytes/partition", K.sb_peak - SB_LO, "of", SB_HI - SB_LO)
    return nc


def _consts():
    c = np.zeros((128, 152), np.float32)
    c[:, 0:128] = np.eye(128, dtype=np.float32)
    c[:, 128:144] = np.arange(16, dtype=np.float32)[None, :]
    inv = (np.float32(500000.0) ** (-np.arange(0, 16, 2, dtype=np.float32) / np.float32(16))).astype(np.float32)
    c[:, 144:152] = inv[None, :]
    return c


def make_in_maps(inputs, S):
    f = lambda a: np.ascontiguousarray(np.asarray(a))
    x = f(inputs["x"])
    B = x.shape[0]
    NT = S // 128
    conv_w = f(inputs["conv_w"])[0]
    cols = []
    for c in range(4):
        sl = slice(c * 128, (c + 1) * 128)
        ks = [conv_w[0, sl], conv_w[1, sl], conv_w[2, sl], conv_w[3, sl], f(inputs["conv_b"])[0, sl],
              f(inputs["lru_ba"])[0, 0, sl], f(inputs["lru_ba"])[0, 1, sl],
              f(inputs["lru_bx"])[0, 0, sl], f(inputs["lru_bx"])[0, 1, sl],
              f(inputs["lru_lambda"])[0, 0, sl], f(inputs["lru_lambda"])[0, 1, sl],
              f(inputs["lru_norm_g"])[0, sl]]
        cols.extend(ks)
    colp = np.ascontiguousarray(np.stack(cols, axis=1).astype(np.float32))
    lam4 = np.ascontiguousarray(np.concatenate([f(inputs["lambda_q1"])[0], f(inputs["lambda_k1"])[0],
                                                f(inputs["lambda_q2"])[0], f(inputs["lambda_k2"])[0]])[None, :])
    shared = {
        "consts": _consts(),
        "w_in": f(inputs["w_in"])[0], "w_out": f(inputs["w_out"])[0], "peer_wq": f(inputs["peer_wq"])[0],
        "ple_w_gate": f(inputs["ple_w_gate"])[0], "ple_w_proj": f(inputs["ple_w_proj"])[0],
        "norm_mix_g": f(inputs["norm_mix_g"]), "norm_ffn_g": f(inputs["norm_ffn_g"]),
        "norm_ple_g": f(inputs["norm_ple_g"]), "final_norm_g": f(inputs["final_norm_g"])[None, :],
        "diff_norm_g": f(inputs["diff_norm_g"]), "lam4": lam4, "colp": colp,
        "lru_wa": f(inputs["lru_wa"])[0], "lru_wx": f(inputs["lru_wx"])[0],
        "keys1T": np.ascontiguousarray(f(inputs["peer_keys1"])[0].T), "keys2T": np.ascontiguousarray(f(inputs["peer_keys2"])[0].T),
        "peer_u": f(inputs["peer_u"])[0], "peer_v": f(inputs["peer_v"])[0],
    }
    maps = []
    for bi in range(B):
        m = dict(shared)
        m["x"] = np.ascontiguousarray(x[bi])
        m["p"] = np.ascontiguousarray(f(inputs["p"])[0, bi])
        m["pos"] = np.ascontiguousarray(f(inputs["positions"])[bi].astype(np.int32).reshape(NT, 128).T)
        maps.append(m)
    return maps


_NC_CACHE = {}


def kernel(**inputs):
    x = np.asarray(inputs["x"])
    B, S, _ = x.shape
    if S not in _NC_CACHE:
        _NC_CACHE[S] = build(S)
    nc = _NC_CACHE[S]
    maps = make_in_maps(inputs, S)
    res = run_bass_kernel_spmd(nc, maps, core_ids=list(range(B)))
    out = np.stack([np.asarray(r["out"]) for r in res.results], axis=0).astype(np.float32)
    return out
```
